# Optimizing a Trainium2 kernel written in Bass

```python
import jax
import jax.numpy as jnp
from jax import lax
import numpy as np

D_MODEL = 1024
BATCH = 8
SEQ = 4096
DEPTH = 4

HEAD_DIM = 64
FOX_HEADS = 4
SWA_HEADS = 8
SWA_KV_HEADS = 2
SWA_WINDOW = 128
MOBA_HEADS = 4
MOBA_BLOCK = 256
MOBA_TOPK = 3
MOBA_Q_CHUNK = 64
Q_BLOCK = 128
N_BRANCH = 3
ALIBI_HEADS = SWA_HEADS + MOBA_HEADS
N_EXPERTS = 32
TOP_K = 4
D_EXPERT = D_MODEL
SWIGLU_ALPHA = 1.702
SWIGLU_LIMIT = 7.0
EPS = 1e-6
NEG = -1e30
W_FOX = FOX_HEADS * HEAD_DIM
W_SWA = SWA_HEADS * HEAD_DIM
W_SWA_KV = SWA_KV_HEADS * HEAD_DIM
W_MOBA = MOBA_HEADS * HEAD_DIM
IN_SPLITS = (W_FOX, W_FOX, W_FOX, FOX_HEADS, W_SWA, W_SWA_KV, W_SWA_KV, W_MOBA, W_MOBA, W_MOBA, N_BRANCH * D_MODEL)
D_IN = sum(IN_SPLITS)

kernel_name = 'hybrid_fox_swa_moba_moe_block'


def rms_norm(x, gain):
    xf = x.astype(jnp.float32)
    y = xf * lax.rsqrt(jnp.mean(xf * xf, axis=-1, keepdims=True) + EPS)
    return (y * gain.astype(jnp.float32)).astype(x.dtype)


def alibi_slopes(n):
    return jnp.exp2(-8.0 * jnp.arange(1, n + 1, dtype=jnp.float32) / n)


def fox_attention(q, k, v, log_f):
    b, s, h, d = q.shape
    nq = s // Q_BLOCK
    cum = jnp.cumsum(log_f, axis=1).transpose(0, 2, 1)
    q_blocks = q.reshape(b, nq, Q_BLOCK, h, d).transpose(1, 0, 2, 3, 4)
    c_blocks = cum.reshape(b, h, nq, Q_BLOCK).transpose(2, 0, 1, 3)
    k_pos = jnp.arange(s)

    def block(args):
        i, q_i, c_i = args
        logits = jnp.einsum('bqhd,bkhd->bhqk', q_i, k, preferred_element_type=jnp.float32) * HEAD_DIM ** -0.5
        logits = logits + (c_i[..., :, None] - cum[:, :, None, :])
        q_pos = i * Q_BLOCK + jnp.arange(Q_BLOCK)
        logits = jnp.where(k_pos[None, :] <= q_pos[:, None], logits, NEG)
        p = jax.nn.softmax(logits, axis=-1).astype(v.dtype)
        return jnp.einsum('bhqk,bkhd->bqhd', p, v)

    out = lax.map(block, (jnp.arange(nq), q_blocks, c_blocks))
    return out.transpose(1, 0, 2, 3, 4).reshape(b, s, h, d)


def swa_attention(q, k, v, sinks, slopes):
    b, s, hq, d = q.shape
    hkv = k.shape[2]
    g = hq // hkv
    w = SWA_WINDOW
    nb = s // w
    q_b = q.reshape(b, nb, w, hkv, g, d)

    def band(t):
        t_b = t.reshape(b, nb, w, hkv, d)
        prev = jnp.pad(t_b, ((0, 0), (1, 0), (0, 0), (0, 0), (0, 0)))[:, :-1]
        return jnp.concatenate([prev, t_b], axis=2)

    k_b, v_b = band(k), band(v)
    logits = jnp.einsum('bnqhgd,bnkhd->bnhgqk', q_b, k_b, preferred_element_type=jnp.float32) * HEAD_DIM ** -0.5
    dist = (jnp.arange(w)[:, None] + w - jnp.arange(2 * w)[None, :]).astype(jnp.float32)
    in_window = (dist >= 0) & (dist < w)
    has_prev = (jnp.arange(nb) > 0)[:, None, None] | (jnp.arange(2 * w) >= w)[None, None, :]
    valid = in_window[None] & has_prev
    logits = logits - slopes.reshape(hkv, g, 1, 1) * dist
    logits = jnp.where(valid[None, :, None, None], logits, NEG)
    sink = jnp.broadcast_to(sinks.astype(jnp.float32).reshape(hkv, g, 1, 1), logits.shape[:-1] + (1,))
    p = jax.nn.softmax(jnp.concatenate([logits, sink], axis=-1), axis=-1)[..., :-1].astype(v.dtype)
    out = jnp.einsum('bnhgqk,bnkhd->bnqhgd', p, v_b)
    return out.reshape(b, s, hq, d)


def moba_attention(q, k, v, slopes):
    b, s, h, d = q.shape
    n_blk = max(-(-s // MOBA_BLOCK), MOBA_TOPK)
    s_pad = n_blk * MOBA_BLOCK
    pad = ((0, 0), (0, s_pad - s), (0, 0), (0, 0))
    q, k, v = jnp.pad(q, pad), jnp.pad(k, pad), jnp.pad(v, pad)
    k_blk = k.reshape(b, n_blk, MOBA_BLOCK, h, d).transpose(0, 3, 1, 2, 4)
    v_blk = v.reshape(b, n_blk, MOBA_BLOCK, h, d).transpose(0, 3, 1, 2, 4)
    k_mean = jnp.mean(k_blk.astype(jnp.float32), axis=3)
    nc = s_pad // MOBA_Q_CHUNK
    q_chunks = q.reshape(b, nc, MOBA_Q_CHUNK, h, d).transpose(1, 0, 3, 2, 4)
    gather = jax.vmap(jax.vmap(lambda blocks, ix: blocks[ix]))
    offsets = jnp.arange(MOBA_BLOCK)
    n_sel = MOBA_TOPK * MOBA_BLOCK
    scale = HEAD_DIM ** -0.5

    def chunk(args):
        i, q_i = args
        q_pos = i * MOBA_Q_CHUNK + jnp.arange(MOBA_Q_CHUNK)
        own = (i * MOBA_Q_CHUNK) // MOBA_BLOCK
        gate = jnp.einsum('bhqd,bhnd->bhqn', q_i.astype(jnp.float32), k_mean)
        gate = jnp.where(jnp.arange(n_blk) < own, gate, NEG)
        _, idx = lax.top_k(gate, MOBA_TOPK)
        k_sel = gather(k_blk, idx)
        v_sel = gather(v_blk, idx)
        l_sel = jnp.einsum('bhqd,bhqjkd->bhqjk', q_i, k_sel, preferred_element_type=jnp.float32) * scale
        dist_sel = (q_pos[:, None, None] - (idx[..., None] * MOBA_BLOCK + offsets)).astype(jnp.float32)
        l_sel = l_sel - slopes[None, :, None, None, None] * dist_sel
        l_sel = jnp.where((jnp.arange(MOBA_TOPK) < own)[:, None], l_sel, NEG)
        k_own = lax.dynamic_index_in_dim(k_blk, own, axis=2, keepdims=False)
        v_own = lax.dynamic_index_in_dim(v_blk, own, axis=2, keepdims=False)
        l_own = jnp.einsum('bhqd,bhkd->bhqk', q_i, k_own, preferred_element_type=jnp.float32) * scale
        dist_own = (q_pos[:, None] - (own * MOBA_BLOCK + offsets)[None, :]).astype(jnp.float32)
        l_own = jnp.where(dist_own >= 0, l_own - slopes[None, :, None, None] * dist_own, NEG)
        logits = jnp.concatenate([l_sel.reshape(b, h, MOBA_Q_CHUNK, n_sel), l_own], axis=-1)
        p = jax.nn.softmax(logits, axis=-1).astype(v_blk.dtype)
        p_sel = p[..., :n_sel].reshape(b, h, MOBA_Q_CHUNK, MOBA_TOPK, MOBA_BLOCK)
        return (jnp.einsum('bhqjk,bhqjkd->bhqd', p_sel, v_sel)
                + jnp.einsum('bhqk,bhkd->bhqd', p[..., n_sel:], v_own))

    out = lax.map(chunk, (jnp.arange(nc), q_chunks))
    return out.transpose(1, 0, 3, 2, 4).reshape(b, s_pad, h, d)[:, :s]


def hybrid_mixer(h, w_in, b_f, qk_gain, sinks, w_br_fox, w_br_swa, w_br_moba, w_out, slopes_swa, slopes_moba):
    b, s, _ = h.shape
    proj = h @ w_in
    points = np.cumsum(IN_SPLITS)[:-1].tolist()
    qa, ka, va, fa, qb, kb, vb, qc, kc, vc, gates = jnp.split(proj, points, axis=-1)
    heads = lambda t: t.reshape(b, s, -1, HEAD_DIM)
    log_f = jax.nn.log_sigmoid(fa.astype(jnp.float32) + b_f.astype(jnp.float32))
    o_a = fox_attention(rms_norm(heads(qa), qk_gain[0]), rms_norm(heads(ka), qk_gain[1]), heads(va), log_f)
    o_b = swa_attention(rms_norm(heads(qb), qk_gain[2]), rms_norm(heads(kb), qk_gain[3]), heads(vb), sinks, slopes_swa)
    o_c = moba_attention(rms_norm(heads(qc), qk_gain[4]), rms_norm(heads(kc), qk_gain[5]), heads(vc), slopes_moba)
    y_a = o_a.reshape(b, s, W_FOX) @ w_br_fox
    y_b = o_b.reshape(b, s, W_SWA) @ w_br_swa
    y_c = o_c.reshape(b, s, W_MOBA) @ w_br_moba
    g_a, g_b, g_c = jnp.split(jax.nn.sigmoid(gates), N_BRANCH, axis=-1)
    return (g_a * y_a + g_b * y_b + g_c * y_c) @ w_out


def clamped_swiglu(u):
    x_glu, x_lin = u[..., ::2], u[..., 1::2]
    x_glu = jnp.minimum(x_glu, SWIGLU_LIMIT)
    x_lin = jnp.clip(x_lin, -SWIGLU_LIMIT, SWIGLU_LIMIT)
    return x_glu * jax.nn.sigmoid(SWIGLU_ALPHA * x_glu) * (x_lin + 1.0)


def moe_ffn(h, w_router, b_router, w1, b1, w2, b2):
    b, s, d = h.shape
    t = h.reshape(b * s, d)
    logits = (t @ w_router + b_router).astype(jnp.float32)
    top_vals, top_idx = lax.top_k(logits, TOP_K)
    top_w = jax.nn.softmax(top_vals, axis=-1)
    gates = jnp.sum(jax.nn.one_hot(top_idx, N_EXPERTS, dtype=jnp.float32) * top_w[..., None], axis=1).astype(t.dtype)
    out = jnp.zeros_like(t)
    for e in range(N_EXPERTS):
        y = clamped_swiglu(t @ w1[e] + b1[e]) @ w2[e] + b2[e]
        out = out + gates[:, e:e + 1] * y
    return out.reshape(b, s, d)


def setup_inputs(seed: int = 0) -> dict:
    key = jax.random.key(seed)
    ks = jax.random.split(key, 20)
    f32 = jnp.float32
    nrm = lambda k, shape, sc: jax.random.normal(k, shape, f32) * sc
    return {
        'x': nrm(ks[0], (BATCH, SEQ, D_MODEL), 1.0),
        'c': nrm(ks[1], (BATCH, D_MODEL), 1.0),
        'w_ada': nrm(ks[2], (DEPTH, D_MODEL, 6 * D_MODEL), 0.5 * D_MODEL ** -0.5),
        'b_ada': nrm(ks[3], (DEPTH, 6 * D_MODEL), 0.02),
        'norm_gain': 1.0 + nrm(ks[4], (DEPTH, 2, D_MODEL), 0.02),
        'w_in': nrm(ks[5], (DEPTH, D_MODEL, D_IN), D_MODEL ** -0.5),
        'b_fgate': 2.0 + nrm(ks[6], (DEPTH, FOX_HEADS), 0.1),
        'qk_gain': 1.0 + nrm(ks[7], (DEPTH, 6, HEAD_DIM), 0.02),
        'attn_sinks': nrm(ks[8], (DEPTH, SWA_HEADS), 0.5),
        'w_br_fox': nrm(ks[9], (DEPTH, W_FOX, D_MODEL), W_FOX ** -0.5),
        'w_br_swa': nrm(ks[10], (DEPTH, W_SWA, D_MODEL), W_SWA ** -0.5),
        'w_br_moba': nrm(ks[11], (DEPTH, W_MOBA, D_MODEL), W_MOBA ** -0.5),
        'w_out': nrm(ks[12], (DEPTH, D_MODEL, D_MODEL), D_MODEL ** -0.5),
        'w_router': nrm(ks[13], (DEPTH, D_MODEL, N_EXPERTS), D_MODEL ** -0.5),
        'b_router': nrm(ks[14], (DEPTH, N_EXPERTS), 0.01),
        'w_exp1': nrm(ks[15], (DEPTH, N_EXPERTS, D_MODEL, 2 * D_EXPERT), D_MODEL ** -0.5),
        'b_exp1': nrm(ks[16], (DEPTH, N_EXPERTS, 2 * D_EXPERT), 0.01),
        'w_exp2': nrm(ks[17], (DEPTH, N_EXPERTS, D_EXPERT, D_MODEL), D_EXPERT ** -0.5),
        'b_exp2': nrm(ks[18], (DEPTH, N_EXPERTS, D_MODEL), 0.01),
    }


def reference(x, c, w_ada, b_ada, norm_gain, w_in, b_fgate, qk_gain, attn_sinks, w_br_fox, w_br_swa, w_br_moba, w_out, w_router, b_router, w_exp1, b_exp1, w_exp2, b_exp2):
    slopes = alibi_slopes(ALIBI_HEADS)
    slopes_swa, slopes_moba = slopes[:SWA_HEADS], slopes[SWA_HEADS:]
    cond = jax.nn.silu(c)
    for l in range(DEPTH):
        mod = cond @ w_ada[l] + b_ada[l]
        sh1, sc1, g1, sh2, sc2, g2 = jnp.split(mod[:, None, :], 6, axis=-1)
        h = rms_norm(x, norm_gain[l, 0]) * (1.0 + sc1) + sh1
        x = x + g1 * hybrid_mixer(h, w_in[l], b_fgate[l], qk_gain[l], attn_sinks[l], w_br_fox[l], w_br_swa[l], w_br_moba[l], w_out[l], slopes_swa, slopes_moba)
        h = rms_norm(x, norm_gain[l, 1]) * (1.0 + sc2) + sh2
        x = x + g2 * moe_ffn(h, w_router[l], b_router[l], w_exp1[l], b_exp1[l], w_exp2[l], b_exp2[l])
    return x
```

```python
import numpy as np
from types import SimpleNamespace
import concourse.bass as bass
import concourse.mybir as mybir
from concourse.bass_utils import run_bass_kernel_spmd

F32 = mybir.dt.float32
BF16 = mybir.dt.bfloat16
AF = mybir.ActivationFunctionType
ALU = mybir.AluOpType
AX = mybir.AxisListType

D = 1024
NEG = -1.0e30
BIG = 30000.0
CSH = 8.0
EPS = 1e-6


class Sem:
    __slots__ = ("h", "issued", "step")

    def __init__(self, h, step):
        self.h = h
        self.issued = 0
        self.step = step


class Obj:
    __slots__ = ("name", "w", "r", "dsem", "t")

    def __init__(self, name, t=None):
        self.name = name
        self.w = []
        self.r = []
        self.dsem = None
        self.t = t

    def __getitem__(self, k):
        return self.t[k]


class Eng:
    __slots__ = ("name", "h", "sem", "seen")

    def __init__(self, name, h, sem):
        self.name = name
        self.h = h
        self.sem = sem
        self.seen = {}


class Ctx:
    def __init__(self, nc):
        self.nc = nc
        self._stack = []
        self.sems = []
        self.E = {}
        for name, h in (("pe", nc.tensor), ("dve", nc.vector), ("act", nc.scalar),
                        ("pool", nc.gpsimd), ("sp", nc.sync)):
            self.E[name] = Eng(name, h, self.new_sem(1, "e_" + name))
        self.n_wait = 0
        self.n_ins = 0

    def new_sem(self, step, name=None):
        cm = self.nc.semaphore(name)
        h = cm.__enter__()
        s = Sem(h, step)
        self.sems.append(s)
        return s

    def mark(self):
        return len(self._stack)

    def release(self, mark):
        while len(self._stack) > mark:
            self._stack.pop().__exit__(None, None, None)

    def sbuf(self, name, shape, dt, dsem=None):
        self.n_ins += 0
        self._uid = getattr(self, "_uid", 0) + 1
        name = f"{name}_u{self._uid}"
        cm = self.nc.sbuf_tensor(name, shape, dt)
        t = cm.__enter__()
        self._stack.append(cm)
        o = Obj(name, t)
        o.dsem = dsem
        return o

    def psum(self, name, shape, dt):
        self._uid = getattr(self, "_uid", 0) + 1
        name = f"{name}_u{self._uid}"
        cm = self.nc.psum_tensor(name, shape, dt)
        t = cm.__enter__()
        self._stack.append(cm)
        return Obj(name, t)

    def _collect(self, eng, reads, writes):
        need = {}
        for o in reads:
            for (s, v) in o.w:
                if need.get(s, 0) < v:
                    need[s] = v
        for o in writes:
            for (s, v) in o.w:
                if need.get(s, 0) < v:
                    need[s] = v
            for (s, v) in o.r:
                if need.get(s, 0) < v:
                    need[s] = v
        for s, v in need.items():
            if s is eng.sem and eng.name == "pe":
                continue
            if s.step == 16:
                v = s.issued
            assert v <= s.issued, f"wait on unsignaled ticket eng={eng.name}"
            if eng.seen.get(s, 0) < v:
                eng.h.wait_ge(s.h, v)
                eng.seen[s] = v
                self.n_wait += 1

    def _record(self, tk, reads, writes):
        for o in writes:
            o.w = [tk]
            o.r = []
        for o in reads:
            if o not in writes:
                o.r = [t for t in o.r if t[0] is not tk[0]] + [tk]

    def op(self, en, fn, reads=(), writes=(), signal=True):
        eng = self.E[en]
        self._collect(eng, reads, writes)
        ins = fn(eng.h)
        self.n_ins += 1
        if signal:
            ins.then_inc(eng.sem.h, 1)
            eng.sem.issued += 1
            tk = (eng.sem, eng.sem.issued)
        else:
            tk = (eng.sem, eng.sem.issued + 1)
        self._record(tk, reads, writes)
        return ins

    def dma(self, qn, out, in_, reads=(), writes=(), sem=None, **kw):
        eng = self.E[qn]
        self._collect(eng, reads, writes)
        ins = eng.h.dma_start(out=out, in_=in_, **kw)
        ins.then_inc(sem.h, 16)
        sem.issued += 16
        self.n_ins += 1
        self._record((sem, sem.issued), reads, writes)
        return ins

    def barrier(self):
        for eng in self.E.values():
            for s in self.sems:
                if s is eng.sem:
                    continue
                if s.issued > 0 and eng.seen.get(s, 0) < s.issued:
                    eng.h.wait_ge(s.h, s.issued)
                    eng.seen[s] = s.issued

    def close(self):
        self.release(0)


def attn_consts(c, nc, NB, s_const, dscr, ident):
    m0 = c.mark()
    CONSTD = Obj("constd")
    CD = {}
    utri = c.sbuf("utri", [128, 128], F32)
    c.op("pool", lambda e: e.memset(utri[:], 1.0), writes=[utri])
    c.op("pool", lambda e: e.affine_select(out=utri[:], in_=utri[:], pattern=[[1, 128]], compare_op=ALU.is_ge, fill=0.0, base=0,
                                           channel_multiplier=-1), reads=[utri], writes=[utri])
    cmask = c.sbuf("cmask", [128, 4, 512], F32)
    c.op("pool", lambda e: e.memset(cmask[:], 0.0), writes=[cmask])
    for j in range(4):
        c.op("pool", lambda e: e.affine_select(out=cmask[:, j, :], in_=cmask[:, j, :], pattern=[[1, 512]], compare_op=ALU.is_ge, fill=NEG,
                                               base=-128 * j, channel_multiplier=-1), reads=[cmask], writes=[cmask])
    dist0 = c.sbuf("dist0", [128, 512], F32)
    c.op("pool", lambda e: e.iota(dist0[:], pattern=[[1, 512]], base=0, channel_multiplier=-1, allow_small_or_imprecise_dtypes=True), writes=[dist0])
    swM = c.sbuf("swM", [128, 5, 512], F32); swD = c.sbuf("swD", [128, 5, 512], F32)
    c.op("pool", lambda e: e.memset(swM[:], 0.0), writes=[swM])
    for jj in range(5):
        j = jj - 1
        c.op("pool", lambda e: e.affine_select(out=swM[:, jj, :], in_=swM[:, jj, :], pattern=[[1, 512]], compare_op=ALU.is_ge, fill=NEG,
                                               base=-128 * j, channel_multiplier=-1), reads=[swM], writes=[swM])
        c.op("pool", lambda e: e.affine_select(out=swM[:, jj, :], in_=swM[:, jj, :], pattern=[[-1, 512]], compare_op=ALU.is_ge, fill=NEG,
                                               base=127 + 128 * j, channel_multiplier=1), reads=[swM], writes=[swM])
        c.op("pool", lambda e: e.iota(swD[:, jj, :], pattern=[[1, 512]], base=-128 * j, channel_multiplier=-1, allow_small_or_imprecise_dtypes=True), writes=[swD])

    ownN = c.sbuf("ownN", [128, NB, NB], F32); vmask = c.sbuf("vmask", [128, NB, NB], F32); eqB = c.sbuf("eqB", [128, NB, NB], F32)
    eself = c.sbuf("eself", [NB, NB, 128], F32)
    c.op("pool", lambda e: e.memset(ownN[:], 0.0), writes=[ownN])
    c.op("pool", lambda e: e.memset(vmask[:], 1.0), writes=[vmask])
    c.op("pool", lambda e: e.memset(eqB[:], 0.0), writes=[eqB])
    c.op("pool", lambda e: e.memset(eself[:], 1.0), writes=[eself])
    for o_ in range(NB):
        pat = [[-1, NB]]
        c.op("pool", lambda e: e.affine_select(out=ownN[:, o_, :], in_=ownN[:, o_, :], pattern=pat, compare_op=ALU.is_ge, fill=NEG, base=o_ - 1, channel_multiplier=0), reads=[ownN], writes=[ownN])
        c.op("pool", lambda e: e.affine_select(out=eqB[:, o_, :], in_=eqB[:, o_, :], pattern=pat, compare_op=ALU.is_equal, fill=-BIG, base=o_, channel_multiplier=0), reads=[eqB], writes=[eqB])
    c.op("dve", lambda e: e.tensor_scalar(out=vmask[:], in0=ownN[:], scalar1=0.0, scalar2=None, op0=ALU.is_equal), reads=[ownN], writes=[vmask])
    for n_ in range(NB):
        c.op("dve", lambda e: e.tensor_scalar(out=eself[:, n_, :], in0=eself[:, n_, :], scalar1=ident[0:NB, n_:n_ + 1], scalar2=None, op0=ALU.mult),
             reads=[eself, ident], writes=[eself])

    for t_, nm_, shp in ((utri, "utri", [128, 128]), (cmask, "cmask", [128, 2048]), (dist0, "dist0", [128, 512]), (swM, "swM", [128, 2560]),
                         (swD, "swD", [128, 2560]), (ownN, "ownN", [128, NB * NB]), (vmask, "vmask", [128, NB * NB]), (eqB, "eqB", [128, NB * NB]),
                         (eself, "eself", [NB, NB * 128])):
        CD[nm_] = dscr("cd_" + nm_, shp, F32)
        src = t_[:] if len(t_[:].shape) == 2 else t_[:].rearrange("p a b -> p (a b)")
        c.dma("sp", CD[nm_], src, reads=[t_], writes=[CONSTD], sem=s_const)
    c.barrier()
    c.release(m0)
    return CD, CONSTD

def mixer_layer(P):
    c, nc, l, S = P.c, P.nc, P.l, P.S
    NT = S // 128; NG = S // 512; NB = S // 256
    ident, identb, ones_f, modc, a1c = P.ident, P.identb, P.ones_f, P.modc, P.a1c
    s_const, s_x, s_w, s_st, s_a = P.s_const, P.s_x, P.s_w, P.s_st, P.s_a
    WCONST = P.WCONST
    x_src, X_SRC = P.x_src, P.X_SRC
    slopes = P.slopes
    hT_v = P.hT_d.rearrange("(kc p) t -> p kc t", p=128)
    qT_v = P.qT_d.rearrange("(cc p) t -> p cc t", p=128)
    kT_v = P.kT_d.rearrange("(cc p) t -> p cc t", p=128)

    mM = c.mark()
    lf_all = c.sbuf("lf_all", [128, 4, NT], F32)
    m1 = c.mark()
    gQ = c.sbuf("gQ", [128, 1024], F32); gK = c.sbuf("gK", [128, 640], F32)
    bfB = c.sbuf("bfB", [128, 4], F32)
    qrow = [0] * 4 + [2] * 8 + [4] * 4
    krow = [1] * 4 + [3] * 2 + [5] * 4
    for h in range(16):
        c.dma("sp", gQ[:, h * 64:(h + 1) * 64], P.qk_gain[l, qrow[h]:qrow[h] + 1, :].partition_broadcast(128), reads=[WCONST], writes=[gQ], sem=s_const)
    for h in range(10):
        c.dma("sp", gK[:, h * 64:(h + 1) * 64], P.qk_gain[l, krow[h]:krow[h] + 1, :].partition_broadcast(128), reads=[WCONST], writes=[gK], sem=s_const)
    c.dma("sp", bfB[:], P.b_fgate[l:l + 1, :].partition_broadcast(128), reads=[WCONST], writes=[bfB], sem=s_const)
    c.op("dve", lambda e: e.tensor_scalar(out=gQ[:], in0=gQ[:], scalar1=0.125, scalar2=None, op0=ALU.mult), reads=[gQ], writes=[gQ])
    wqkv = c.sbuf("wqkv", [128, 8, 2308], BF16)
    wst = [c.sbuf(f"wst{i}", [128, 2308], F32) for i in range(2)]
    segs = [(0, 256, 0), (772, 1284, 256), (1540, 1796, 768),
            (256, 512, 1024), (1284, 1412, 1280), (1796, 2052, 1408),
            (512, 768, 1664), (1412, 1540, 1920), (2052, 2308, 2048),
            (768, 772, 2304)]
    for kc in range(8):
        st = wst[kc % 2]
        c.dma("sp", st[:], P.w_in[l, kc * 128:(kc + 1) * 128, 0:2308], reads=[WCONST], writes=[st], sem=s_w[kc % 2])
        for si, (a, b, d0) in enumerate(segs):
            en = "act" if si % 2 == 0 else "dve"
            if en == "act":
                c.op("act", lambda e: e.copy(out=wqkv[:, kc, d0:d0 + (b - a)], in_=st[:, a:b]), reads=[st], writes=[wqkv])
            else:
                c.op("dve", lambda e: e.tensor_copy(out=wqkv[:, kc, d0:d0 + (b - a)], in_=st[:, a:b]), reads=[st], writes=[wqkv])
    xs = [c.sbuf(f"xs{i}", [128, D], F32) for i in range(2)]
    junk = c.sbuf("junk", [128, D], F32)
    ss = [c.sbuf(f"ss{i}", [128, 4], F32) for i in range(2)]
    xn = c.sbuf("xn", [128, 4, D], F32)
    hTg = c.sbuf("hTg", [128, 8, 512], BF16)
    sqs = c.sbuf("sqs", [128, 1664], F32)
    rq = c.sbuf("rq", [128, 64], F32)
    qn = c.sbuf("qn", [128, 1024], BF16); kn = c.sbuf("kn", [128, 640], BF16)
    qTs = c.sbuf("qTs", [128, 8, 512], BF16); kTs = c.sbuf("kTs", [128, 5, 512], BF16)
    vst = c.sbuf("vst", [128, 4, 650], BF16)
    lft = c.sbuf("lft", [128, 8], F32)
    c.op("pool", lambda e: e.memset(vst[:], 1.0), writes=[vst])
    T0 = c.psum("T0", [128, 512], F32); T1 = c.psum("T1", [128, 512], F32)
    pq = c.psum("pq", [128, 1024], F32); pk = c.psum("pk", [128, 1024], F32); pv = c.psum("pv", [128, 1024], F32)
    TT = [T0, T1]
    for q in range(NG):
        for sub in range(4):
            i = q * 4 + sub
            r0 = i * 128
            xb = xs[i % 2]; sb = ss[i % 2]
            c.dma("sp", xb[:], x_src[r0:r0 + 128, :], reads=[X_SRC], writes=[xb], sem=s_x[i % 2])
            c.op("act", lambda e: e.activation(out=junk[:], in_=xb[:], func=AF.Square, accum_out=sb[:, 0:1]), reads=[xb], writes=[junk, sb])
            c.op("act", lambda e: e.activation(out=sb[:, 1:2], in_=sb[:, 0:1], func=AF.Sqrt, scale=1.0 / D, bias=EPS), reads=[sb], writes=[sb])
            c.op("dve", lambda e: e.reciprocal(out=sb[:, 2:3], in_=sb[:, 1:2]), reads=[sb], writes=[sb])
            c.op("dve", lambda e: e.tensor_scalar(out=xn[:, sub, :], in0=xb[:], scalar1=sb[:, 2:3], scalar2=None, op0=ALU.mult), reads=[xb, sb], writes=[xn])
        for kc in range(8):
            pp = TT[kc % 2]
            for sub in range(4):
                c.op("pe", lambda e: e.transpose(pp[:, sub * 128:(sub + 1) * 128], xn[:, sub, kc * 128:(kc + 1) * 128], ident[:]),
                     reads=[xn, ident], writes=[pp], signal=(sub == 3))
            c.op("act", lambda e: e.activation(out=hTg[:, kc, :], in_=pp[:], func=AF.Identity, scale=a1c[:, kc:kc + 1], bias=modc[:, l, kc:kc + 1]),
                 reads=[pp, a1c, modc], writes=[hTg])
        c.dma("sp", hT_v[:, :, q * 512:(q + 1) * 512], hTg[:], reads=[hTg], writes=[P.HT], sem=s_st[0])
        for sub in range(4):
            i = q * 4 + sub
            lhs = lambda kc: hTg[:, kc, sub * 128:(sub + 1) * 128]
            for (pt_, c0, n, w0) in ((pq, 0, 512, 0), (pq, 512, 512, 512), (pk, 0, 512, 1024), (pk, 512, 128, 1536),
                                     (pv, 0, 512, 1664), (pv, 512, 132, 2176)):
                for kc in range(8):
                    c.op("pe", lambda e: e.matmul(pt_[:, c0:c0 + n], lhsT=lhs(kc), rhs=wqkv[:, kc, w0:w0 + n], start=(kc == 0), stop=(kc == 7)),
                         reads=[hTg, wqkv], writes=[pt_], signal=(kc == 7))
            c.op("act", lambda e: e.activation(out=sqs[:, 0:1024], in_=pq[:], func=AF.Square), reads=[pq], writes=[sqs])
            c.op("act", lambda e: e.activation(out=sqs[:, 1024:1664], in_=pk[:, 0:640], func=AF.Square), reads=[pk], writes=[sqs])
            c.op("dve", lambda e: e.tensor_reduce(out=rq[:, 0:26], in_=sqs[:].rearrange("p (h d) -> p h d", d=64), axis=AX.X, op=ALU.add), reads=[sqs], writes=[rq])
            c.op("act", lambda e: e.activation(out=rq[:, 32:58], in_=rq[:, 0:26], func=AF.Sqrt, scale=1.0 / 64, bias=EPS), reads=[rq], writes=[rq])
            c.op("dve", lambda e: e.reciprocal(out=rq[:, 0:26], in_=rq[:, 32:58]), reads=[rq], writes=[rq])
            for h in range(16):
                c.op("dve", lambda e: e.scalar_tensor_tensor(out=qn[:, h * 64:(h + 1) * 64], in0=pq[:, h * 64:(h + 1) * 64], scalar=rq[:, h:h + 1],
                                                             in1=gQ[:, h * 64:(h + 1) * 64], op0=ALU.mult, op1=ALU.mult),
                     reads=[pq, rq, gQ], writes=[qn])
            for h in range(10):
                c.op("dve", lambda e: e.scalar_tensor_tensor(out=kn[:, h * 64:(h + 1) * 64], in0=pk[:, h * 64:(h + 1) * 64], scalar=rq[:, 16 + h:17 + h],
                                                             in1=gK[:, h * 64:(h + 1) * 64], op0=ALU.mult, op1=ALU.mult),
                     reads=[pk, rq, gK], writes=[kn])
            c.op("act", lambda e: e.copy(out=vst[:, sub, :].rearrange("p (h d) -> p h d", d=65)[:, :, 0:64],
                                         in_=pv[:, 0:640].rearrange("p (h d) -> p h d", d=64)), reads=[pv], writes=[vst])
            c.op("dve", lambda e: e.tensor_tensor(out=lft[:, 0:4], in0=pv[:, 640:644], in1=bfB[:], op=ALU.add), reads=[pv, bfB], writes=[lft])
            c.op("act", lambda e: e.activation(out=lft[:, 4:8], in_=lft[:, 0:4], func=AF.Exp, scale=-1.0), reads=[lft], writes=[lft])
            c.op("act", lambda e: e.activation(out=lft[:, 0:4], in_=lft[:, 4:8], func=AF.Ln, bias=1.0), reads=[lft], writes=[lft])
            c.op("dve", lambda e: e.tensor_scalar(out=lf_all[:, :, i], in0=lft[:, 0:4], scalar1=-1.0, scalar2=None, op0=ALU.mult), reads=[lft], writes=[lf_all])
            tq = T0[:].bitcast(BF16); tk = T1[:].bitcast(BF16)
            for cc in range(8):
                c.op("pe", lambda e: e.transpose(tq[:, cc * 128:(cc + 1) * 128], qn[:, cc * 128:(cc + 1) * 128], identb[:]),
                     reads=[qn, identb], writes=[T0], signal=(cc == 7))
            for cc in range(5):
                c.op("pe", lambda e: e.transpose(tk[:, cc * 128:(cc + 1) * 128], kn[:, cc * 128:(cc + 1) * 128], identb[:]),
                     reads=[kn, identb], writes=[T1], signal=(cc == 4))
            c.op("act", lambda e: e.copy(out=qTs[:, :, sub * 128:(sub + 1) * 128], in_=tq.rearrange("p (c t) -> p c t", t=128)), reads=[T0], writes=[qTs])
            c.op("dve", lambda e: e.tensor_copy(out=kTs[:, :, sub * 128:(sub + 1) * 128], in_=tk[:, 0:640].rearrange("p (c t) -> p c t", t=128)), reads=[T1], writes=[kTs])
        c.dma("sp", qT_v[:, :, q * 512:(q + 1) * 512], qTs[:], reads=[qTs], writes=[P.QT], sem=s_st[1])
        c.dma("sp", kT_v[:, :, q * 512:(q + 1) * 512], kTs[:], reads=[kTs], writes=[P.KT], sem=s_st[2])
        c.dma("sp", P.v_d[q * 512:(q + 1) * 512, :].rearrange("(s p) n -> p s n", p=128), vst[:], reads=[vst], writes=[P.VD], sem=s_st[3])
    c.barrier()
    c.release(m1)

    o_sb = c.sbuf("o_sb", [128, NT, D], BF16)
    m2 = c.mark()
    utri = c.sbuf("utri", [128, 128], F32); cmask = c.sbuf("cmask", [128, 4, 512], F32); dist0 = c.sbuf("dist0", [128, 512], F32)
    swM = c.sbuf("swM", [128, 5, 512], F32); swD = c.sbuf("swD", [128, 5, 512], F32)
    for t_, nm_ in ((utri, "utri"), (cmask, "cmask"), (dist0, "dist0"), (swM, "swM"), (swD, "swD")):
        c.dma("sp", t_[:], P.CD[nm_].rearrange("p (a b) -> p a b", b=512) if nm_ in ("cmask", "swM", "swD") else P.CD[nm_], reads=[P.CONSTD], writes=[t_], sem=s_const)
    sinkE = c.sbuf("sinkE", [128, 8], F32)
    c.dma("sp", sinkE[:], P.attn_sinks[l:l + 1, :].partition_broadcast(128), reads=[WCONST], writes=[sinkE], sem=s_const)
    c.op("act", lambda e: e.activation(out=sinkE[:], in_=sinkE[:], func=AF.Exp, bias=-CSH), reads=[sinkE], writes=[sinkE])

    cumK = c.sbuf("cumK", [128, 4, NT], F32)
    negck = c.sbuf("negck", [128, 4, NT], F32)
    tot = c.sbuf("tot", [128, 4, NT], F32)
    off = c.sbuf("off", [128, 4, NT], F32)
    cTs = c.sbuf("cTs", [128, 128], F32)
    S0 = c.psum("S0", [128, 512], F32); S1 = c.psum("S1", [128, 512], F32)
    OA = [c.psum(f"OA{j}", [128, 512], F32) for j in range(4)]
    G0 = c.psum("G0", [128, 512], F32); G1 = c.psum("G1", [128, 512], F32)
    lf2 = lf_all[:].rearrange("p h i -> p (h i)")
    c.op("pe", lambda e: e.matmul(S0[:, 0:4 * NT], lhsT=ones_f[:], rhs=lf2, start=True, stop=True), reads=[ones_f, lf_all], writes=[S0])
    c.op("pe", lambda e: e.matmul(S1[:, 0:4 * NT], lhsT=utri[:], rhs=lf2, start=True, stop=True), reads=[utri, lf_all], writes=[S1])
    c.op("dve", lambda e: e.tensor_copy(out=tot[:].rearrange("p h i -> p (h i)"), in_=S0[:, 0:4 * NT]), reads=[S0], writes=[tot])
    c.op("dve", lambda e: e.memset(off[:], 0.0), writes=[off])
    for i in range(1, NT):
        c.op("dve", lambda e: e.tensor_tensor(out=off[:, :, i], in0=off[:, :, i - 1], in1=tot[:, :, i - 1], op=ALU.add), reads=[off, tot], writes=[off])
    c.op("dve", lambda e: e.tensor_tensor(out=cumK[:].rearrange("p h i -> p (h i)"), in0=S1[:, 0:4 * NT], in1=off[:].rearrange("p h i -> p (h i)"), op=ALU.add),
         reads=[S1, off], writes=[cumK])
    c.op("dve", lambda e: e.tensor_scalar(out=negck[:], in0=cumK[:], scalar1=-1.0, scalar2=-CSH, op0=ALU.mult, op1=ALU.add), reads=[cumK], writes=[negck])
    c.op("pe", lambda e: e.transpose(S0[0:4 * NT, 0:128], cumK[:].rearrange("p h i -> p (h i)"), ident[:]), reads=[cumK, ident], writes=[S0])
    c.op("dve", lambda e: e.tensor_copy(out=cTs[0:4 * NT, :], in_=S0[0:4 * NT, 0:128]), reads=[S0], writes=[cTs])
    c.dma("sp", P.cumT_d.rearrange("h (i t) -> (h i) t", t=128), cTs[0:4 * NT, :], reads=[cTs], writes=[P.CUMT], sem=s_st[0])

    qTc = [c.sbuf(f"qTc{i}", [128, S], BF16) for i in range(2)]
    kTc = [c.sbuf(f"kTc{i}", [128, S], BF16) for i in range(2)]
    vaug = [c.sbuf(f"vaug{i}", [128, NT, 65], BF16) for i in range(2)]
    biasb = [c.sbuf(f"biasb{i}", [128, max(S, 2560)], F32) for i in range(2)]
    tts = [c.sbuf(f"tts{i}", [128, 512], F32) for i in range(2)]
    pex = [c.sbuf(f"pex{i}", [128, 512], BF16) for i in range(2)]
    rz = c.sbuf("rz", [128, 8], F32)
    SS = [S0, S1]
    cnt = [0]

    def attn_tile(qap, kap, extra, bias_ap, mask_ap, act_bias, vap, first, last, rd):
        k_ = cnt[0] % 2
        cnt[0] += 1
        ps = SS[k_]; tt = tts[k_]; pe_ = pex[k_]
        c.op("pe", lambda e: e.matmul(ps[:], lhsT=kap, rhs=qap, start=True, stop=(extra is None)), reads=rd, writes=[ps], signal=(extra is None))
        if extra is not None:
            c.op("pe", lambda e: e.matmul(ps[:], lhsT=extra[0], rhs=extra[1], start=False, stop=True), reads=rd, writes=[ps])
        c.op("dve", lambda e: e.tensor_tensor(out=tt[:], in0=ps[:], in1=bias_ap, op=ALU.add), reads=[ps] + rd, writes=[tt])
        if mask_ap is not None:
            c.op("dve", lambda e: e.tensor_tensor(out=tt[:], in0=tt[:], in1=mask_ap, op=ALU.add), reads=[tt] + rd, writes=[tt])
        c.op("act", lambda e: e.activation(out=pe_[:], in_=tt[:], func=AF.Exp, bias=act_bias), reads=[tt] + rd, writes=[pe_])
        for j in range(4):
            c.op("pe", lambda e: e.matmul(OA[j][:, 0:65], lhsT=pe_[:, j * 128:(j + 1) * 128], rhs=vap, start=first, stop=last),
                 reads=[pe_] + rd, writes=[OA[j]], signal=(j == 3 or last))

    def finish_tile(qt, col, sink_ap):
        for j in range(4):
            if sink_ap is not None:
                c.op("dve", lambda e: e.tensor_tensor(out=rz[:, j:j + 1], in0=OA[j][:, 64:65], in1=sink_ap, op=ALU.add), reads=[OA[j], sinkE], writes=[rz])
                c.op("dve", lambda e: e.reciprocal(out=rz[:, 4 + j:5 + j], in_=rz[:, j:j + 1]), reads=[rz], writes=[rz])
            else:
                c.op("dve", lambda e: e.reciprocal(out=rz[:, 4 + j:5 + j], in_=OA[j][:, 64:65]), reads=[OA[j]], writes=[rz])
            c.op("dve", lambda e: e.tensor_scalar(out=o_sb[:, qt * 4 + j, col:col + 64], in0=OA[j][:, 0:64], scalar1=rz[:, 4 + j:5 + j], scalar2=None, op0=ALU.mult),
                 reads=[OA[j], rz], writes=[o_sb])

    def load_v(buf, vh, semi):
        with nc.allow_non_contiguous_dma("v head slice"):
            c.dma("sp", buf[:], P.v_d[:, vh * 65:(vh + 1) * 65].rearrange("(i p) n -> p i n", p=128), reads=[P.VD], writes=[buf], sem=s_a[semi])

    hcount = [0]
    for h in range(4):
        b = hcount[0] % 2; hcount[0] += 1
        hp = (h % 2) * 64
        qc, kc_, vb, bb = qTc[b], kTc[b], vaug[b], biasb[b]
        c.dma("sp", qc[hp:hp + 64, :], P.qT_d[h * 64:(h + 1) * 64, :], reads=[P.QT], writes=[qc], sem=s_a[b])
        c.dma("sp", kc_[hp:hp + 64, :], P.kT_d[h * 64:(h + 1) * 64, :], reads=[P.KT], writes=[kc_], sem=s_a[2 + b])
        load_v(vb, h, 4 + b)
        c.dma("sp", bb[:, 0:S], P.cumT_d[h:h + 1, :].partition_broadcast(128), reads=[P.CUMT], writes=[bb], sem=s_x[b])
        rd = [qc, kc_, vb, bb, negck, cmask]
        for qt in range(NG):
            nk = 4 * qt + 4
            for kt in range(nk):
                j = kt - 4 * qt
                attn_tile(qc[hp:hp + 64, qt * 512:(qt + 1) * 512], kc_[hp:hp + 64, kt * 128:(kt + 1) * 128], None,
                          bb[:, qt * 512:(qt + 1) * 512], cmask[:, j, :] if j >= 0 else None, negck[:, h, kt:kt + 1],
                          vb[:, kt, :], kt == 0, kt == nk - 1, rd)
            finish_tile(qt, h * 64, None)
    for h in range(8):
        b = hcount[0] % 2; hcount[0] += 1
        hp = (h % 2) * 64
        kvh = h // 4
        qc, kc_, vb, bb = qTc[b], kTc[b], vaug[b], biasb[b]
        c.dma("sp", qc[hp:hp + 64, :], P.qT_d[256 + h * 64:256 + (h + 1) * 64, :], reads=[P.QT], writes=[qc], sem=s_a[b])
        c.dma("sp", kc_[hp:hp + 64, :], P.kT_d[256 + kvh * 64:256 + (kvh + 1) * 64, :], reads=[P.KT], writes=[kc_], sem=s_a[2 + b])
        load_v(vb, 4 + kvh, 4 + b)
        for jj in range(5):
            c.op("dve", lambda e: e.scalar_tensor_tensor(out=bb[:, jj * 512:(jj + 1) * 512], in0=swD[:, jj, :], scalar=-slopes[h], in1=swM[:, jj, :],
                                                          op0=ALU.mult, op1=ALU.add), reads=[swD, swM], writes=[bb])
        rd = [qc, kc_, vb, bb]
        for qt in range(NG):
            kts = [kt for kt in range(4 * qt - 1, 4 * qt + 4) if kt >= 0]
            for kt in kts:
                jj = kt - 4 * qt + 1
                attn_tile(qc[hp:hp + 64, qt * 512:(qt + 1) * 512], kc_[hp:hp + 64, kt * 128:(kt + 1) * 128], None,
                          bb[:, jj * 512:(jj + 1) * 512], None, -CSH, vb[:, kt, :], kt == kts[0], kt == kts[-1], rd)
            finish_tile(qt, 256 + h * 64, sinkE[:, h:h + 1])
    m3 = c.mark()
    ownN = c.sbuf("ownN", [128, NB, NB], F32); vmask = c.sbuf("vmask", [128, NB, NB], F32); eqB = c.sbuf("eqB", [128, NB, NB], F32)
    eself = c.sbuf("eself", [NB, NB, 128], F32); esel = c.sbuf("esel", [NB, NB, 128], BF16)
    for t_, nm_ in ((ownN, "ownN"), (vmask, "vmask"), (eqB, "eqB")):
        c.dma("sp", t_[:], P.CD[nm_].rearrange("p (a b) -> p a b", b=NB), reads=[P.CONSTD], writes=[t_], sem=s_const)
    c.dma("sp", eself[:], P.CD["eself"].rearrange("p (a b) -> p a b", b=128), reads=[P.CONSTD], writes=[eself], sem=s_const)
    c.op("dve", lambda e: e.tensor_copy(out=esel[:], in_=eself[:]), reads=[eself], writes=[esel])
    kms = c.sbuf("kms", [128, NB], F32); kmb = c.sbuf("kmb", [128, NB], BF16)
    selT = c.sbuf("selT", [NB, S], BF16)
    gw = [c.sbuf(f"gw{i}", [128, 4 * NB + 8], F32) for i in range(2)]
    GG = [G0, G1]
    for h in range(4):
        b = hcount[0] % 2; hcount[0] += 1
        hp = (h % 2) * 64
        qc, kc_, vb, bb = qTc[b], kTc[b], vaug[b], biasb[b]
        sl = slopes[8 + h]
        c.dma("sp", qc[hp:hp + 64, :], P.qT_d[768 + h * 64:768 + (h + 1) * 64, :], reads=[P.QT], writes=[qc], sem=s_a[b])
        c.dma("sp", kc_[hp:hp + 64, :], P.kT_d[384 + h * 64:384 + (h + 1) * 64, :], reads=[P.KT], writes=[kc_], sem=s_a[2 + b])
        load_v(vb, 6 + h, 4 + b)
        c.op("pool", lambda e: e.tensor_scalar(out=bb[:, 0:512], in0=dist0[:], scalar1=-sl, scalar2=None, op0=ALU.mult), reads=[dist0], writes=[bb])
        c.op("dve", lambda e: e.tensor_reduce(out=kms[hp:hp + 64, :], in_=kc_[hp:hp + 64, :].rearrange("p (n t) -> p n t", t=256), axis=AX.X, op=ALU.add), reads=[kc_], writes=[kms])
        c.op("dve", lambda e: e.tensor_scalar(out=kmb[hp:hp + 64, :], in0=kms[hp:hp + 64, :], scalar1=1.0 / 256, scalar2=None, op0=ALU.mult), reads=[kms], writes=[kmb])
        for i in range(NT):
            own = i // 2
            gp = GG[i % 2]; w = gw[i % 2]
            gm = w[:, 0:NB]; sel = w[:, NB:2 * NB]; mv = w[:, 2 * NB:3 * NB]; m8 = w[:, 4 * NB:4 * NB + 8]
            c.op("pe", lambda e: e.matmul(gp[:, 0:NB], lhsT=qc[hp:hp + 64, i * 128:(i + 1) * 128], rhs=kmb[hp:hp + 64, :], start=True, stop=True), reads=[qc, kmb], writes=[gp])
            c.op("dve", lambda e: e.tensor_tensor(out=gm, in0=gp[:, 0:NB], in1=ownN[:, own, :], op=ALU.add), reads=[gp, ownN], writes=[w])
            c.op("dve", lambda e: e.max(out=m8, in_=gm), reads=[w], writes=[w])
            c.op("dve", lambda e: e.tensor_scalar(out=sel, in0=gm, scalar1=m8[:, 2:3], scalar2=None, op0=ALU.is_ge), reads=[w], writes=[w])
            c.op("dve", lambda e: e.tensor_tensor(out=sel, in0=sel, in1=vmask[:, own, :], op=ALU.mult), reads=[w, vmask], writes=[w])
            c.op("dve", lambda e: e.scalar_tensor_tensor(out=mv, in0=sel, scalar=BIG, in1=eqB[:, own, :], op0=ALU.mult, op1=ALU.add), reads=[w, eqB], writes=[w])
            c.op("pe", lambda e: e.transpose(gp[0:NB, 128:256], mv, ident[:]), reads=[w, ident], writes=[gp])
            c.op("act", lambda e: e.copy(out=selT[:, i * 128:(i + 1) * 128], in_=gp[0:NB, 128:256]), reads=[gp], writes=[selT])
        rd = [qc, kc_, vb, bb, selT, esel, cmask]
        for qt in range(NG):
            nk = 4 * qt + 4
            for kt in range(nk):
                j = kt - 4 * qt
                attn_tile(qc[hp:hp + 64, qt * 512:(qt + 1) * 512], kc_[hp:hp + 64, kt * 128:(kt + 1) * 128],
                          (esel[:, kt // 2, :], selT[:, qt * 512:(qt + 1) * 512]),
                          bb[:, 0:512], cmask[:, j, :] if j >= 0 else None, float(-sl * (qt * 512 - kt * 128) - CSH),
                          vb[:, kt, :], kt == 0, kt == nk - 1, rd)
            finish_tile(qt, 768 + h * 64, None)
    c.barrier()
    c.release(m2)

    m4 = c.mark()
    g1B = c.sbuf("g1B", [128, D], F32)
    c.dma("sp", g1B[:], P.mod_d[l:l + 1, 2 * D:3 * D].partition_broadcast(128), reads=[P.MODD], writes=[g1B], sem=s_const)
    wg = c.sbuf("wg", [128, 8, 3072], BF16)
    wbr = c.sbuf("wbr", [128, 8, D], BF16)
    wo = c.sbuf("wo", [128, 8, D], BF16)
    st3 = [c.sbuf(f"st3_{i}", [128, D], F32) for i in range(2)]
    pc = [0]

    def load_cast(dst_ap, src_ap):
        k_ = pc[0] % 2; pc[0] += 1
        st = st3[k_]
        c.dma("sp", st[:], src_ap, reads=[WCONST], writes=[st], sem=s_w[k_])
        return st

    for kc in range(8):
        for part in range(3):
            st = load_cast(None, P.w_in[l, kc * 128:(kc + 1) * 128, 2308 + part * 1024:2308 + (part + 1) * 1024])
            c.op("act", lambda e: e.copy(out=wg[:, kc, part * 1024:(part + 1) * 1024], in_=st[:]), reads=[st], writes=[wg])
        srcbr = (P.w_br_fox[l, kc * 128:(kc + 1) * 128, :] if kc < 2 else
                 P.w_br_swa[l, (kc - 2) * 128:(kc - 1) * 128, :] if kc < 6 else P.w_br_moba[l, (kc - 6) * 128:(kc - 5) * 128, :])
        st = load_cast(None, srcbr)
        c.op("dve", lambda e: e.tensor_copy(out=wbr[:, kc, :], in_=st[:]), reads=[st], writes=[wbr])
        st = load_cast(None, P.w_out[l, kc * 128:(kc + 1) * 128, :])
        c.op("dve", lambda e: e.tensor_copy(out=wo[:, kc, :], in_=st[:]), reads=[st], writes=[wo])
    hTg = c.sbuf("hTg3", [128, 8, 512], BF16)
    oT = c.sbuf("oT", [128, 8, 512], BF16)
    mT = c.sbuf("mT", [128, 8, 512], BF16)
    sg = [c.sbuf(f"sg{i}", [128, 512], F32) for i in range(2)]
    macc = [c.sbuf(f"macc{i}", [128, 512], F32) for i in range(2)]
    xt = [c.sbuf(f"xt{i}", [128, D], F32) for i in range(2)]
    xo = [c.sbuf(f"xo{i}", [128, D], F32) for i in range(2)]
    PT = c.psum("PT", [128, 512], F32)
    PG = [c.psum(f"PG{i}", [128, 512], F32) for i in range(2)]
    PY = [c.psum(f"PY{i}", [128, 512], F32) for i in range(2)]
    PO = c.psum("PO", [128, D], F32)
    br_k = [(0, 2), (2, 6), (6, 8)]
    for q in range(NG):
        c.dma("sp", hTg[:], hT_v[:, :, q * 512:(q + 1) * 512], reads=[P.HT], writes=[hTg], sem=s_a[0])
        ptb = PT[:].bitcast(BF16)
        for cc in range(8):
            for sub in range(4):
                c.op("pe", lambda e: e.transpose(ptb[:, sub * 128:(sub + 1) * 128], o_sb[:, q * 4 + sub, cc * 128:(cc + 1) * 128], identb[:]),
                     reads=[o_sb, identb], writes=[PT], signal=(sub == 3))
            c.op("act", lambda e: e.copy(out=oT[:, cc, :], in_=ptb[:, 0:512]), reads=[PT], writes=[oT])
        for jc in range(8):
            ma = macc[jc % 2]
            for br in range(3):
                pg_ = PG[(jc * 3 + br) % 2]; py_ = PY[(jc * 3 + br) % 2]; sgt = sg[(jc * 3 + br) % 2]
                gcol = br * 1024 + jc * 128
                for kc in range(8):
                    c.op("pe", lambda e: e.matmul(pg_[:], lhsT=wg[:, kc, gcol:gcol + 128], rhs=hTg[:, kc, :], start=(kc == 0), stop=(kc == 7)),
                         reads=[wg, hTg], writes=[pg_], signal=(kc == 7))
                k0, k1 = br_k[br]
                for kc in range(k0, k1):
                    c.op("pe", lambda e: e.matmul(py_[:], lhsT=wbr[:, kc, jc * 128:(jc + 1) * 128], rhs=oT[:, kc, :], start=(kc == k0), stop=(kc == k1 - 1)),
                         reads=[wbr, oT], writes=[py_], signal=(kc == k1 - 1))
                c.op("act", lambda e: e.activation(out=sgt[:], in_=pg_[:], func=AF.Sigmoid), reads=[pg_], writes=[sgt])
                if br == 0:
                    c.op("dve", lambda e: e.tensor_tensor(out=ma[:], in0=py_[:], in1=sgt[:], op=ALU.mult), reads=[py_, sgt], writes=[ma])
                else:
                    c.op("dve", lambda e: e.tensor_tensor(out=sgt[:], in0=py_[:], in1=sgt[:], op=ALU.mult), reads=[py_, sgt], writes=[sgt])
                    if br == 1:
                        c.op("pool", lambda e: e.tensor_tensor(out=ma[:], in0=ma[:], in1=sgt[:], op=ALU.add), reads=[ma, sgt], writes=[ma])
                    else:
                        c.op("pool", lambda e: e.tensor_tensor(out=mT[:, jc, :], in0=ma[:], in1=sgt[:], op=ALU.add), reads=[ma, sgt], writes=[mT])
        for sub in range(4):
            i = q * 4 + sub
            r0 = i * 128
            xb = xt[i % 2]; ob = xo[i % 2]
            c.dma("sp", xb[:], x_src[r0:r0 + 128, :], reads=[X_SRC], writes=[xb], sem=s_x[i % 2])
            for half in range(2):
                for kc in range(8):
                    c.op("pe", lambda e: e.matmul(PO[:, half * 512:(half + 1) * 512], lhsT=mT[:, kc, sub * 128:(sub + 1) * 128], rhs=wo[:, kc, half * 512:(half + 1) * 512],
                                                  start=(kc == 0), stop=(kc == 7)), reads=[mT, wo], writes=[PO], signal=(kc == 7))
            c.op("dve", lambda e: e.tensor_tensor(out=ob[:], in0=PO[:], in1=g1B[:], op=ALU.mult), reads=[PO, g1B], writes=[ob])
            c.op("pool", lambda e: e.tensor_tensor(out=ob[:], in0=ob[:], in1=xb[:], op=ALU.add), reads=[ob, xb], writes=[ob])
            c.dma("sp", P.xres_d[r0:r0 + 128, :], ob[:], reads=[ob], writes=[P.XRES], sem=s_st[i % 2])
    c.barrier()
    c.release(mM)


def moe_layer(P):
    c, nc, l, S, NE = P.c, P.nc, P.l, P.S, P.NE
    GT = min(2048, S)
    NGRP = S // GT
    NTG = GT // 128
    NQ = GT // 512
    ident, modc, a2c = P.ident, P.modc, P.a2c
    s_const, s_x, s_w, s_st = P.s_const, P.s_x, P.s_w, P.s_st
    WCONST = P.WCONST
    x_src, X_SRC = P.x_src, P.X_SRC
    if not (P.skip_mixer and l == 0):
        x_src, X_SRC = P.xres_d, P.XRES
    x_dst, X_DST = (P.y_out, P.YOUT) if P.last else (P.xres_d, P.XRES)

    mL = c.mark()
    g2B = c.sbuf("g2B", [128, D], F32)
    c.dma("sp", g2B[:], P.mod_d[l:l + 1, 5 * D:6 * D].partition_broadcast(128), reads=[P.MODD], writes=[g2B], sem=s_const)
    yacc = c.sbuf("yacc", [128, NTG, D], F32)
    h2T = c.sbuf("h2T", [128, 8, GT], BF16)
    gates = c.sbuf("gates", [128, NTG, NE], F32)
    b1T = c.sbuf("b1T", [128, NE, 16], F32)
    b2s = c.sbuf("b2s", [NE, D], F32)
    brB = c.sbuf("brB", [128, NE], F32)
    wr = c.sbuf("wr", [128, 8, NE], F32)

    m1 = c.mark()
    b1tm = c.sbuf("b1tm", [NE, 2 * D], F32)
    pt = c.psum("pt_b1", [128, 512], F32)
    c.dma("sp", b1tm[:], P.b_exp1[l], reads=[WCONST], writes=[b1tm], sem=s_const)
    c.dma("sp", b2s[:], P.b_exp2[l], reads=[WCONST], writes=[b2s], sem=s_const)
    c.dma("sp", brB[:], P.b_router[l:l + 1, :].partition_broadcast(128), reads=[WCONST], writes=[brB], sem=s_const)
    with nc.allow_non_contiguous_dma("router weights relayout"):
        c.dma("sp", wr[:], P.w_router[l].rearrange("(kc p) e -> p kc e", p=128), reads=[WCONST], writes=[wr], sem=s_const)
    for t in range(2):
        for fc in range(8):
            src = b1tm[:, fc * 256: (fc + 1) * 256].rearrange("e (p t) -> e t p", t=2)[:, t, :]
            c.op("pe", lambda e: e.transpose(pt[:, 0:NE], src, ident[0:NE, 0:NE]), reads=[b1tm, ident], writes=[pt])
            c.op("dve", lambda e: e.tensor_copy(out=b1T[:, :, t * 8 + fc], in_=pt[:, 0:NE]), reads=[pt], writes=[b1T])
    c.barrier()
    c.release(m1)

    for g in range(NGRP):
        tok0 = g * GT
        m2 = c.mark()
        xs = [c.sbuf(f"xs{i}", [128, D], F32) for i in range(2)]
        junk = c.sbuf("junk", [128, D], F32)
        ss = [c.sbuf(f"ss{i}", [128, 4], F32) for i in range(2)]
        xn = c.sbuf("xn", [128, 4, D], F32)
        h32 = c.sbuf("h32", [128, 8, 512], F32)
        sm = [c.sbuf(f"sm{i}", [128, 4 * NE + 16], F32) for i in range(2)]
        gTs = c.sbuf("gTs", [NE, 128], F32)
        ptr = [c.psum(f"ptr{i}", [128, 512], F32) for i in range(2)]
        pr = [c.psum(f"pr{i}", [128, 512], F32) for i in range(2)]
        pb = c.psum("pb", [128, D], F32)
        for q in range(NQ):
            for sub in range(4):
                i = q * 4 + sub
                r0 = tok0 + i * 128
                xb = xs[i % 2]; sb = ss[i % 2]
                c.dma("sp", xb[:], x_src[r0:r0 + 128, :], reads=[X_SRC], writes=[xb], sem=s_x[i % 2])
                c.op("act", lambda e: e.activation(out=junk[:], in_=xb[:], func=AF.Square, accum_out=sb[:, 0:1]),
                     reads=[xb], writes=[junk, sb])
                c.op("act", lambda e: e.activation(out=sb[:, 1:2], in_=sb[:, 0:1], func=AF.Sqrt, scale=1.0 / D, bias=EPS),
                     reads=[sb], writes=[sb])
                c.op("dve", lambda e: e.reciprocal(out=sb[:, 2:3], in_=sb[:, 1:2]), reads=[sb], writes=[sb])
                c.op("dve", lambda e: e.tensor_scalar(out=xn[:, sub, :], in0=xb[:], scalar1=sb[:, 2:3], scalar2=None, op0=ALU.mult),
                     reads=[xb, sb], writes=[xn])
            for kc in range(8):
                pp = ptr[kc % 2]
                for sub in range(4):
                    c.op("pe", lambda e: e.transpose(pp[:, sub * 128:(sub + 1) * 128], xn[:, sub, kc * 128:(kc + 1) * 128], ident[:]),
                         reads=[xn, ident], writes=[pp], signal=(sub == 3))
                c.op("act", lambda e: e.activation(out=h32[:, kc, :], in_=pp[:], func=AF.Identity,
                                                   scale=a2c[:, kc:kc + 1], bias=modc[:, l, 24 + kc:25 + kc]),
                     reads=[pp, a2c, modc], writes=[h32])
                c.op("dve", lambda e: e.tensor_copy(out=h2T[:, kc, q * 512:(q + 1) * 512], in_=h32[:, kc, :]),
                     reads=[h32], writes=[h2T])
            for sub in range(4):
                ti = q * 4 + sub
                pq = pr[sub % 2]; w = sm[sub % 2]
                for kc in range(8):
                    c.op("pe", lambda e: e.matmul(pq[:, 0:NE], lhsT=h32[:, kc, sub * 128:(sub + 1) * 128], rhs=wr[:, kc, :],
                                                  start=(kc == 0), stop=(kc == 7)), reads=[h32, wr], writes=[pq], signal=(kc == 7))
                lg = w[:, 0:NE]; mk = w[:, NE:2 * NE]; ex = w[:, 2 * NE:3 * NE]; em = w[:, 3 * NE:4 * NE]
                m8 = w[:, 4 * NE:4 * NE + 8]; nm = w[:, 4 * NE + 8:4 * NE + 9]; sv = w[:, 4 * NE + 9:4 * NE + 10]
                rs = w[:, 4 * NE + 10:4 * NE + 11]
                c.op("dve", lambda e: e.tensor_tensor(out=lg, in0=pq[:, 0:NE], in1=brB[:], op=ALU.add), reads=[pq, brB], writes=[w])
                c.op("dve", lambda e: e.max(out=m8, in_=lg), reads=[w], writes=[w])
                c.op("dve", lambda e: e.tensor_scalar(out=mk, in0=lg, scalar1=m8[:, 3:4], scalar2=None, op0=ALU.is_ge), reads=[w], writes=[w])
                c.op("dve", lambda e: e.tensor_scalar(out=nm, in0=m8[:, 0:1], scalar1=-1.0, scalar2=None, op0=ALU.mult), reads=[w], writes=[w])
                c.op("act", lambda e: e.activation(out=ex, in_=lg, func=AF.Exp, bias=nm), reads=[w], writes=[w])
                c.op("dve", lambda e: e.scalar_tensor_tensor(out=em, in0=ex, scalar=1.0, in1=mk, op0=ALU.mult, op1=ALU.mult, accum_out=sv),
                     reads=[w], writes=[w])
                c.op("dve", lambda e: e.reciprocal(out=rs, in_=sv), reads=[w], writes=[w])
                c.op("dve", lambda e: e.tensor_scalar(out=gates[:, ti, :], in0=em, scalar1=rs, scalar2=None, op0=ALU.mult),
                     reads=[w], writes=[gates])
                c.op("pe", lambda e: e.transpose(pq[0:NE, 128:256], gates[:, ti, :], ident[:]), reads=[gates, ident], writes=[pq])
                c.op("act", lambda e: e.copy(out=gTs[:], in_=pq[0:NE, 128:256]), reads=[pq], writes=[gTs])
                for half in range(2):
                    c.op("pe", lambda e: e.matmul(pb[:, half * 512:(half + 1) * 512], lhsT=gTs[:], rhs=b2s[:, half * 512:(half + 1) * 512],
                                                  start=True, stop=True), reads=[gTs, b2s], writes=[pb])
                c.op("act", lambda e: e.copy(out=yacc[:, ti, :], in_=pb[:]), reads=[pb], writes=[yacc])
        c.barrier()
        c.release(m2)

        m3 = c.mark()
        w1b = [c.sbuf(f"w1b{i}", [128, 8, 2, 512], BF16) for i in range(2)]
        w2b = [c.sbuf(f"w2b{i}", [128, 4, D], BF16) for i in range(2)]
        stg = [c.sbuf(f"stg{i}", [128, 2, D], F32) for i in range(2)]
        abuf = [c.sbuf(f"abuf{i}", [128, 4, 512], BF16) for i in range(2)]
        t1 = [c.sbuf(f"t1_{i}", [128, 512], F32) for i in range(2)]
        t2 = [c.sbuf(f"t2_{i}", [128, 512], F32) for i in range(2)]
        t3 = [c.sbuf(f"t3_{i}", [128, 512], F32) for i in range(2)]
        pg = [c.psum(f"pg{i}", [128, 512], F32) for i in range(2)]
        pl = [c.psum(f"pl{i}", [128, 512], F32) for i in range(2)]
        py = [c.psum(f"py{i}", [128, D], F32) for i in range(2)]
        units = [(e, hf) for e in range(NE) for hf in range(2)]
        pcount = [0]

        def load_piece(u, pi):
            e, hf = units[u]
            bi = u % 2
            si = pcount[0] % 2
            pcount[0] += 1
            st = stg[si]
            if pi < 4:
                kc0 = pi * 2
                src = P.w_exp1[l, e, kc0 * 128:(kc0 + 2) * 128, hf * 1024:(hf + 1) * 1024].rearrange("(kc p) n -> p kc n", p=128)
                c.dma("sp", st[:], src, reads=[WCONST], writes=[st], sem=s_w[si])
                c.op("act", lambda en: en.copy(out=w1b[bi][:, kc0:kc0 + 2, :, :],
                                               in_=st[:].rearrange("p kc (f t) -> p kc t f", t=2)),
                     reads=[st], writes=[w1b[bi]])
            else:
                fc0 = (pi - 4) * 2
                r0 = (hf * 4 + fc0) * 128
                src = P.w_exp2[l, e, r0:r0 + 256, :].rearrange("(fc p) n -> p fc n", p=128)
                c.dma("sp", st[:], src, reads=[WCONST], writes=[st], sem=s_w[si])
                c.op("act", lambda en: en.copy(out=w2b[bi][:, fc0:fc0 + 2, :], in_=st[:]), reads=[st], writes=[w2b[bi]])

        for pi in range(6):
            load_piece(0, pi)
        ai = 0
        for u, (e, hf) in enumerate(units):
            bi = u % 2
            for q in range(NQ):
                if u + 1 < len(units):
                    lo, hi = (q * 6) // NQ, ((q + 1) * 6) // NQ
                    for pi in range(lo, hi):
                        load_piece(u + 1, pi)
                ab = abuf[ai % 2]
                ai += 1
                for fc in range(4):
                    bg = pg[fc % 2]; bl = pl[fc % 2]
                    a1_, a2_, a3_ = t1[fc % 2], t2[fc % 2], t3[fc % 2]
                    for kc in range(8):
                        c.op("pe", lambda en: en.matmul(bg[:], lhsT=w1b[bi][:, kc, 0, fc * 128:(fc + 1) * 128],
                                                        rhs=h2T[:, kc, q * 512:(q + 1) * 512], start=(kc == 0), stop=(kc == 7)),
                             reads=[w1b[bi], h2T], writes=[bg], signal=(kc == 7))
                    for kc in range(8):
                        c.op("pe", lambda en: en.matmul(bl[:], lhsT=w1b[bi][:, kc, 1, fc * 128:(fc + 1) * 128],
                                                        rhs=h2T[:, kc, q * 512:(q + 1) * 512], start=(kc == 0), stop=(kc == 7)),
                             reads=[w1b[bi], h2T], writes=[bl], signal=(kc == 7))
                    bcol = hf * 4 + fc
                    c.op("dve", lambda en: en.tensor_scalar(out=a1_[:], in0=bg[:], scalar1=b1T[:, e, bcol:bcol + 1], scalar2=7.0,
                                                            op0=ALU.add, op1=ALU.min), reads=[bg, b1T], writes=[a1_])
                    c.op("act", lambda en: en.activation(out=a2_[:], in_=a1_[:], func=AF.Sigmoid, scale=1.702), reads=[a1_], writes=[a2_])
                    c.op("dve", lambda en: en.tensor_scalar(out=a3_[:], in0=bl[:], scalar1=b1T[:, e, 8 + bcol:9 + bcol], scalar2=7.0,
                                                            op0=ALU.add, op1=ALU.min), reads=[bl, b1T], writes=[a3_])
                    c.op("dve", lambda en: en.tensor_scalar(out=a3_[:], in0=a3_[:], scalar1=-7.0, scalar2=1.0,
                                                            op0=ALU.max, op1=ALU.add), reads=[a3_], writes=[a3_])
                    c.op("pool", lambda en: en.tensor_tensor(out=a1_[:], in0=a1_[:], in1=a2_[:], op=ALU.mult), reads=[a1_, a2_], writes=[a1_])
                    c.op("pool", lambda en: en.tensor_tensor(out=ab[:, fc, :], in0=a1_[:], in1=a3_[:], op=ALU.mult), reads=[a1_, a3_], writes=[ab])
                for sub in range(4):
                    ti = q * 4 + sub
                    yy = py[sub % 2]
                    for half in range(2):
                        for fc in range(4):
                            c.op("pe", lambda en: en.matmul(yy[:, half * 512:(half + 1) * 512], lhsT=ab[:, fc, sub * 128:(sub + 1) * 128],
                                                            rhs=w2b[bi][:, fc, half * 512:(half + 1) * 512], start=(fc == 0), stop=(fc == 3)),
                                 reads=[ab, w2b[bi]], writes=[yy], signal=(fc == 3 and half == 1))
                    c.op("dve", lambda en: en.scalar_tensor_tensor(out=yacc[:, ti, :], in0=yy[:], scalar=gates[:, ti, e:e + 1], in1=yacc[:, ti, :],
                                                                   op0=ALU.mult, op1=ALU.add), reads=[yy, gates, yacc], writes=[yacc])
        c.barrier()
        c.release(m3)

        m4 = c.mark()
        xt = [c.sbuf(f"xt{i}", [128, D], F32) for i in range(2)]
        xo = [c.sbuf(f"xo{i}", [128, D], F32) for i in range(2)]
        for ti in range(NTG):
            r0 = tok0 + ti * 128
            xb = xt[ti % 2]; ob = xo[ti % 2]
            c.dma("sp", xb[:], x_src[r0:r0 + 128, :], reads=[X_SRC], writes=[xb], sem=s_x[ti % 2])
            c.op("dve", lambda en: en.tensor_tensor(out=ob[:], in0=yacc[:, ti, :], in1=g2B[:], op=ALU.mult), reads=[yacc, g2B], writes=[ob])
            c.op("pool", lambda en: en.tensor_tensor(out=ob[:], in0=ob[:], in1=xb[:], op=ALU.add), reads=[ob, xb], writes=[ob])
            c.dma("sp", x_dst[r0:r0 + 128, :], ob[:], reads=[ob], writes=[X_DST], sem=s_st[ti % 2])
        c.barrier()
        c.release(m4)
    c.release(mL)


def alibi_slopes():
    return [2.0 ** (-8.0 * i / 12.0) for i in range(1, 13)]


def build_program(S=4096, L=4, NE=32, dbg=False):
    NT = S // 128
    NG = S // 512
    NB = S // 256
    nc = bass.Bass("TRN2", target_bir_lowering=False)
    c = Ctx(nc)

    def din(name, shape):
        return nc.dram_tensor(name, shape, F32, kind="ExternalInput").ap()

    x_in = din("x", [S, D]); c_in = din("c", [1, D])
    w_ada = din("w_ada", [L, D, 6 * D]); b_ada = din("b_ada", [L, 6 * D])
    norm_gain = din("norm_gain", [L, 2, D]); w_in = din("w_in", [L, D, 5380])
    b_fgate = din("b_fgate", [L, 4]); qk_gain = din("qk_gain", [L, 6, 64])
    attn_sinks = din("attn_sinks", [L, 8])
    w_br_fox = din("w_br_fox", [L, 256, D]); w_br_swa = din("w_br_swa", [L, 512, D])
    w_br_moba = din("w_br_moba", [L, 256, D]); w_out = din("w_out", [L, D, D])
    w_router = din("w_router", [L, D, NE]); b_router = din("b_router", [L, NE])
    w_exp1 = din("w_exp1", [L, NE, D, 2 * D]); b_exp1 = din("b_exp1", [L, NE, 2 * D])
    w_exp2 = din("w_exp2", [L, NE, D, D]); b_exp2 = din("b_exp2", [L, NE, D])
    y_out = nc.dram_tensor("y", [S, D], F32, kind="ExternalOutput").ap()

    def dscr(name, shape, dt):
        kind = "ExternalOutput" if dbg else "Internal"
        return nc.dram_tensor(name, shape, dt, kind=kind).ap()

    xres_d = dscr("xres_d", [S, D], F32)
    hT_d = dscr("hT_d", [D, S], BF16)
    qT_d = dscr("qT_d", [1024, S], BF16)
    kT_d = dscr("kT_d", [640, S], BF16)
    v_d = dscr("v_d", [S, 650], BF16)
    cumT_d = dscr("cumT_d", [4, S], F32)
    mod_d = dscr("mod_d", [L, 6 * D], F32)

    X_IN = Obj("x_in"); XRES = Obj("xres"); HT = Obj("hT"); QT = Obj("qT"); KT = Obj("kT")
    VD = Obj("vd"); CUMT = Obj("cumT"); MODD = Obj("modd"); YOUT = Obj("yout"); WCONST = Obj("w")

    s_const = c.new_sem(16, "s_const")
    s_x = [c.new_sem(16, f"s_x{i}") for i in range(2)]
    s_w = [c.new_sem(16, f"s_w{i}") for i in range(4)]
    s_st = [c.new_sem(16, f"s_st{i}") for i in range(4)]
    s_a = [c.new_sem(16, f"s_a{i}") for i in range(6)]

    slopes = alibi_slopes()

    ident = c.sbuf("ident", [128, 128], F32)
    identb = c.sbuf("identb", [128, 128], BF16)
    ones_f = c.sbuf("ones_f", [128, 128], F32)
    c.op("pool", lambda e: e.memset(ident[:], 0.0), writes=[ident])
    c.op("pool", lambda e: e.affine_select(out=ident[:], in_=ident[:], pattern=[[-1, 128]],
                                           compare_op=ALU.not_equal, fill=1.0, base=0,
                                           channel_multiplier=1), reads=[ident], writes=[ident])
    c.op("dve", lambda e: e.tensor_copy(out=identb[:], in_=ident[:]), reads=[ident], writes=[identb])
    c.op("pool", lambda e: e.memset(ones_f[:], 1.0), writes=[ones_f])
    modc = c.sbuf("modc", [128, L, 48], F32)
    a1c = c.sbuf("a1c", [128, 8], F32); a2c = c.sbuf("a2c", [128, 8], F32)
    ngc = c.sbuf("ngc", [128, 2, 8], F32)

    m0 = c.mark()
    condc = c.sbuf("condc", [128, 8], F32)
    ccol = c.sbuf("ccol", [128, 8], F32)
    with nc.allow_non_contiguous_dma("small vector relayout"):
        c.dma("sp", ccol[:], c_in.rearrange("o (k p) -> p (o k)", p=128), reads=[WCONST], writes=[ccol], sem=s_const)
    c.op("act", lambda e: e.activation(out=condc[:], in_=ccol[:], func=AF.Silu), reads=[ccol], writes=[condc])
    wa = [c.sbuf(f"wa{i}", [128, 6 * D], F32) for i in range(2)]
    modrow = c.sbuf("modrow", [1, 6 * D], F32)
    brow = c.sbuf("brow", [1, 6 * D], F32)
    pm = [c.psum(f"pm{i}", [128, 512], F32) for i in range(4)]
    for l in range(L):
        c.dma("sp", brow[:], b_ada[l:l + 1, :], reads=[WCONST], writes=[brow], sem=s_const)
        for half in range(3):
            for kc in range(8):
                wt = wa[kc % 2]
                c.dma("sp", wt[:, half * 2048:(half + 1) * 2048],
                      w_ada[l, kc * 128:(kc + 1) * 128, half * 2048:(half + 1) * 2048],
                      reads=[WCONST], writes=[wt], sem=s_w[kc % 2])
                for n in range(4):
                    c.op("pe", lambda e: e.matmul(pm[n][0:1, :], lhsT=condc[:, kc:kc + 1],
                                                  rhs=wt[:, half * 2048 + n * 512: half * 2048 + (n + 1) * 512],
                                                  start=(kc == 0), stop=(kc == 7)),
                         reads=[condc, wt], writes=[pm[n]], signal=(kc == 7 or n == 3))
            for n in range(4):
                cs = half * 2048 + n * 512
                c.op("dve", lambda e: e.tensor_tensor(out=modrow[:, cs:cs + 512], in0=pm[n][0:1, :],
                                                      in1=brow[:, cs:cs + 512], op=ALU.add),
                     reads=[pm[n], brow], writes=[modrow])
        c.dma("sp", mod_d[l:l + 1, :], modrow[:], reads=[modrow], writes=[MODD], sem=s_st[0])
    with nc.allow_non_contiguous_dma("small vector relayout"):
        for l in range(L):
            c.dma("sp", modc[:, l, :], mod_d[l:l + 1, :].rearrange("o (j p) -> p (o j)", p=128),
                  reads=[MODD], writes=[modc], sem=s_const)
    c.barrier()
    c.release(m0)

    CD, CONSTD = attn_consts(c, nc, NB, s_const, dscr, ident)
    for l in range(L):
        last = (l == L - 1)
        x_src, X_SRC = (x_in, X_IN) if l == 0 else (xres_d, XRES)

        with nc.allow_non_contiguous_dma("small vector relayout"):
            c.dma("sp", ngc[:], norm_gain[l].rearrange("t (j p) -> p t j", p=128), reads=[WCONST], writes=[ngc], sem=s_const)
        c.op("dve", lambda e: e.scalar_tensor_tensor(out=a1c[:], in0=modc[:, l, 8:16], scalar=1.0, in1=ngc[:, 0, :],
                                                     op0=ALU.add, op1=ALU.mult), reads=[modc, ngc], writes=[a1c])
        c.op("dve", lambda e: e.scalar_tensor_tensor(out=a2c[:], in0=modc[:, l, 32:40], scalar=1.0, in1=ngc[:, 1, :],
                                                     op0=ALU.add, op1=ALU.mult), reads=[modc, ngc], writes=[a2c])

        skip_mixer = getattr(build_program, 'skip_mixer', False)
        skip_moe = getattr(build_program, 'skip_moe', False)
        P = SimpleNamespace(**locals())
        if not skip_mixer:
            mixer_layer(P)
        if not skip_moe:
            moe_layer(P)

    sp = c.E["sp"]
    c._collect(sp, [YOUT], ())
    c.close()
    return nc


_NAMES = ["w_ada", "b_ada", "norm_gain", "w_in", "b_fgate", "qk_gain", "attn_sinks", "w_br_fox", "w_br_swa",
          "w_br_moba", "w_out", "w_router", "b_router", "w_exp1", "b_exp1", "w_exp2", "b_exp2"]


def kernel(**inputs):
    x = np.ascontiguousarray(np.asarray(inputs["x"], dtype=np.float32))
    cvec = np.ascontiguousarray(np.asarray(inputs["c"], dtype=np.float32))
    B, S, _ = x.shape
    L = int(np.asarray(inputs["w_ada"]).shape[0])
    NE = int(np.asarray(inputs["w_router"]).shape[2])
    shared = {k: np.ascontiguousarray(np.asarray(inputs[k], dtype=np.float32)) for k in _NAMES}
    nc = build_program(S=S, L=L, NE=NE)
    in_maps = []
    for b in range(B):
        m = {"x": x[b], "c": cvec[b:b + 1]}
        m.update(shared)
        in_maps.append(m)
    res = run_bass_kernel_spmd(nc, in_maps, core_ids=list(range(B)))
    return np.stack([np.asarray(r["y"], dtype=np.float32) for r in res.results], axis=0)
```

```python
import numpy as np
from types import SimpleNamespace
import concourse.bass as bass
import concourse.mybir as mybir
from concourse.bass_utils import run_bass_kernel_spmd

F32 = mybir.dt.float32
BF16 = mybir.dt.bfloat16
AF = mybir.ActivationFunctionType
ALU = mybir.AluOpType
AX = mybir.AxisListType

D = 1024
NEG = -1.0e30
BIG = 30000.0
CSH = 8.0
EPS = 1e-6


class Sem:
    __slots__ = ("h", "issued", "step")

    def __init__(self, h, step):
        self.h = h
        self.issued = 0
        self.step = step


class Obj:
    __slots__ = ("name", "w", "r", "dsem", "t")

    def __init__(self, name, t=None):
        self.name = name
        self.w = []
        self.r = []
        self.dsem = None
        self.t = t

    def __getitem__(self, k):
        return self.t[k]


class Eng:
    __slots__ = ("name", "h", "sem", "seen")

    def __init__(self, name, h, sem):
        self.name = name
        self.h = h
        self.sem = sem
        self.seen = {}


class Ctx:
    def __init__(self, nc):
        self.nc = nc
        self._stack = []
        self.sems = []
        self.E = {}
        for name, h in (("pe", nc.tensor), ("dve", nc.vector), ("act", nc.scalar),
                        ("pool", nc.gpsimd), ("sp", nc.sync)):
            self.E[name] = Eng(name, h, self.new_sem(1, "e_" + name))
        self.n_wait = 0
        self.n_ins = 0

    def new_sem(self, step, name=None):
        cm = self.nc.semaphore(name)
        h = cm.__enter__()
        s = Sem(h, step)
        self.sems.append(s)
        return s

    def mark(self):
        return len(self._stack)

    def release(self, mark):
        while len(self._stack) > mark:
            self._stack.pop().__exit__(None, None, None)

    def sbuf(self, name, shape, dt, dsem=None):
        self.n_ins += 0
        self._uid = getattr(self, "_uid", 0) + 1
        name = f"{name}_u{self._uid}"
        cm = self.nc.sbuf_tensor(name, shape, dt)
        t = cm.__enter__()
        self._stack.append(cm)
        o = Obj(name, t)
        o.dsem = dsem
        return o

    def psum(self, name, shape, dt):
        self._uid = getattr(self, "_uid", 0) + 1
        name = f"{name}_u{self._uid}"
        cm = self.nc.psum_tensor(name, shape, dt)
        t = cm.__enter__()
        self._stack.append(cm)
        return Obj(name, t)

    def _collect(self, eng, reads, writes):
        need = {}
        for o in reads:
            for (s, v) in o.w:
                if need.get(s, 0) < v:
                    need[s] = v
        for o in writes:
            for (s, v) in o.w:
                if need.get(s, 0) < v:
                    need[s] = v
            for (s, v) in o.r:
                if need.get(s, 0) < v:
                    need[s] = v
        for s, v in need.items():
            if s is eng.sem and eng.name == "pe":
                continue
            if s.step == 16:
                v = s.issued
            assert v <= s.issued, f"wait on unsignaled ticket eng={eng.name}"
            if eng.seen.get(s, 0) < v:
                eng.h.wait_ge(s.h, v)
                eng.seen[s] = v
                self.n_wait += 1

    def _record(self, tk, reads, writes):
        for o in writes:
            o.w = [tk]
            o.r = []
        for o in reads:
            if o not in writes:
                o.r = [t for t in o.r if t[0] is not tk[0]] + [tk]

    def op(self, en, fn, reads=(), writes=(), signal=True):
        eng = self.E[en]
        self._collect(eng, reads, writes)
        ins = fn(eng.h)
        self.n_ins += 1
        if signal:
            ins.then_inc(eng.sem.h, 1)
            eng.sem.issued += 1
            tk = (eng.sem, eng.sem.issued)
        else:
            tk = (eng.sem, eng.sem.issued + 1)
        self._record(tk, reads, writes)
        return ins

    def dma(self, qn, out, in_, reads=(), writes=(), sem=None, **kw):
        eng = self.E[qn]
        self._collect(eng, reads, writes)
        ins = eng.h.dma_start(out=out, in_=in_, **kw)
        ins.then_inc(sem.h, 16)
        sem.issued += 16
        self.n_ins += 1
        self._record((sem, sem.issued), reads, writes)
        return ins

    def barrier(self):
        for eng in self.E.values():
            for s in self.sems:
                if s is eng.sem:
                    continue
                if s.issued > 0 and eng.seen.get(s, 0) < s.issued:
                    eng.h.wait_ge(s.h, s.issued)
                    eng.seen[s] = s.issued

    def close(self):
        self.release(0)


def attn_consts(c, nc, NB, s_const, dscr, ident):
    m0 = c.mark()
    CONSTD = Obj("constd")
    CD = {}
    utri = c.sbuf("utri", [128, 128], F32)
    c.op("pool", lambda e: e.memset(utri[:], 1.0), writes=[utri])
    c.op("pool", lambda e: e.affine_select(out=utri[:], in_=utri[:], pattern=[[1, 128]], compare_op=ALU.is_ge, fill=0.0, base=0,
                                           channel_multiplier=-1), reads=[utri], writes=[utri])
    cmask = c.sbuf("cmask", [128, 4, 512], F32)
    c.op("pool", lambda e: e.memset(cmask[:], 0.0), writes=[cmask])
    for j in range(4):
        c.op("pool", lambda e: e.affine_select(out=cmask[:, j, :], in_=cmask[:, j, :], pattern=[[1, 512]], compare_op=ALU.is_ge, fill=NEG,
                                               base=-128 * j, channel_multiplier=-1), reads=[cmask], writes=[cmask])
    dist0 = c.sbuf("dist0", [128, 512], F32)
    c.op("pool", lambda e: e.iota(dist0[:], pattern=[[1, 512]], base=0, channel_multiplier=-1, allow_small_or_imprecise_dtypes=True), writes=[dist0])
    swM = c.sbuf("swM", [128, 5, 512], F32); swD = c.sbuf("swD", [128, 5, 512], F32)
    c.op("pool", lambda e: e.memset(swM[:], 0.0), writes=[swM])
    for jj in range(5):
        j = jj - 1
        c.op("pool", lambda e: e.affine_select(out=swM[:, jj, :], in_=swM[:, jj, :], pattern=[[1, 512]], compare_op=ALU.is_ge, fill=NEG,
                                               base=-128 * j, channel_multiplier=-1), reads=[swM], writes=[swM])
        c.op("pool", lambda e: e.affine_select(out=swM[:, jj, :], in_=swM[:, jj, :], pattern=[[-1, 512]], compare_op=ALU.is_ge, fill=NEG,
                                               base=127 + 128 * j, channel_multiplier=1), reads=[swM], writes=[swM])
        c.op("pool", lambda e: e.iota(swD[:, jj, :], pattern=[[1, 512]], base=-128 * j, channel_multiplier=-1, allow_small_or_imprecise_dtypes=True), writes=[swD])

    ownN = c.sbuf("ownN", [128, NB, NB], F32); vmask = c.sbuf("vmask", [128, NB, NB], F32); eqB = c.sbuf("eqB", [128, NB, NB], F32)
    eself = c.sbuf("eself", [NB, NB, 128], F32)
    c.op("pool", lambda e: e.memset(ownN[:], 0.0), writes=[ownN])
    c.op("pool", lambda e: e.memset(vmask[:], 1.0), writes=[vmask])
    c.op("pool", lambda e: e.memset(eqB[:], 0.0), writes=[eqB])
    c.op("pool", lambda e: e.memset(eself[:], 1.0), writes=[eself])
    for o_ in range(NB):
        pat = [[-1, NB]]
        c.op("pool", lambda e: e.affine_select(out=ownN[:, o_, :], in_=ownN[:, o_, :], pattern=pat, compare_op=ALU.is_ge, fill=NEG, base=o_ - 1, channel_multiplier=0), reads=[ownN], writes=[ownN])
        c.op("pool", lambda e: e.affine_select(out=eqB[:, o_, :], in_=eqB[:, o_, :], pattern=pat, compare_op=ALU.is_equal, fill=-BIG, base=o_, channel_multiplier=0), reads=[eqB], writes=[eqB])
    c.op("dve", lambda e: e.tensor_scalar(out=vmask[:], in0=ownN[:], scalar1=0.0, scalar2=None, op0=ALU.is_equal), reads=[ownN], writes=[vmask])
    for n_ in range(NB):
        c.op("dve", lambda e: e.tensor_scalar(out=eself[:, n_, :], in0=eself[:, n_, :], scalar1=ident[0:NB, n_:n_ + 1], scalar2=None, op0=ALU.mult),
             reads=[eself, ident], writes=[eself])

    for t_, nm_, shp in ((utri, "utri", [128, 128]), (cmask, "cmask", [128, 2048]), (dist0, "dist0", [128, 512]), (swM, "swM", [128, 2560]),
                         (swD, "swD", [128, 2560]), (ownN, "ownN", [128, NB * NB]), (vmask, "vmask", [128, NB * NB]), (eqB, "eqB", [128, NB * NB]),
                         (eself, "eself", [NB, NB * 128])):
        CD[nm_] = dscr("cd_" + nm_, shp, F32)
        src = t_[:] if len(t_[:].shape) == 2 else t_[:].rearrange("p a b -> p (a b)")
        c.dma("sp", CD[nm_], src, reads=[t_], writes=[CONSTD], sem=s_const)
    c.barrier()
    c.release(m0)
    return CD, CONSTD

def mixer_layer(P):
    c, nc, l, S = P.c, P.nc, P.l, P.S
    NT = S // 128; NG = S // 512; NB = S // 256
    ident, identb, ones_f, modc, a1c = P.ident, P.identb, P.ones_f, P.modc, P.a1c
    s_const, s_x, s_w, s_st, s_a = P.s_const, P.s_x, P.s_w, P.s_st, P.s_a
    WCONST = P.WCONST
    x_src, X_SRC = P.x_src, P.X_SRC
    slopes = P.slopes
    hT_v = P.hT_d.rearrange("(kc p) t -> p kc t", p=128)
    qT_v = P.qT_d.rearrange("(cc p) t -> p cc t", p=128)
    kT_v = P.kT_d.rearrange("(cc p) t -> p cc t", p=128)

    mM = c.mark()
    lf_all = c.sbuf("lf_all", [128, 4, NT], F32)
    m1 = c.mark()
    gQ = c.sbuf("gQ", [128, 1024], F32); gK = c.sbuf("gK", [128, 640], F32)
    bfB = c.sbuf("bfB", [128, 4], F32)
    qrow = [0] * 4 + [2] * 8 + [4] * 4
    krow = [1] * 4 + [3] * 2 + [5] * 4
    for h in range(16):
        c.dma("sp", gQ[:, h * 64:(h + 1) * 64], P.qk_gain[l, qrow[h]:qrow[h] + 1, :].partition_broadcast(128), reads=[WCONST], writes=[gQ], sem=s_const)
    for h in range(10):
        c.dma("sp", gK[:, h * 64:(h + 1) * 64], P.qk_gain[l, krow[h]:krow[h] + 1, :].partition_broadcast(128), reads=[WCONST], writes=[gK], sem=s_const)
    c.dma("sp", bfB[:], P.b_fgate[l:l + 1, :].partition_broadcast(128), reads=[WCONST], writes=[bfB], sem=s_const)
    c.op("dve", lambda e: e.tensor_scalar(out=gQ[:], in0=gQ[:], scalar1=0.125, scalar2=None, op0=ALU.mult), reads=[gQ], writes=[gQ])
    wqkv = c.sbuf("wqkv", [128, 8, 2308], BF16)
    wst = [c.sbuf(f"wst{i}", [128, 2308], F32) for i in range(2)]
    segs = [(0, 256, 0), (772, 1284, 256), (1540, 1796, 768),
            (256, 512, 1024), (1284, 1412, 1280), (1796, 2052, 1408),
            (512, 768, 1664), (1412, 1540, 1920), (2052, 2308, 2048),
            (768, 772, 2304)]
    for kc in range(8):
        st = wst[kc % 2]
        c.dma("sp", st[:], P.w_in[l, kc * 128:(kc + 1) * 128, 0:2308], reads=[WCONST], writes=[st], sem=s_w[kc % 2])
        for si, (a, b, d0) in enumerate(segs):
            en = "act" if si % 2 == 0 else "dve"
            if en == "act":
                c.op("act", lambda e: e.copy(out=wqkv[:, kc, d0:d0 + (b - a)], in_=st[:, a:b]), reads=[st], writes=[wqkv])
            else:
                c.op("dve", lambda e: e.tensor_copy(out=wqkv[:, kc, d0:d0 + (b - a)], in_=st[:, a:b]), reads=[st], writes=[wqkv])
    xs = [c.sbuf(f"xs{i}", [128, D], F32) for i in range(2)]
    junk = c.sbuf("junk", [128, D], F32)
    ss = [c.sbuf(f"ss{i}", [128, 4], F32) for i in range(2)]
    xn = c.sbuf("xn", [128, 4, D], F32)
    hTg = c.sbuf("hTg", [128, 8, 512], BF16)
    sqs = c.sbuf("sqs", [128, 1664], F32)
    rq = c.sbuf("rq", [128, 64], F32)
    qn = c.sbuf("qn", [128, 1024], BF16); kn = c.sbuf("kn", [128, 640], BF16)
    qTs = c.sbuf("qTs", [128, 8, 512], BF16); kTs = c.sbuf("kTs", [128, 5, 512], BF16)
    vst = c.sbuf("vst", [128, 4, 650], BF16)
    lft = c.sbuf("lft", [128, 8], F32)
    c.op("pool", lambda e: e.memset(vst[:], 1.0), writes=[vst])
    T0 = c.psum("T0", [128, 512], F32); T1 = c.psum("T1", [128, 512], F32)
    pq = c.psum("pq", [128, 1024], F32); pk = c.psum("pk", [128, 1024], F32); pv = c.psum("pv", [128, 1024], F32)
    TT = [T0, T1]
    for q in range(NG):
        for sub in range(4):
            i = q * 4 + sub
            r0 = i * 128
            xb = xs[i % 2]; sb = ss[i % 2]
            c.dma("sp", xb[:], x_src[r0:r0 + 128, :], reads=[X_SRC], writes=[xb], sem=s_x[i % 2])
            c.op("act", lambda e: e.activation(out=junk[:], in_=xb[:], func=AF.Square, accum_out=sb[:, 0:1]), reads=[xb], writes=[junk, sb])
            c.op("act", lambda e: e.activation(out=sb[:, 1:2], in_=sb[:, 0:1], func=AF.Sqrt, scale=1.0 / D, bias=EPS), reads=[sb], writes=[sb])
            c.op("dve", lambda e: e.reciprocal(out=sb[:, 2:3], in_=sb[:, 1:2]), reads=[sb], writes=[sb])
            c.op("dve", lambda e: e.tensor_scalar(out=xn[:, sub, :], in0=xb[:], scalar1=sb[:, 2:3], scalar2=None, op0=ALU.mult), reads=[xb, sb], writes=[xn])
        for kc in range(8):
            pp = TT[kc % 2]
            for sub in range(4):
                c.op("pe", lambda e: e.transpose(pp[:, sub * 128:(sub + 1) * 128], xn[:, sub, kc * 128:(kc + 1) * 128], ident[:]),
                     reads=[xn, ident], writes=[pp], signal=(sub == 3))
            c.op("act", lambda e: e.activation(out=hTg[:, kc, :], in_=pp[:], func=AF.Identity, scale=a1c[:, kc:kc + 1], bias=modc[:, l, kc:kc + 1]),
                 reads=[pp, a1c, modc], writes=[hTg])
        c.dma("sp", hT_v[:, :, q * 512:(q + 1) * 512], hTg[:], reads=[hTg], writes=[P.HT], sem=s_st[0])
        for sub in range(4):
            i = q * 4 + sub
            lhs = lambda kc: hTg[:, kc, sub * 128:(sub + 1) * 128]
            for (pt_, c0, n, w0) in ((pq, 0, 512, 0), (pq, 512, 512, 512), (pk, 0, 512, 1024), (pk, 512, 128, 1536),
                                     (pv, 0, 512, 1664), (pv, 512, 132, 2176)):
                for kc in range(8):
                    c.op("pe", lambda e: e.matmul(pt_[:, c0:c0 + n], lhsT=lhs(kc), rhs=wqkv[:, kc, w0:w0 + n], start=(kc == 0), stop=(kc == 7)),
                         reads=[hTg, wqkv], writes=[pt_], signal=(kc == 7))
            c.op("act", lambda e: e.activation(out=sqs[:, 0:1024], in_=pq[:], func=AF.Square), reads=[pq], writes=[sqs])
            c.op("act", lambda e: e.activation(out=sqs[:, 1024:1664], in_=pk[:, 0:640], func=AF.Square), reads=[pk], writes=[sqs])
            c.op("dve", lambda e: e.tensor_reduce(out=rq[:, 0:26], in_=sqs[:].rearrange("p (h d) -> p h d", d=64), axis=AX.X, op=ALU.add), reads=[sqs], writes=[rq])
            c.op("act", lambda e: e.activation(out=rq[:, 32:58], in_=rq[:, 0:26], func=AF.Sqrt, scale=1.0 / 64, bias=EPS), reads=[rq], writes=[rq])
            c.op("dve", lambda e: e.reciprocal(out=rq[:, 0:26], in_=rq[:, 32:58]), reads=[rq], writes=[rq])
            for h in range(16):
                c.op("dve", lambda e: e.scalar_tensor_tensor(out=qn[:, h * 64:(h + 1) * 64], in0=pq[:, h * 64:(h + 1) * 64], scalar=rq[:, h:h + 1],
                                                             in1=gQ[:, h * 64:(h + 1) * 64], op0=ALU.mult, op1=ALU.mult),
                     reads=[pq, rq, gQ], writes=[qn])
            for h in range(10):
                c.op("dve", lambda e: e.scalar_tensor_tensor(out=kn[:, h * 64:(h + 1) * 64], in0=pk[:, h * 64:(h + 1) * 64], scalar=rq[:, 16 + h:17 + h],
                                                             in1=gK[:, h * 64:(h + 1) * 64], op0=ALU.mult, op1=ALU.mult),
                     reads=[pk, rq, gK], writes=[kn])
            c.op("act", lambda e: e.copy(out=vst[:, sub, :].rearrange("p (h d) -> p h d", d=65)[:, :, 0:64],
                                         in_=pv[:, 0:640].rearrange("p (h d) -> p h d", d=64)), reads=[pv], writes=[vst])
            c.op("dve", lambda e: e.tensor_tensor(out=lft[:, 0:4], in0=pv[:, 640:644], in1=bfB[:], op=ALU.add), reads=[pv, bfB], writes=[lft])
            c.op("act", lambda e: e.activation(out=lft[:, 4:8], in_=lft[:, 0:4], func=AF.Exp, scale=-1.0), reads=[lft], writes=[lft])
            c.op("act", lambda e: e.activation(out=lft[:, 0:4], in_=lft[:, 4:8], func=AF.Ln, bias=1.0), reads=[lft], writes=[lft])
            c.op("dve", lambda e: e.tensor_scalar(out=lf_all[:, :, i], in0=lft[:, 0:4], scalar1=-1.0, scalar2=None, op0=ALU.mult), reads=[lft], writes=[lf_all])
            tq = T0[:].bitcast(BF16); tk = T1[:].bitcast(BF16)
            for cc in range(8):
                c.op("pe", lambda e: e.transpose(tq[:, cc * 128:(cc + 1) * 128], qn[:, cc * 128:(cc + 1) * 128], identb[:]),
                     reads=[qn, identb], writes=[T0], signal=(cc == 7))
            for cc in range(5):
                c.op("pe", lambda e: e.transpose(tk[:, cc * 128:(cc + 1) * 128], kn[:, cc * 128:(cc + 1) * 128], identb[:]),
                     reads=[kn, identb], writes=[T1], signal=(cc == 4))
            c.op("act", lambda e: e.copy(out=qTs[:, :, sub * 128:(sub + 1) * 128], in_=tq.rearrange("p (c t) -> p c t", t=128)), reads=[T0], writes=[qTs])
            c.op("dve", lambda e: e.tensor_copy(out=kTs[:, :, sub * 128:(sub + 1) * 128], in_=tk[:, 0:640].rearrange("p (c t) -> p c t", t=128)), reads=[T1], writes=[kTs])
        c.dma("sp", qT_v[:, :, q * 512:(q + 1) * 512], qTs[:], reads=[qTs], writes=[P.QT], sem=s_st[1])
        c.dma("sp", kT_v[:, :, q * 512:(q + 1) * 512], kTs[:], reads=[kTs], writes=[P.KT], sem=s_st[2])
        c.dma("sp", P.v_d[q * 512:(q + 1) * 512, :].rearrange("(s p) n -> p s n", p=128), vst[:], reads=[vst], writes=[P.VD], sem=s_st[3])
    c.barrier()
    c.release(m1)
    if getattr(build_program, "mixer_stop", 0) == 1:
        c.release(mM)
        return

    o_sb = c.sbuf("o_sb", [128, NT, D], BF16)
    m2 = c.mark()
    utri = c.sbuf("utri", [128, 128], F32); cmask = c.sbuf("cmask", [128, 4, 512], F32); dist0 = c.sbuf("dist0", [128, 512], F32)
    swM = c.sbuf("swM", [128, 5, 512], F32); swD = c.sbuf("swD", [128, 5, 512], F32)
    for t_, nm_ in ((utri, "utri"), (cmask, "cmask"), (dist0, "dist0"), (swM, "swM"), (swD, "swD")):
        c.dma("sp", t_[:], P.CD[nm_].rearrange("p (a b) -> p a b", b=512) if nm_ in ("cmask", "swM", "swD") else P.CD[nm_], reads=[P.CONSTD], writes=[t_], sem=s_const)
    sinkE = c.sbuf("sinkE", [128, 8], F32)
    c.dma("sp", sinkE[:], P.attn_sinks[l:l + 1, :].partition_broadcast(128), reads=[WCONST], writes=[sinkE], sem=s_const)
    c.op("act", lambda e: e.activation(out=sinkE[:], in_=sinkE[:], func=AF.Exp, bias=-CSH), reads=[sinkE], writes=[sinkE])

    cumK = c.sbuf("cumK", [128, 4, NT], F32)
    negck = c.sbuf("negck", [128, 4, NT], F32)
    tot = c.sbuf("tot", [128, 4, NT], F32)
    off = c.sbuf("off", [128, 4, NT], F32)
    cTs = c.sbuf("cTs", [128, 128], F32)
    S0 = c.psum("S0", [128, 512], F32); S1 = c.psum("S1", [128, 512], F32)
    OA = [c.psum(f"OA{j}", [128, 512], F32) for j in range(2)]
    G0 = c.psum("G0", [128, 512], F32); G1 = c.psum("G1", [128, 512], F32)
    lf2 = lf_all[:].rearrange("p h i -> p (h i)")
    c.op("pe", lambda e: e.matmul(S0[:, 0:4 * NT], lhsT=ones_f[:], rhs=lf2, start=True, stop=True), reads=[ones_f, lf_all], writes=[S0])
    c.op("pe", lambda e: e.matmul(S1[:, 0:4 * NT], lhsT=utri[:], rhs=lf2, start=True, stop=True), reads=[utri, lf_all], writes=[S1])
    c.op("dve", lambda e: e.tensor_copy(out=tot[:].rearrange("p h i -> p (h i)"), in_=S0[:, 0:4 * NT]), reads=[S0], writes=[tot])
    c.op("dve", lambda e: e.memset(off[:], 0.0), writes=[off])
    for i in range(1, NT):
        c.op("dve", lambda e: e.tensor_tensor(out=off[:, :, i], in0=off[:, :, i - 1], in1=tot[:, :, i - 1], op=ALU.add), reads=[off, tot], writes=[off])
    c.op("dve", lambda e: e.tensor_tensor(out=cumK[:].rearrange("p h i -> p (h i)"), in0=S1[:, 0:4 * NT], in1=off[:].rearrange("p h i -> p (h i)"), op=ALU.add),
         reads=[S1, off], writes=[cumK])
    c.op("dve", lambda e: e.tensor_scalar(out=negck[:], in0=cumK[:], scalar1=-1.0, scalar2=-CSH, op0=ALU.mult, op1=ALU.add), reads=[cumK], writes=[negck])
    c.op("pe", lambda e: e.transpose(S0[0:4 * NT, 0:128], cumK[:].rearrange("p h i -> p (h i)"), ident[:]), reads=[cumK, ident], writes=[S0])
    c.op("dve", lambda e: e.tensor_copy(out=cTs[0:4 * NT, :], in_=S0[0:4 * NT, 0:128]), reads=[S0], writes=[cTs])
    c.dma("sp", P.cumT_d.rearrange("h (i t) -> (h i) t", t=128), cTs[0:4 * NT, :], reads=[cTs], writes=[P.CUMT], sem=s_st[0])

    qTc = [c.sbuf(f"qTc{i}", [128, S], BF16) for i in range(2)]
    kTc = [c.sbuf(f"kTc{i}", [128, S], BF16) for i in range(2)]
    vaug = [c.sbuf(f"vaug{i}", [128, NT, 65], BF16) for i in range(2)]
    biasb = [c.sbuf(f"biasb{i}", [128, max(S, 2560)], F32) for i in range(2)]
    tts = [c.sbuf(f"tts{i}", [128, 512], F32) for i in range(4)]
    pex = [c.sbuf(f"pex{i}", [128, 512], BF16) for i in range(4)]
    rz = c.sbuf("rz", [128, 8], F32)
    S2 = c.psum("S2", [128, 512], F32); S3 = c.psum("S3", [128, 512], F32)
    SS = [S0, S1, S2, S3]
    cnt = [0]

    seq = []

    def attn_tile(qap, kap, extra, bias_ap, mask_ap, act_bias, vap, first, last, rd, oset, fin=None, qlo=0, qhi=512):
        k_ = cnt[0] % 4
        cnt[0] += 1
        ps = SS[k_]; tt = tts[k_]; pe_ = pex[k_]
        OAb = OA[oset]

        def A():
            c.op("pe", lambda e: e.matmul(ps[:, qlo:qhi], lhsT=kap, rhs=qap[:, qlo:qhi], start=True, stop=(extra is None)), reads=rd, writes=[ps], signal=(extra is None))
            if extra is not None:
                c.op("pe", lambda e: e.matmul(ps[:, qlo:qhi], lhsT=extra[0], rhs=extra[1][:, qlo:qhi], start=False, stop=True), reads=rd, writes=[ps])

        def B():
            c.op("dve", lambda e: e.tensor_tensor(out=tt[:, qlo:qhi], in0=ps[:, qlo:qhi], in1=bias_ap[:, qlo:qhi], op=ALU.add), reads=[ps] + rd, writes=[tt])
            if mask_ap is not None:
                c.op("dve", lambda e: e.tensor_tensor(out=tt[:, qlo:qhi], in0=tt[:, qlo:qhi], in1=mask_ap[:, qlo:qhi], op=ALU.add), reads=[tt] + rd, writes=[tt])
            c.op("act", lambda e: e.activation(out=pe_[:, qlo:qhi], in_=tt[:, qlo:qhi], func=AF.Exp, bias=act_bias), reads=[tt] + rd, writes=[pe_])

        def C():
            for j in range(qlo // 128, qhi // 128):
                c.op("pe", lambda e: e.matmul(OAb[:, j * 128:j * 128 + 65], lhsT=pe_[:, j * 128:(j + 1) * 128], rhs=vap, start=(first and j == 0), stop=last,
                                              skip_group_check=True),
                     reads=[pe_] + rd, writes=[OAb], signal=(j == qhi // 128 - 1))
            if fin is not None:
                qt, col, sink_ap = fin
                for j in range(4):
                    zc = OAb[:, j * 128 + 64:j * 128 + 65]
                    if sink_ap is not None:
                        c.op("dve", lambda e: e.tensor_tensor(out=rz[:, j:j + 1], in0=zc, in1=sink_ap, op=ALU.add), reads=[OAb, sinkE], writes=[rz])
                        c.op("dve", lambda e: e.reciprocal(out=rz[:, 4 + j:5 + j], in_=rz[:, j:j + 1]), reads=[rz], writes=[rz])
                    else:
                        c.op("dve", lambda e: e.reciprocal(out=rz[:, 4 + j:5 + j], in_=zc), reads=[OAb], writes=[rz])
                    c.op("dve", lambda e: e.tensor_scalar(out=o_sb[:, qt * 4 + j, col:col + 64], in0=OAb[:, j * 128:j * 128 + 64], scalar1=rz[:, 4 + j:5 + j],
                                                          scalar2=None, op0=ALU.mult), reads=[OAb, rz], writes=[o_sb])
        seq.append(("tile", A, B, C))

    def load_v(buf, vh, semi):
        with nc.allow_non_contiguous_dma("v head slice"):
            c.dma("sp", buf[:], P.v_d[:, vh * 65:(vh + 1) * 65].rearrange("(i p) n -> p i n", p=128), reads=[P.VD], writes=[buf], sem=s_a[semi])

    hcount = [0]
    qtc = [0]
    for h in range(4):
        b = hcount[0] % 2; hcount[0] += 1
        hp = (h % 2) * 64
        qc, kc_, vb, bb = qTc[b], kTc[b], vaug[b], biasb[b]

        def prep(h=h, b=b, hp=hp, qc=qc, kc_=kc_, vb=vb, bb=bb):
            c.dma("sp", qc[hp:hp + 64, :], P.qT_d[h * 64:(h + 1) * 64, :], reads=[P.QT], writes=[qc], sem=s_a[b])
            c.dma("sp", kc_[hp:hp + 64, :], P.kT_d[h * 64:(h + 1) * 64, :], reads=[P.KT], writes=[kc_], sem=s_a[2 + b])
            load_v(vb, h, 4 + b)
            c.dma("sp", bb[:, 0:S], P.cumT_d[h:h + 1, :].partition_broadcast(128), reads=[P.CUMT], writes=[bb], sem=s_x[b])
        seq.append(("prep", prep))
        rd = [qc, kc_, vb, bb, negck, cmask]
        for qt in range(NG):
            nk = 4 * qt + 4
            oset = qtc[0] % 2; qtc[0] += 1
            for kt in range(nk):
                j = kt - 4 * qt
                attn_tile(qc[hp:hp + 64, qt * 512:(qt + 1) * 512], kc_[hp:hp + 64, kt * 128:(kt + 1) * 128], None,
                          bb[:, qt * 512:(qt + 1) * 512], cmask[:, j, :] if j >= 0 else None, negck[:, h, kt:kt + 1],
                          vb[:, kt, :], kt == 0, kt == nk - 1, rd, oset, (qt, h * 64, None) if kt == nk - 1 else None, qlo=max(j, 0) * 128)
    for h in range(8):
        b = hcount[0] % 2; hcount[0] += 1
        hp = (h % 2) * 64
        kvh = h // 4
        qc, kc_, vb, bb = qTc[b], kTc[b], vaug[b], biasb[b]

        def prep(h=h, b=b, hp=hp, kvh=kvh, qc=qc, kc_=kc_, vb=vb, bb=bb):
            c.dma("sp", qc[hp:hp + 64, :], P.qT_d[256 + h * 64:256 + (h + 1) * 64, :], reads=[P.QT], writes=[qc], sem=s_a[b])
            c.dma("sp", kc_[hp:hp + 64, :], P.kT_d[256 + kvh * 64:256 + (kvh + 1) * 64, :], reads=[P.KT], writes=[kc_], sem=s_a[2 + b])
            load_v(vb, 4 + kvh, 4 + b)
            for jj in range(5):
                c.op("dve", lambda e: e.scalar_tensor_tensor(out=bb[:, jj * 512:(jj + 1) * 512], in0=swD[:, jj, :], scalar=-slopes[h], in1=swM[:, jj, :],
                                                             op0=ALU.mult, op1=ALU.add), reads=[swD, swM], writes=[bb])
        seq.append(("prep", prep))
        rd = [qc, kc_, vb, bb]
        for qt in range(NG):
            kts = [kt for kt in range(4 * qt - 1, 4 * qt + 4) if kt >= 0]
            oset = qtc[0] % 2; qtc[0] += 1
            for kt in kts:
                jj = kt - 4 * qt + 1
                attn_tile(qc[hp:hp + 64, qt * 512:(qt + 1) * 512], kc_[hp:hp + 64, kt * 128:(kt + 1) * 128], None,
                          bb[:, jj * 512:(jj + 1) * 512], None, -CSH, vb[:, kt, :], kt == kts[0], kt == kts[-1], rd, oset,
                          (qt, 256 + h * 64, sinkE[:, h:h + 1]) if kt == kts[-1] else None,
                          qlo=max(jj - 1, 0) * 128, qhi=min(jj + 1, 4) * 128)
    m3 = c.mark()
    ownN = c.sbuf("ownN", [128, NB, NB], F32); vmask = c.sbuf("vmask", [128, NB, NB], F32); eqB = c.sbuf("eqB", [128, NB, NB], F32)
    eself = c.sbuf("eself", [NB, NB, 128], F32); esel = c.sbuf("esel", [NB, NB, 128], BF16)
    for t_, nm_ in ((ownN, "ownN"), (vmask, "vmask"), (eqB, "eqB")):
        c.dma("sp", t_[:], P.CD[nm_].rearrange("p (a b) -> p a b", b=NB), reads=[P.CONSTD], writes=[t_], sem=s_const)
    c.dma("sp", eself[:], P.CD["eself"].rearrange("p (a b) -> p a b", b=128), reads=[P.CONSTD], writes=[eself], sem=s_const)
    c.op("dve", lambda e: e.tensor_copy(out=esel[:], in_=eself[:]), reads=[eself], writes=[esel])
    kms = c.sbuf("kms", [128, NB], F32); kmb = c.sbuf("kmb", [128, NB], BF16)
    selT = [c.sbuf("selT0", [NB, S], BF16)] * 2
    gw = [c.sbuf(f"gw{i}", [128, 4 * NB + 8], F32) for i in range(2)]
    GG = [G0, G1]
    for h in range(4):
        b = hcount[0] % 2; hcount[0] += 1
        hp = (h % 2) * 64
        qc, kc_, vb, bb = qTc[b], kTc[b], vaug[b], biasb[b]
        sl = slopes[8 + h]
        sT = selT[h % 2]

        def prep(h=h, b=b, hp=hp, qc=qc, kc_=kc_, vb=vb, bb=bb, sl=sl, sT=sT):
            c.dma("sp", qc[hp:hp + 64, :], P.qT_d[768 + h * 64:768 + (h + 1) * 64, :], reads=[P.QT], writes=[qc], sem=s_a[b])
            c.dma("sp", kc_[hp:hp + 64, :], P.kT_d[384 + h * 64:384 + (h + 1) * 64, :], reads=[P.KT], writes=[kc_], sem=s_a[2 + b])
            load_v(vb, 6 + h, 4 + b)
            c.op("dve", lambda e: e.tensor_scalar(out=bb[:, 0:512], in0=dist0[:], scalar1=-sl, scalar2=None, op0=ALU.mult), reads=[dist0], writes=[bb])
            c.op("dve", lambda e: e.tensor_reduce(out=kms[hp:hp + 64, :], in_=kc_[hp:hp + 64, :].rearrange("p (n t) -> p n t", t=256), axis=AX.X, op=ALU.add), reads=[kc_], writes=[kms])
            c.op("dve", lambda e: e.tensor_scalar(out=kmb[hp:hp + 64, :], in0=kms[hp:hp + 64, :], scalar1=1.0 / 256, scalar2=None, op0=ALU.mult), reads=[kms], writes=[kmb])
            for i in range(NT):
                own = i // 2
                gp = GG[i % 2]; w = gw[i % 2]
                gm = w[:, 0:NB]; sel = w[:, NB:2 * NB]; mv = w[:, 2 * NB:3 * NB]; m8 = w[:, 4 * NB:4 * NB + 8]
                c.op("pe", lambda e: e.matmul(gp[:, 0:NB], lhsT=qc[hp:hp + 64, i * 128:(i + 1) * 128], rhs=kmb[hp:hp + 64, :], start=True, stop=True), reads=[qc, kmb], writes=[gp])
                c.op("dve", lambda e: e.tensor_tensor(out=gm, in0=gp[:, 0:NB], in1=ownN[:, own, :], op=ALU.add), reads=[gp, ownN], writes=[w])
                c.op("dve", lambda e: e.max(out=m8, in_=gm), reads=[w], writes=[w])
                c.op("dve", lambda e: e.tensor_scalar(out=sel, in0=gm, scalar1=m8[:, 2:3], scalar2=None, op0=ALU.is_ge), reads=[w], writes=[w])
                c.op("dve", lambda e: e.tensor_tensor(out=sel, in0=sel, in1=vmask[:, own, :], op=ALU.mult), reads=[w, vmask], writes=[w])
                c.op("dve", lambda e: e.scalar_tensor_tensor(out=mv, in0=sel, scalar=BIG, in1=eqB[:, own, :], op0=ALU.mult, op1=ALU.add), reads=[w, eqB], writes=[w])
                c.op("pe", lambda e: e.transpose(gp[0:NB, 128:256], mv, ident[:]), reads=[w, ident], writes=[gp])
                c.op("act", lambda e: e.copy(out=sT[:, i * 128:(i + 1) * 128], in_=gp[0:NB, 128:256]), reads=[gp], writes=[sT])
        seq.append(("prep", prep))
        rd = [qc, kc_, vb, bb, sT, esel, cmask]
        for qt in range(NG):
            nk = 4 * qt + 4
            oset = qtc[0] % 2; qtc[0] += 1
            for kt in range(nk):
                j = kt - 4 * qt
                attn_tile(qc[hp:hp + 64, qt * 512:(qt + 1) * 512], kc_[hp:hp + 64, kt * 128:(kt + 1) * 128],
                          (esel[:, kt // 2, :], sT[:, qt * 512:(qt + 1) * 512]),
                          bb[:, 0:512], cmask[:, j, :] if j >= 0 else None, float(-sl * (qt * 512 - kt * 128) - CSH),
                          vb[:, kt, :], kt == 0, kt == nk - 1, rd, oset, (qt, 768 + h * 64, None) if kt == nk - 1 else None, qlo=max(j, 0) * 128)
    LOOK = getattr(build_program, "look", 3)
    pend = []

    def flush(n):
        while len(pend) > n:
            it_ = pend.pop(0)
            it_[2](); it_[3]()
    for item in seq:
        if item[0] == "prep":
            flush(0)
            item[1]()
        else:
            item[1]()
            pend.append(item)
            flush(LOOK)
    flush(0)
    c.barrier()
    c.release(m2)
    if getattr(build_program, "mixer_stop", 0) == 2:
        c.release(mM)
        return

    m4 = c.mark()
    g1B = c.sbuf("g1B", [128, D], F32)
    c.dma("sp", g1B[:], P.mod_d[l:l + 1, 2 * D:3 * D].partition_broadcast(128), reads=[P.MODD], writes=[g1B], sem=s_const)
    wg = c.sbuf("wg", [128, 8, 3072], BF16)
    wbr = c.sbuf("wbr", [128, 8, D], BF16)
    wo = c.sbuf("wo", [128, 8, D], BF16)
    st3 = [c.sbuf(f"st3_{i}", [128, D], F32) for i in range(2)]
    pc = [0]

    def load_cast(dst_ap, src_ap):
        k_ = pc[0] % 2; pc[0] += 1
        st = st3[k_]
        c.dma("sp", st[:], src_ap, reads=[WCONST], writes=[st], sem=s_w[k_])
        return st

    for kc in range(8):
        for part in range(3):
            st = load_cast(None, P.w_in[l, kc * 128:(kc + 1) * 128, 2308 + part * 1024:2308 + (part + 1) * 1024])
            c.op("act", lambda e: e.copy(out=wg[:, kc, part * 1024:(part + 1) * 1024], in_=st[:]), reads=[st], writes=[wg])
        srcbr = (P.w_br_fox[l, kc * 128:(kc + 1) * 128, :] if kc < 2 else
                 P.w_br_swa[l, (kc - 2) * 128:(kc - 1) * 128, :] if kc < 6 else P.w_br_moba[l, (kc - 6) * 128:(kc - 5) * 128, :])
        st = load_cast(None, srcbr)
        c.op("dve", lambda e: e.tensor_copy(out=wbr[:, kc, :], in_=st[:]), reads=[st], writes=[wbr])
        st = load_cast(None, P.w_out[l, kc * 128:(kc + 1) * 128, :])
        c.op("dve", lambda e: e.tensor_copy(out=wo[:, kc, :], in_=st[:]), reads=[st], writes=[wo])
    hTg = c.sbuf("hTg3", [128, 8, 512], BF16)
    oT = c.sbuf("oT", [128, 8, 512], BF16)
    mT = c.sbuf("mT", [128, 8, 512], BF16)
    sg = [c.sbuf(f"sg{i}", [128, 512], F32) for i in range(2)]
    macc = [c.sbuf(f"macc{i}", [128, 512], F32) for i in range(2)]
    xt = [c.sbuf(f"xt{i}", [128, D], F32) for i in range(2)]
    xo = [c.sbuf(f"xo{i}", [128, D], F32) for i in range(2)]
    PT = c.psum("PT", [128, 512], F32)
    PG = [c.psum(f"PG{i}", [128, 512], F32) for i in range(2)]
    PY = [c.psum(f"PY{i}", [128, 512], F32) for i in range(2)]
    PO = c.psum("PO", [128, D], F32)
    br_k = [(0, 2), (2, 6), (6, 8)]
    for q in range(NG):
        c.dma("sp", hTg[:], hT_v[:, :, q * 512:(q + 1) * 512], reads=[P.HT], writes=[hTg], sem=s_a[0])
        ptb = PT[:].bitcast(BF16)
        for cc in range(8):
            for sub in range(4):
                c.op("pe", lambda e: e.transpose(ptb[:, sub * 128:(sub + 1) * 128], o_sb[:, q * 4 + sub, cc * 128:(cc + 1) * 128], identb[:]),
                     reads=[o_sb, identb], writes=[PT], signal=(sub == 3))
            c.op("act", lambda e: e.copy(out=oT[:, cc, :], in_=ptb[:, 0:512]), reads=[PT], writes=[oT])
        for jc in range(8):
            ma = macc[jc % 2]
            for br in range(3):
                pg_ = PG[(jc * 3 + br) % 2]; py_ = PY[(jc * 3 + br) % 2]; sgt = sg[(jc * 3 + br) % 2]
                gcol = br * 1024 + jc * 128
                for kc in range(8):
                    c.op("pe", lambda e: e.matmul(pg_[:], lhsT=wg[:, kc, gcol:gcol + 128], rhs=hTg[:, kc, :], start=(kc == 0), stop=(kc == 7)),
                         reads=[wg, hTg], writes=[pg_], signal=(kc == 7))
                k0, k1 = br_k[br]
                for kc in range(k0, k1):
                    c.op("pe", lambda e: e.matmul(py_[:], lhsT=wbr[:, kc, jc * 128:(jc + 1) * 128], rhs=oT[:, kc, :], start=(kc == k0), stop=(kc == k1 - 1)),
                         reads=[wbr, oT], writes=[py_], signal=(kc == k1 - 1))
                c.op("act", lambda e: e.activation(out=sgt[:], in_=pg_[:], func=AF.Sigmoid), reads=[pg_], writes=[sgt])
                if br == 0:
                    c.op("dve", lambda e: e.tensor_tensor(out=ma[:], in0=py_[:], in1=sgt[:], op=ALU.mult), reads=[py_, sgt], writes=[ma])
                else:
                    c.op("dve", lambda e: e.tensor_tensor(out=sgt[:], in0=py_[:], in1=sgt[:], op=ALU.mult), reads=[py_, sgt], writes=[sgt])
                    if br == 1:
                        c.op("dve", lambda e: e.tensor_tensor(out=ma[:], in0=ma[:], in1=sgt[:], op=ALU.add), reads=[ma, sgt], writes=[ma])
                    else:
                        c.op("dve", lambda e: e.tensor_tensor(out=mT[:, jc, :], in0=ma[:], in1=sgt[:], op=ALU.add), reads=[ma, sgt], writes=[mT])
        for sub in range(4):
            i = q * 4 + sub
            r0 = i * 128
            xb = xt[i % 2]; ob = xo[i % 2]
            c.dma("sp", xb[:], x_src[r0:r0 + 128, :], reads=[X_SRC], writes=[xb], sem=s_x[i % 2])
            for half in range(2):
                for kc in range(8):
                    c.op("pe", lambda e: e.matmul(PO[:, half * 512:(half + 1) * 512], lhsT=mT[:, kc, sub * 128:(sub + 1) * 128], rhs=wo[:, kc, half * 512:(half + 1) * 512],
                                                  start=(kc == 0), stop=(kc == 7)), reads=[mT, wo], writes=[PO], signal=(kc == 7))
            c.op("dve", lambda e: e.tensor_tensor(out=ob[:], in0=PO[:], in1=g1B[:], op=ALU.mult), reads=[PO, g1B], writes=[ob])
            c.op("dve", lambda e: e.tensor_tensor(out=ob[:], in0=ob[:], in1=xb[:], op=ALU.add), reads=[ob, xb], writes=[ob])
            c.dma("sp", P.xres_d[r0:r0 + 128, :], ob[:], reads=[ob], writes=[P.XRES], sem=s_st[i % 2])
    c.barrier()
    c.release(mM)


def moe_layer(P):
    c, nc, l, S, NE = P.c, P.nc, P.l, P.S, P.NE
    GT = min(2048, S)
    NGRP = S // GT
    NTG = GT // 128
    NQ = GT // 512
    ident, modc, a2c = P.ident, P.modc, P.a2c
    s_const, s_x, s_w, s_st = P.s_const, P.s_x, P.s_w, P.s_st
    WCONST = P.WCONST
    x_src, X_SRC = P.x_src, P.X_SRC
    if not (P.skip_mixer and l == 0):
        x_src, X_SRC = P.xres_d, P.XRES
    x_dst, X_DST = (P.y_out, P.YOUT) if P.last else (P.xres_d, P.XRES)

    mL = c.mark()
    g2B = c.sbuf("g2B", [128, D], F32)
    c.dma("sp", g2B[:], P.mod_d[l:l + 1, 5 * D:6 * D].partition_broadcast(128), reads=[P.MODD], writes=[g2B], sem=s_const)
    yacc = c.sbuf("yacc", [128, NTG, D], F32)
    h2T = c.sbuf("h2T", [128, 8, GT], BF16)
    gates = c.sbuf("gates", [128, NTG, NE], F32)
    b1T = c.sbuf("b1T", [128, NE, 16], F32)
    b2s = c.sbuf("b2s", [NE, D], F32)
    brB = c.sbuf("brB", [128, NE], F32)
    wr = c.sbuf("wr", [128, 8, NE], F32)

    m1 = c.mark()
    b1tm = c.sbuf("b1tm", [NE, 2 * D], F32)
    pt = c.psum("pt_b1", [128, 512], F32)
    c.dma("sp", b1tm[:], P.b_exp1[l], reads=[WCONST], writes=[b1tm], sem=s_const)
    c.dma("sp", b2s[:], P.b_exp2[l], reads=[WCONST], writes=[b2s], sem=s_const)
    c.dma("sp", brB[:], P.b_router[l:l + 1, :].partition_broadcast(128), reads=[WCONST], writes=[brB], sem=s_const)
    with nc.allow_non_contiguous_dma("router weights relayout"):
        c.dma("sp", wr[:], P.w_router[l].rearrange("(kc p) e -> p kc e", p=128), reads=[WCONST], writes=[wr], sem=s_const)
    for t in range(2):
        for fc in range(8):
            src = b1tm[:, fc * 256: (fc + 1) * 256].rearrange("e (p t) -> e t p", t=2)[:, t, :]
            c.op("pe", lambda e: e.transpose(pt[:, 0:NE], src, ident[0:NE, 0:NE]), reads=[b1tm, ident], writes=[pt])
            if t == 0:
                c.op("dve", lambda e: e.tensor_copy(out=b1T[:, :, t * 8 + fc], in_=pt[:, 0:NE]), reads=[pt], writes=[b1T])
            else:
                c.op("dve", lambda e: e.tensor_scalar(out=b1T[:, :, t * 8 + fc], in0=pt[:, 0:NE], scalar1=1.0, scalar2=None, op0=ALU.add), reads=[pt], writes=[b1T])
    c.barrier()
    c.release(m1)

    for g in range(NGRP):
        tok0 = g * GT
        m2 = c.mark()
        xs = [c.sbuf(f"xs{i}", [128, D], F32) for i in range(2)]
        junk = c.sbuf("junk", [128, D], F32)
        ss = [c.sbuf(f"ss{i}", [128, 4], F32) for i in range(2)]
        xn = c.sbuf("xn", [128, 4, D], F32)
        h32 = c.sbuf("h32", [128, 8, 512], F32)
        sm = [c.sbuf(f"sm{i}", [128, 4 * NE + 16], F32) for i in range(2)]
        gTs = c.sbuf("gTs", [NE, 128], F32)
        ptr = [c.psum(f"ptr{i}", [128, 512], F32) for i in range(2)]
        pr = [c.psum(f"pr{i}", [128, 512], F32) for i in range(2)]
        pb = c.psum("pb", [128, D], F32)
        for q in range(NQ):
            for sub in range(4):
                i = q * 4 + sub
                r0 = tok0 + i * 128
                xb = xs[i % 2]; sb = ss[i % 2]
                c.dma("sp", xb[:], x_src[r0:r0 + 128, :], reads=[X_SRC], writes=[xb], sem=s_x[i % 2])
                c.op("act", lambda e: e.activation(out=junk[:], in_=xb[:], func=AF.Square, accum_out=sb[:, 0:1]),
                     reads=[xb], writes=[junk, sb])
                c.op("act", lambda e: e.activation(out=sb[:, 1:2], in_=sb[:, 0:1], func=AF.Sqrt, scale=1.0 / D, bias=EPS),
                     reads=[sb], writes=[sb])
                c.op("dve", lambda e: e.reciprocal(out=sb[:, 2:3], in_=sb[:, 1:2]), reads=[sb], writes=[sb])
                c.op("dve", lambda e: e.tensor_scalar(out=xn[:, sub, :], in0=xb[:], scalar1=sb[:, 2:3], scalar2=None, op0=ALU.mult),
                     reads=[xb, sb], writes=[xn])
            for kc in range(8):
                pp = ptr[kc % 2]
                for sub in range(4):
                    c.op("pe", lambda e: e.transpose(pp[:, sub * 128:(sub + 1) * 128], xn[:, sub, kc * 128:(kc + 1) * 128], ident[:]),
                         reads=[xn, ident], writes=[pp], signal=(sub == 3))
                c.op("act", lambda e: e.activation(out=h32[:, kc, :], in_=pp[:], func=AF.Identity,
                                                   scale=a2c[:, kc:kc + 1], bias=modc[:, l, 24 + kc:25 + kc]),
                     reads=[pp, a2c, modc], writes=[h32])
                c.op("dve", lambda e: e.tensor_copy(out=h2T[:, kc, q * 512:(q + 1) * 512], in_=h32[:, kc, :]),
                     reads=[h32], writes=[h2T])
            for sub in range(4):
                ti = q * 4 + sub
                pq = pr[sub % 2]; w = sm[sub % 2]
                for kc in range(8):
                    c.op("pe", lambda e: e.matmul(pq[:, 0:NE], lhsT=h32[:, kc, sub * 128:(sub + 1) * 128], rhs=wr[:, kc, :],
                                                  start=(kc == 0), stop=(kc == 7)), reads=[h32, wr], writes=[pq], signal=(kc == 7))
                lg = w[:, 0:NE]; mk = w[:, NE:2 * NE]; ex = w[:, 2 * NE:3 * NE]; em = w[:, 3 * NE:4 * NE]
                m8 = w[:, 4 * NE:4 * NE + 8]; nm = w[:, 4 * NE + 8:4 * NE + 9]; sv = w[:, 4 * NE + 9:4 * NE + 10]
                rs = w[:, 4 * NE + 10:4 * NE + 11]
                c.op("dve", lambda e: e.tensor_tensor(out=lg, in0=pq[:, 0:NE], in1=brB[:], op=ALU.add), reads=[pq, brB], writes=[w])
                c.op("dve", lambda e: e.max(out=m8, in_=lg), reads=[w], writes=[w])
                c.op("dve", lambda e: e.tensor_scalar(out=mk, in0=lg, scalar1=m8[:, 3:4], scalar2=None, op0=ALU.is_ge), reads=[w], writes=[w])
                c.op("dve", lambda e: e.tensor_scalar(out=nm, in0=m8[:, 0:1], scalar1=-1.0, scalar2=None, op0=ALU.mult), reads=[w], writes=[w])
                c.op("act", lambda e: e.activation(out=ex, in_=lg, func=AF.Exp, bias=nm), reads=[w], writes=[w])
                c.op("dve", lambda e: e.scalar_tensor_tensor(out=em, in0=ex, scalar=1.0, in1=mk, op0=ALU.mult, op1=ALU.mult, accum_out=sv),
                     reads=[w], writes=[w])
                c.op("dve", lambda e: e.reciprocal(out=rs, in_=sv), reads=[w], writes=[w])
                c.op("dve", lambda e: e.tensor_scalar(out=gates[:, ti, :], in0=em, scalar1=rs, scalar2=None, op0=ALU.mult),
                     reads=[w], writes=[gates])
                c.op("pe", lambda e: e.transpose(pq[0:NE, 128:256], gates[:, ti, :], ident[:]), reads=[gates, ident], writes=[pq])
                c.op("act", lambda e: e.copy(out=gTs[:], in_=pq[0:NE, 128:256]), reads=[pq], writes=[gTs])
                for half in range(2):
                    c.op("pe", lambda e: e.matmul(pb[:, half * 512:(half + 1) * 512], lhsT=gTs[:], rhs=b2s[:, half * 512:(half + 1) * 512],
                                                  start=True, stop=True), reads=[gTs, b2s], writes=[pb])
                c.op("act", lambda e: e.copy(out=yacc[:, ti, :], in_=pb[:]), reads=[pb], writes=[yacc])
        c.barrier()
        c.release(m2)

        m3 = c.mark()
        w1b = [c.sbuf(f"w1b{i}", [128, 8, 2, 512], BF16) for i in range(2)]
        w2b = [c.sbuf(f"w2b{i}", [128, 4, D], BF16) for i in range(2)]
        stg = [c.sbuf(f"stg{i}", [128, 2, D], F32) for i in range(2)]
        abuf = [c.sbuf(f"abuf{i}", [128, 4, 512], BF16) for i in range(2)]
        t1 = [c.sbuf(f"t1_{i}", [128, 512], F32) for i in range(2)]
        t2 = [c.sbuf(f"t2_{i}", [128, 512], F32) for i in range(2)]
        t3 = [c.sbuf(f"t3_{i}", [128, 512], F32) for i in range(2)]
        pg = [c.psum(f"pg{i}", [128, 512], F32) for i in range(2)]
        pl = [c.psum(f"pl{i}", [128, 512], F32) for i in range(2)]
        py = [c.psum(f"py{i}", [128, D], F32) for i in range(2)]
        units = [(e, hf) for e in range(NE) for hf in range(2)]
        pcount = [0]

        def load_piece(u, pi):
            e, hf = units[u]
            bi = u % 2
            si = pcount[0] % 2
            pcount[0] += 1
            st = stg[si]
            if pi < 4:
                kc0 = pi * 2
                src = P.w_exp1[l, e, kc0 * 128:(kc0 + 2) * 128, hf * 1024:(hf + 1) * 1024].rearrange("(kc p) n -> p kc n", p=128)
                c.dma("sp", st[:], src, reads=[WCONST], writes=[st], sem=s_w[si])
                c.op("act", lambda en: en.copy(out=w1b[bi][:, kc0:kc0 + 2, :, :],
                                               in_=st[:].rearrange("p kc (f t) -> p kc t f", t=2)),
                     reads=[st], writes=[w1b[bi]])
            else:
                fc0 = (pi - 4) * 2
                r0 = (hf * 4 + fc0) * 128
                src = P.w_exp2[l, e, r0:r0 + 256, :].rearrange("(fc p) n -> p fc n", p=128)
                c.dma("sp", st[:], src, reads=[WCONST], writes=[st], sem=s_w[si])
                c.op("act", lambda en: en.copy(out=w2b[bi][:, fc0:fc0 + 2, :], in_=st[:]), reads=[st], writes=[w2b[bi]])

        for pi in range(6):
            load_piece(0, pi)
        its = [(u, q) for u in range(len(units)) for q in range(NQ)]

        def stage_w1(it):
            u, q = its[it]
            e, hf = units[u]
            bi = u % 2
            ab = abuf[it % 2]
            for fc in range(4):
                bg = pg[fc % 2]; bl = pl[fc % 2]
                a1_, a2_, a3_ = t1[fc % 2], t2[fc % 2], t3[fc % 2]
                for kc in range(8):
                    c.op("pe", lambda en: en.matmul(bg[:], lhsT=w1b[bi][:, kc, 0, fc * 128:(fc + 1) * 128],
                                                    rhs=h2T[:, kc, q * 512:(q + 1) * 512], start=(kc == 0), stop=(kc == 7)),
                         reads=[w1b[bi], h2T], writes=[bg], signal=(kc == 7))
                for kc in range(8):
                    c.op("pe", lambda en: en.matmul(bl[:], lhsT=w1b[bi][:, kc, 1, fc * 128:(fc + 1) * 128],
                                                    rhs=h2T[:, kc, q * 512:(q + 1) * 512], start=(kc == 0), stop=(kc == 7)),
                         reads=[w1b[bi], h2T], writes=[bl], signal=(kc == 7))
                bcol = hf * 4 + fc
                c.op("dve", lambda en: en.tensor_scalar(out=a1_[:], in0=bg[:], scalar1=b1T[:, e, bcol:bcol + 1], scalar2=7.0,
                                                        op0=ALU.add, op1=ALU.min), reads=[bg, b1T], writes=[a1_])
                c.op("act", lambda en: en.activation(out=a2_[:], in_=a1_[:], func=AF.Sigmoid, scale=1.702), reads=[a1_], writes=[a2_])
                c.op("dve", lambda en: en.tensor_scalar(out=a3_[:], in0=bl[:], scalar1=b1T[:, e, 8 + bcol:9 + bcol], scalar2=8.0,
                                                        op0=ALU.add, op1=ALU.min), reads=[bl, b1T], writes=[a3_])
                c.op("dve", lambda en: en.tensor_tensor(out=a1_[:], in0=a1_[:], in1=a2_[:], op=ALU.mult), reads=[a1_, a2_], writes=[a1_])
                c.op("dve", lambda en: en.scalar_tensor_tensor(out=ab[:, fc, :], in0=a3_[:], scalar=-6.0, in1=a1_[:], op0=ALU.max, op1=ALU.mult),
                     reads=[a1_, a3_], writes=[ab])

        def stage_w2(it):
            u, q = its[it]
            e, hf = units[u]
            bi = u % 2
            ab = abuf[it % 2]
            for sub in range(4):
                ti = q * 4 + sub
                yy = py[sub % 2]
                for half in range(2):
                    for fc in range(4):
                        c.op("pe", lambda en: en.matmul(yy[:, half * 512:(half + 1) * 512], lhsT=ab[:, fc, sub * 128:(sub + 1) * 128],
                                                        rhs=w2b[bi][:, fc, half * 512:(half + 1) * 512], start=(fc == 0), stop=(fc == 3)),
                             reads=[ab, w2b[bi]], writes=[yy], signal=(fc == 3 and half == 1))
                c.op("dve", lambda en: en.scalar_tensor_tensor(out=yacc[:, ti, :], in0=yy[:], scalar=gates[:, ti, e:e + 1], in1=yacc[:, ti, :],
                                                               op0=ALU.mult, op1=ALU.add), reads=[yy, gates, yacc], writes=[yacc])

        stage_w1(0)
        for it, (u, q) in enumerate(its):
            if u + 1 < len(units):
                lo, hi = (q * 6) // NQ, ((q + 1) * 6) // NQ
                for pi in range(lo, hi):
                    load_piece(u + 1, pi)
            if it + 1 < len(its):
                stage_w1(it + 1)
            stage_w2(it)
        c.barrier()
        c.release(m3)

        m4 = c.mark()
        xt = [c.sbuf(f"xt{i}", [128, D], F32) for i in range(2)]
        xo = [c.sbuf(f"xo{i}", [128, D], F32) for i in range(2)]
        for ti in range(NTG):
            r0 = tok0 + ti * 128
            xb = xt[ti % 2]; ob = xo[ti % 2]
            c.dma("sp", xb[:], x_src[r0:r0 + 128, :], reads=[X_SRC], writes=[xb], sem=s_x[ti % 2])
            c.op("dve", lambda en: en.tensor_tensor(out=ob[:], in0=yacc[:, ti, :], in1=g2B[:], op=ALU.mult), reads=[yacc, g2B], writes=[ob])
            c.op("dve", lambda en: en.tensor_tensor(out=ob[:], in0=ob[:], in1=xb[:], op=ALU.add), reads=[ob, xb], writes=[ob])
            c.dma("sp", x_dst[r0:r0 + 128, :], ob[:], reads=[ob], writes=[X_DST], sem=s_st[ti % 2])
        c.barrier()
        c.release(m4)
    c.release(mL)


def alibi_slopes():
    return [2.0 ** (-8.0 * i / 12.0) for i in range(1, 13)]


def build_program(S=4096, L=4, NE=32, dbg=False):
    NT = S // 128
    NG = S // 512
    NB = S // 256
    nc = bass.Bass("TRN2", target_bir_lowering=False)
    c = Ctx(nc)

    def din(name, shape):
        return nc.dram_tensor(name, shape, F32, kind="ExternalInput").ap()

    x_in = din("x", [S, D]); c_in = din("c", [1, D])
    w_ada = din("w_ada", [L, D, 6 * D]); b_ada = din("b_ada", [L, 6 * D])
    norm_gain = din("norm_gain", [L, 2, D]); w_in = din("w_in", [L, D, 5380])
    b_fgate = din("b_fgate", [L, 4]); qk_gain = din("qk_gain", [L, 6, 64])
    attn_sinks = din("attn_sinks", [L, 8])
    w_br_fox = din("w_br_fox", [L, 256, D]); w_br_swa = din("w_br_swa", [L, 512, D])
    w_br_moba = din("w_br_moba", [L, 256, D]); w_out = din("w_out", [L, D, D])
    w_router = din("w_router", [L, D, NE]); b_router = din("b_router", [L, NE])
    w_exp1 = din("w_exp1", [L, NE, D, 2 * D]); b_exp1 = din("b_exp1", [L, NE, 2 * D])
    w_exp2 = din("w_exp2", [L, NE, D, D]); b_exp2 = din("b_exp2", [L, NE, D])
    y_out = nc.dram_tensor("y", [S, D], F32, kind="ExternalOutput").ap()

    def dscr(name, shape, dt):
        kind = "ExternalOutput" if dbg else "Internal"
        return nc.dram_tensor(name, shape, dt, kind=kind).ap()

    xres_d = dscr("xres_d", [S, D], F32)
    hT_d = dscr("hT_d", [D, S], BF16)
    qT_d = dscr("qT_d", [1024, S], BF16)
    kT_d = dscr("kT_d", [640, S], BF16)
    v_d = dscr("v_d", [S, 650], BF16)
    cumT_d = dscr("cumT_d", [4, S], F32)
    mod_d = dscr("mod_d", [L, 6 * D], F32)

    X_IN = Obj("x_in"); XRES = Obj("xres"); HT = Obj("hT"); QT = Obj("qT"); KT = Obj("kT")
    VD = Obj("vd"); CUMT = Obj("cumT"); MODD = Obj("modd"); YOUT = Obj("yout"); WCONST = Obj("w")

    s_const = c.new_sem(16, "s_const")
    s_x = [c.new_sem(16, f"s_x{i}") for i in range(2)]
    s_w = [c.new_sem(16, f"s_w{i}") for i in range(4)]
    s_st = [c.new_sem(16, f"s_st{i}") for i in range(4)]
    s_a = [c.new_sem(16, f"s_a{i}") for i in range(6)]

    slopes = alibi_slopes()

    ident = c.sbuf("ident", [128, 128], F32)
    identb = c.sbuf("identb", [128, 128], BF16)
    ones_f = c.sbuf("ones_f", [128, 128], F32)
    c.op("pool", lambda e: e.memset(ident[:], 0.0), writes=[ident])
    c.op("pool", lambda e: e.affine_select(out=ident[:], in_=ident[:], pattern=[[-1, 128]],
                                           compare_op=ALU.not_equal, fill=1.0, base=0,
                                           channel_multiplier=1), reads=[ident], writes=[ident])
    c.op("dve", lambda e: e.tensor_copy(out=identb[:], in_=ident[:]), reads=[ident], writes=[identb])
    c.op("pool", lambda e: e.memset(ones_f[:], 1.0), writes=[ones_f])
    modc = c.sbuf("modc", [128, L, 48], F32)
    a1c = c.sbuf("a1c", [128, 8], F32); a2c = c.sbuf("a2c", [128, 8], F32)
    ngc = c.sbuf("ngc", [128, 2, 8], F32)

    m0 = c.mark()
    condc = c.sbuf("condc", [128, 8], F32)
    ccol = c.sbuf("ccol", [128, 8], F32)
    with nc.allow_non_contiguous_dma("small vector relayout"):
        c.dma("sp", ccol[:], c_in.rearrange("o (k p) -> p (o k)", p=128), reads=[WCONST], writes=[ccol], sem=s_const)
    c.op("act", lambda e: e.activation(out=condc[:], in_=ccol[:], func=AF.Silu), reads=[ccol], writes=[condc])
    wa = [c.sbuf(f"wa{i}", [128, 6 * D], F32) for i in range(2)]
    modrow = c.sbuf("modrow", [1, 6 * D], F32)
    brow = c.sbuf("brow", [1, 6 * D], F32)
    pm = [c.psum(f"pm{i}", [128, 512], F32) for i in range(4)]
    for l in range(L):
        c.dma("sp", brow[:], b_ada[l:l + 1, :], reads=[WCONST], writes=[brow], sem=s_const)
        for half in range(3):
            for kc in range(8):
                wt = wa[kc % 2]
                c.dma("sp", wt[:, half * 2048:(half + 1) * 2048],
                      w_ada[l, kc * 128:(kc + 1) * 128, half * 2048:(half + 1) * 2048],
                      reads=[WCONST], writes=[wt], sem=s_w[kc % 2])
                for n in range(4):
                    c.op("pe", lambda e: e.matmul(pm[n][0:1, :], lhsT=condc[:, kc:kc + 1],
                                                  rhs=wt[:, half * 2048 + n * 512: half * 2048 + (n + 1) * 512],
                                                  start=(kc == 0), stop=(kc == 7)),
                         reads=[condc, wt], writes=[pm[n]], signal=(kc == 7 or n == 3))
            for n in range(4):
                cs = half * 2048 + n * 512
                c.op("dve", lambda e: e.tensor_tensor(out=modrow[:, cs:cs + 512], in0=pm[n][0:1, :],
                                                      in1=brow[:, cs:cs + 512], op=ALU.add),
                     reads=[pm[n], brow], writes=[modrow])
        c.dma("sp", mod_d[l:l + 1, :], modrow[:], reads=[modrow], writes=[MODD], sem=s_st[0])
    with nc.allow_non_contiguous_dma("small vector relayout"):
        for l in range(L):
            c.dma("sp", modc[:, l, :], mod_d[l:l + 1, :].rearrange("o (j p) -> p (o j)", p=128),
                  reads=[MODD], writes=[modc], sem=s_const)
    c.barrier()
    c.release(m0)

    CD, CONSTD = attn_consts(c, nc, NB, s_const, dscr, ident)
    for l in range(L):
        last = (l == L - 1)
        x_src, X_SRC = (x_in, X_IN) if l == 0 else (xres_d, XRES)

        with nc.allow_non_contiguous_dma("small vector relayout"):
            c.dma("sp", ngc[:], norm_gain[l].rearrange("t (j p) -> p t j", p=128), reads=[WCONST], writes=[ngc], sem=s_const)
        c.op("dve", lambda e: e.scalar_tensor_tensor(out=a1c[:], in0=modc[:, l, 8:16], scalar=1.0, in1=ngc[:, 0, :],
                                                     op0=ALU.add, op1=ALU.mult), reads=[modc, ngc], writes=[a1c])
        c.op("dve", lambda e: e.scalar_tensor_tensor(out=a2c[:], in0=modc[:, l, 32:40], scalar=1.0, in1=ngc[:, 1, :],
                                                     op0=ALU.add, op1=ALU.mult), reads=[modc, ngc], writes=[a2c])

        skip_mixer = getattr(build_program, 'skip_mixer', False)
        skip_moe = getattr(build_program, 'skip_moe', False)
        P = SimpleNamespace(**locals())
        if not skip_mixer:
            mixer_layer(P)
        if not skip_moe:
            moe_layer(P)

    sp = c.E["sp"]
    c._collect(sp, [YOUT], ())
    c.close()
    return nc


_NAMES = ["w_ada", "b_ada", "norm_gain", "w_in", "b_fgate", "qk_gain", "attn_sinks", "w_br_fox", "w_br_swa",
          "w_br_moba", "w_out", "w_router", "b_router", "w_exp1", "b_exp1", "w_exp2", "b_exp2"]


def kernel(**inputs):
    x = np.ascontiguousarray(np.asarray(inputs["x"], dtype=np.float32))
    cvec = np.ascontiguousarray(np.asarray(inputs["c"], dtype=np.float32))
    B, S, _ = x.shape
    L = int(np.asarray(inputs["w_ada"]).shape[0])
    NE = int(np.asarray(inputs["w_router"]).shape[2])
    shared = {k: np.ascontiguousarray(np.asarray(inputs[k], dtype=np.float32)) for k in _NAMES}
    nc = build_program(S=S, L=L, NE=NE)
    in_maps = []
    for b in range(B):
        m = {"x": x[b], "c": cvec[b:b + 1]}
        m.update(shared)
        in_maps.append(m)
    res = run_bass_kernel_spmd(nc, in_maps, core_ids=list(range(B)))
    return np.stack([np.asarray(r["y"], dtype=np.float32) for r in res.results], axis=0)
```

```python
import numpy as np
from types import SimpleNamespace
import concourse.bass as bass
import concourse.mybir as mybir
from concourse.bass_utils import run_bass_kernel_spmd

F32 = mybir.dt.float32
BF16 = mybir.dt.bfloat16
AF = mybir.ActivationFunctionType
ALU = mybir.AluOpType
AX = mybir.AxisListType

D = 1024
NEG = -1.0e30
BIG = 30000.0
CSH = 8.0
EPS = 1e-6


class Sem:
    __slots__ = ("h", "issued", "step")

    def __init__(self, h, step):
        self.h = h
        self.issued = 0
        self.step = step


class Obj:
    __slots__ = ("name", "w", "r", "dsem", "t")

    def __init__(self, name, t=None):
        self.name = name
        self.w = []
        self.r = []
        self.dsem = None
        self.t = t

    def __getitem__(self, k):
        return self.t[k]


class Eng:
    __slots__ = ("name", "h", "sem", "seen")

    def __init__(self, name, h, sem):
        self.name = name
        self.h = h
        self.sem = sem
        self.seen = {}


class Ctx:
    def __init__(self, nc):
        self.nc = nc
        self._stack = []
        self.sems = []
        self.E = {}
        for name, h in (("pe", nc.tensor), ("dve", nc.vector), ("act", nc.scalar),
                        ("pool", nc.gpsimd), ("sp", nc.sync)):
            self.E[name] = Eng(name, h, self.new_sem(1, "e_" + name))
        self.n_wait = 0
        self.n_ins = 0

    def new_sem(self, step, name=None):
        cm = self.nc.semaphore(name)
        h = cm.__enter__()
        s = Sem(h, step)
        self.sems.append(s)
        return s

    def mark(self):
        return len(self._stack)

    def release(self, mark):
        while len(self._stack) > mark:
            self._stack.pop().__exit__(None, None, None)

    def sbuf(self, name, shape, dt, dsem=None):
        self.n_ins += 0
        self._uid = getattr(self, "_uid", 0) + 1
        name = f"{name}_u{self._uid}"
        cm = self.nc.sbuf_tensor(name, shape, dt)
        t = cm.__enter__()
        self._stack.append(cm)
        o = Obj(name, t)
        o.dsem = dsem
        return o

    def psum(self, name, shape, dt):
        self._uid = getattr(self, "_uid", 0) + 1
        name = f"{name}_u{self._uid}"
        cm = self.nc.psum_tensor(name, shape, dt)
        t = cm.__enter__()
        self._stack.append(cm)
        return Obj(name, t)

    def _collect(self, eng, reads, writes):
        need = {}
        for o in reads:
            for (s, v) in o.w:
                if need.get(s, 0) < v:
                    need[s] = v
        for o in writes:
            for (s, v) in o.w:
                if need.get(s, 0) < v:
                    need[s] = v
            for (s, v) in o.r:
                if need.get(s, 0) < v:
                    need[s] = v
        for s, v in need.items():
            if s is eng.sem and eng.name == "pe":
                continue
            if s.step == 16:
                v = s.issued
            assert v <= s.issued, f"wait on unsignaled ticket eng={eng.name}"
            if eng.seen.get(s, 0) < v:
                eng.h.wait_ge(s.h, v)
                eng.seen[s] = v
                self.n_wait += 1

    def _record(self, tk, reads, writes):
        for o in writes:
            o.w = [tk]
            o.r = []
        for o in reads:
            if o not in writes:
                o.r = [t for t in o.r if t[0] is not tk[0]] + [tk]

    def op(self, en, fn, reads=(), writes=(), signal=True):
        eng = self.E[en]
        self._collect(eng, reads, writes)
        ins = fn(eng.h)
        self.n_ins += 1
        if signal:
            ins.then_inc(eng.sem.h, 1)
            eng.sem.issued += 1
            tk = (eng.sem, eng.sem.issued)
        else:
            tk = (eng.sem, eng.sem.issued + 1)
        self._record(tk, reads, writes)
        return ins

    def dma(self, qn, out, in_, reads=(), writes=(), sem=None, **kw):
        eng = self.E[qn]
        self._collect(eng, reads, writes)
        ins = eng.h.dma_start(out=out, in_=in_, **kw)
        ins.then_inc(sem.h, 16)
        sem.issued += 16
        self.n_ins += 1
        self._record((sem, sem.issued), reads, writes)
        return ins

    def barrier(self):
        for eng in self.E.values():
            for s in self.sems:
                if s is eng.sem:
                    continue
                if s.issued > 0 and eng.seen.get(s, 0) < s.issued:
                    eng.h.wait_ge(s.h, s.issued)
                    eng.seen[s] = s.issued

    def close(self):
        self.release(0)


def attn_consts(c, nc, NB, s_const, dscr, ident, S):
    m0 = c.mark()
    CONSTD = Obj("constd")
    CD = {}
    utri = c.sbuf("utri", [128, 128], F32)
    c.op("pool", lambda e: e.memset(utri[:], 1.0), writes=[utri])
    c.op("pool", lambda e: e.affine_select(out=utri[:], in_=utri[:], pattern=[[1, 128]], compare_op=ALU.is_ge, fill=0.0, base=0,
                                           channel_multiplier=-1), reads=[utri], writes=[utri])
    cmask = c.sbuf("cmask", [128, 4, 512], F32)
    c.op("pool", lambda e: e.memset(cmask[:], 0.0), writes=[cmask])
    for j in range(4):
        c.op("pool", lambda e: e.affine_select(out=cmask[:, j, :], in_=cmask[:, j, :], pattern=[[1, 512]], compare_op=ALU.is_ge, fill=NEG,
                                               base=-128 * j, channel_multiplier=-1), reads=[cmask], writes=[cmask])
    dist0 = c.sbuf("dist0", [128, 512], F32)
    c.op("pool", lambda e: e.iota(dist0[:], pattern=[[1, 512]], base=0, channel_multiplier=-1, allow_small_or_imprecise_dtypes=True), writes=[dist0])
    swM = c.sbuf("swM", [128, 5, 512], F32); swD = c.sbuf("swD", [128, 5, 512], F32)
    c.op("pool", lambda e: e.memset(swM[:], 0.0), writes=[swM])
    for jj in range(5):
        j = jj - 1
        c.op("pool", lambda e: e.affine_select(out=swM[:, jj, :], in_=swM[:, jj, :], pattern=[[1, 512]], compare_op=ALU.is_ge, fill=NEG,
                                               base=-128 * j, channel_multiplier=-1), reads=[swM], writes=[swM])
        c.op("pool", lambda e: e.affine_select(out=swM[:, jj, :], in_=swM[:, jj, :], pattern=[[-1, 512]], compare_op=ALU.is_ge, fill=NEG,
                                               base=127 + 128 * j, channel_multiplier=1), reads=[swM], writes=[swM])
        c.op("pool", lambda e: e.iota(swD[:, jj, :], pattern=[[1, 512]], base=-128 * j, channel_multiplier=-1, allow_small_or_imprecise_dtypes=True), writes=[swD])

    ownN = c.sbuf("ownN", [128, NB, NB], F32); vmask = c.sbuf("vmask", [128, NB, NB], F32); eqB = c.sbuf("eqB", [128, NB, NB], F32)
    eself = c.sbuf("eself", [NB, NB, 128], F32)
    c.op("pool", lambda e: e.memset(ownN[:], 0.0), writes=[ownN])
    c.op("pool", lambda e: e.memset(vmask[:], 1.0), writes=[vmask])
    c.op("pool", lambda e: e.memset(eqB[:], 0.0), writes=[eqB])
    c.op("pool", lambda e: e.memset(eself[:], 1.0), writes=[eself])
    for o_ in range(NB):
        pat = [[-1, NB]]
        c.op("pool", lambda e: e.affine_select(out=ownN[:, o_, :], in_=ownN[:, o_, :], pattern=pat, compare_op=ALU.is_ge, fill=NEG, base=o_ - 1, channel_multiplier=0), reads=[ownN], writes=[ownN])
        c.op("pool", lambda e: e.affine_select(out=eqB[:, o_, :], in_=eqB[:, o_, :], pattern=pat, compare_op=ALU.is_equal, fill=-BIG, base=o_, channel_multiplier=0), reads=[eqB], writes=[eqB])
    c.op("dve", lambda e: e.tensor_scalar(out=vmask[:], in0=ownN[:], scalar1=0.0, scalar2=None, op0=ALU.is_equal), reads=[ownN], writes=[vmask])
    for n_ in range(NB):
        c.op("dve", lambda e: e.tensor_scalar(out=eself[:, n_, :], in0=eself[:, n_, :], scalar1=ident[0:NB, n_:n_ + 1], scalar2=None, op0=ALU.mult),
             reads=[eself, ident], writes=[eself])

    for t_, nm_, shp in ((utri, "utri", [128, 128]), (cmask, "cmask", [128, 2048]), (dist0, "dist0", [128, 512]), (swM, "swM", [128, 2560]),
                         (swD, "swD", [128, 2560]), (ownN, "ownN", [128, NB * NB]), (vmask, "vmask", [128, NB * NB]), (eqB, "eqB", [128, NB * NB]),
                         (eself, "eself", [NB, NB * 128])):
        CD[nm_] = dscr("cd_" + nm_, shp, F32)
        src = t_[:] if len(t_[:].shape) == 2 else t_[:].rearrange("p a b -> p (a b)")
        c.dma("sp", CD[nm_], src, reads=[t_], writes=[CONSTD], sem=s_const)
    ek = c.sbuf("ek", [32, S], F32); ekb = c.sbuf("ekb", [32, S], BF16)
    c.op("pool", lambda e: e.memset(ek[:], 0.0), writes=[ek])
    c.op("pool", lambda e: e.memset(ek[0:NB, :], 1.0), reads=[ek], writes=[ek])
    c.op("pool", lambda e: e.affine_select(out=ek[0:NB, :], in_=ek[0:NB, :], pattern=[[1, S]], compare_op=ALU.is_ge, fill=0.0, base=0,
                                           channel_multiplier=-256), reads=[ek], writes=[ek])
    c.op("pool", lambda e: e.affine_select(out=ek[0:NB, :], in_=ek[0:NB, :], pattern=[[-1, S]], compare_op=ALU.is_ge, fill=0.0, base=255,
                                           channel_multiplier=256), reads=[ek], writes=[ek])
    c.op("dve", lambda e: e.tensor_copy(out=ekb[:], in_=ek[:]), reads=[ek], writes=[ekb])
    CD["eselk"] = dscr("cd_eselk", [32, S], BF16)
    c.dma("sp", CD["eselk"][16:16 + NB, :], ekb[0:NB, :], reads=[ekb], writes=[CONSTD], sem=s_const)
    c.dma("sp", CD["eselk"][0:16, :], ekb[16:32, :], reads=[ekb], writes=[CONSTD], sem=s_const)
    if NB < 16:
        c.dma("sp", CD["eselk"][16 + NB:32, :], ekb[16:32 - NB, :], reads=[ekb], writes=[CONSTD], sem=s_const)
    c.barrier()
    c.release(m0)
    return CD, CONSTD

def mixer_layer(P):
    c, nc, l, S = P.c, P.nc, P.l, P.S
    NT = S // 128; NG = S // 512; NB = S // 256
    ident, identb, ones_f, modc, a1c = P.ident, P.identb, P.ones_f, P.modc, P.a1c
    s_const, s_x, s_w, s_st, s_a = P.s_const, P.s_x, P.s_w, P.s_st, P.s_a
    WCONST = P.WCONST
    x_src, X_SRC = P.x_src, P.X_SRC
    slopes = P.slopes
    hT_v = P.hT_d.rearrange("(kc p) t -> p kc t", p=128)
    qT_v = P.qT_d.rearrange("(cc p) t -> p cc t", p=128)
    kT_v = P.kT_d.rearrange("(cc p) t -> p cc t", p=128)

    mM = c.mark()
    lf_all = c.sbuf("lf_all", [128, 4, NT], F32)
    m1 = c.mark()
    gQ = c.sbuf("gQ", [128, 1024], F32); gK = c.sbuf("gK", [128, 640], F32)
    bfB = c.sbuf("bfB", [128, 4], F32)
    qrow = [0] * 4 + [2] * 8 + [4] * 4
    krow = [1] * 4 + [3] * 2 + [5] * 4
    for h in range(16):
        c.dma("sp", gQ[:, h * 64:(h + 1) * 64], P.qk_gain[l, qrow[h]:qrow[h] + 1, :].partition_broadcast(128), reads=[WCONST], writes=[gQ], sem=s_const)
    for h in range(10):
        c.dma("sp", gK[:, h * 64:(h + 1) * 64], P.qk_gain[l, krow[h]:krow[h] + 1, :].partition_broadcast(128), reads=[WCONST], writes=[gK], sem=s_const)
    c.dma("sp", bfB[:], P.b_fgate[l:l + 1, :].partition_broadcast(128), reads=[WCONST], writes=[bfB], sem=s_const)
    c.op("dve", lambda e: e.tensor_scalar(out=gQ[:], in0=gQ[:], scalar1=0.125, scalar2=None, op0=ALU.mult), reads=[gQ], writes=[gQ])
    wqkv = c.sbuf("wqkv", [128, 8, 2308], BF16)
    wst = [c.sbuf(f"wst{i}", [128, 2308], F32) for i in range(2)]
    segs = [(0, 256, 0), (772, 1284, 256), (1540, 1796, 768),
            (256, 512, 1024), (1284, 1412, 1280), (1796, 2052, 1408),
            (512, 768, 1664), (1412, 1540, 1920), (2052, 2308, 2048),
            (768, 772, 2304)]
    for kc in range(8):
        st = wst[kc % 2]
        c.dma("sp", st[:], P.w_in[l, kc * 128:(kc + 1) * 128, 0:2308], reads=[WCONST], writes=[st], sem=s_w[kc % 2])
        for si, (a, b, d0) in enumerate(segs):
            en = "act" if si % 2 == 0 else "dve"
            if en == "act":
                c.op("act", lambda e: e.copy(out=wqkv[:, kc, d0:d0 + (b - a)], in_=st[:, a:b]), reads=[st], writes=[wqkv])
            else:
                c.op("dve", lambda e: e.tensor_copy(out=wqkv[:, kc, d0:d0 + (b - a)], in_=st[:, a:b]), reads=[st], writes=[wqkv])
    xs = [c.sbuf(f"xs{i}", [128, D], F32) for i in range(2)]
    junk = c.sbuf("junk", [128, D], F32)
    ss = [c.sbuf(f"ss{i}", [128, 4], F32) for i in range(2)]
    xn = c.sbuf("xn", [128, 4, D], F32)
    hTg = c.sbuf("hTg", [128, 8, 512], BF16)
    sqs = c.sbuf("sqs", [128, 1664], F32)
    rq = c.sbuf("rq", [128, 64], F32)
    qn = c.sbuf("qn", [128, 1024], BF16); kn = c.sbuf("kn", [128, 640], BF16)
    qTs = c.sbuf("qTs", [128, 8, 512], BF16); kTs = c.sbuf("kTs", [128, 5, 512], BF16)
    vst = c.sbuf("vst", [128, 4, 650], BF16)
    lft = c.sbuf("lft", [128, 8], F32)
    c.op("pool", lambda e: e.memset(vst[:], 1.0), writes=[vst])
    T0 = c.psum("T0", [128, 512], F32); T1 = c.psum("T1", [128, 512], F32)
    pq = c.psum("pq", [128, 1024], F32); pk = c.psum("pk", [128, 1024], F32); pv = c.psum("pv", [128, 1024], F32)
    TT = [T0, T1]
    for q in range(NG):
        for sub in range(4):
            i = q * 4 + sub
            r0 = i * 128
            xb = xs[i % 2]; sb = ss[i % 2]
            c.dma("sp", xb[:], x_src[r0:r0 + 128, :], reads=[X_SRC], writes=[xb], sem=s_x[i % 2])
            c.op("act", lambda e: e.activation(out=junk[:], in_=xb[:], func=AF.Square, accum_out=sb[:, 0:1]), reads=[xb], writes=[junk, sb])
            c.op("act", lambda e: e.activation(out=sb[:, 1:2], in_=sb[:, 0:1], func=AF.Sqrt, scale=1.0 / D, bias=EPS), reads=[sb], writes=[sb])
            c.op("dve", lambda e: e.reciprocal(out=sb[:, 2:3], in_=sb[:, 1:2]), reads=[sb], writes=[sb])
            c.op("dve", lambda e: e.tensor_scalar(out=xn[:, sub, :], in0=xb[:], scalar1=sb[:, 2:3], scalar2=None, op0=ALU.mult), reads=[xb, sb], writes=[xn])
        for kc in range(8):
            pp = TT[kc % 2]
            for sub in range(4):
                c.op("pe", lambda e: e.transpose(pp[:, sub * 128:(sub + 1) * 128], xn[:, sub, kc * 128:(kc + 1) * 128], ident[:]),
                     reads=[xn, ident], writes=[pp], signal=(sub == 3))
            c.op("act", lambda e: e.activation(out=hTg[:, kc, :], in_=pp[:], func=AF.Identity, scale=a1c[:, kc:kc + 1], bias=modc[:, l, kc:kc + 1]),
                 reads=[pp, a1c, modc], writes=[hTg])
        c.dma("sp", hT_v[:, :, q * 512:(q + 1) * 512], hTg[:], reads=[hTg], writes=[P.HT], sem=s_st[0])
        for sub in range(4):
            i = q * 4 + sub
            lhs = lambda kc: hTg[:, kc, sub * 128:(sub + 1) * 128]
            for (pt_, c0, n, w0) in ((pq, 0, 512, 0), (pq, 512, 512, 512), (pk, 0, 512, 1024), (pk, 512, 128, 1536),
                                     (pv, 0, 512, 1664), (pv, 512, 132, 2176)):
                for kc in range(8):
                    c.op("pe", lambda e: e.matmul(pt_[:, c0:c0 + n], lhsT=lhs(kc), rhs=wqkv[:, kc, w0:w0 + n], start=(kc == 0), stop=(kc == 7)),
                         reads=[hTg, wqkv], writes=[pt_], signal=(kc == 7))
            c.op("act", lambda e: e.activation(out=sqs[:, 0:1024], in_=pq[:], func=AF.Square), reads=[pq], writes=[sqs])
            c.op("act", lambda e: e.activation(out=sqs[:, 1024:1664], in_=pk[:, 0:640], func=AF.Square), reads=[pk], writes=[sqs])
            c.op("dve", lambda e: e.tensor_reduce(out=rq[:, 0:26], in_=sqs[:].rearrange("p (h d) -> p h d", d=64), axis=AX.X, op=ALU.add), reads=[sqs], writes=[rq])
            c.op("act", lambda e: e.activation(out=rq[:, 32:58], in_=rq[:, 0:26], func=AF.Sqrt, scale=1.0 / 64, bias=EPS), reads=[rq], writes=[rq])
            c.op("dve", lambda e: e.reciprocal(out=rq[:, 0:26], in_=rq[:, 32:58]), reads=[rq], writes=[rq])
            for h in range(16):
                c.op("dve", lambda e: e.scalar_tensor_tensor(out=qn[:, h * 64:(h + 1) * 64], in0=pq[:, h * 64:(h + 1) * 64], scalar=rq[:, h:h + 1],
                                                             in1=gQ[:, h * 64:(h + 1) * 64], op0=ALU.mult, op1=ALU.mult),
                     reads=[pq, rq, gQ], writes=[qn])
            for h in range(10):
                c.op("dve", lambda e: e.scalar_tensor_tensor(out=kn[:, h * 64:(h + 1) * 64], in0=pk[:, h * 64:(h + 1) * 64], scalar=rq[:, 16 + h:17 + h],
                                                             in1=gK[:, h * 64:(h + 1) * 64], op0=ALU.mult, op1=ALU.mult),
                     reads=[pk, rq, gK], writes=[kn])
            c.op("act", lambda e: e.copy(out=vst[:, sub, :].rearrange("p (h d) -> p h d", d=65)[:, :, 0:64],
                                         in_=pv[:, 0:640].rearrange("p (h d) -> p h d", d=64)), reads=[pv], writes=[vst])
            c.op("dve", lambda e: e.tensor_tensor(out=lft[:, 0:4], in0=pv[:, 640:644], in1=bfB[:], op=ALU.add), reads=[pv, bfB], writes=[lft])
            c.op("act", lambda e: e.activation(out=lft[:, 4:8], in_=lft[:, 0:4], func=AF.Exp, scale=-1.0), reads=[lft], writes=[lft])
            c.op("act", lambda e: e.activation(out=lft[:, 0:4], in_=lft[:, 4:8], func=AF.Ln, bias=1.0), reads=[lft], writes=[lft])
            c.op("dve", lambda e: e.tensor_scalar(out=lf_all[:, :, i], in0=lft[:, 0:4], scalar1=-1.0, scalar2=None, op0=ALU.mult), reads=[lft], writes=[lf_all])
            tq = T0[:].bitcast(BF16); tk = T1[:].bitcast(BF16)
            for cc in range(8):
                c.op("pe", lambda e: e.transpose(tq[:, cc * 128:(cc + 1) * 128], qn[:, cc * 128:(cc + 1) * 128], identb[:]),
                     reads=[qn, identb], writes=[T0], signal=(cc == 7))
            for cc in range(5):
                c.op("pe", lambda e: e.transpose(tk[:, cc * 128:(cc + 1) * 128], kn[:, cc * 128:(cc + 1) * 128], identb[:]),
                     reads=[kn, identb], writes=[T1], signal=(cc == 4))
            c.op("act", lambda e: e.copy(out=qTs[:, :, sub * 128:(sub + 1) * 128], in_=tq.rearrange("p (c t) -> p c t", t=128)), reads=[T0], writes=[qTs])
            c.op("dve", lambda e: e.tensor_copy(out=kTs[:, :, sub * 128:(sub + 1) * 128], in_=tk[:, 0:640].rearrange("p (c t) -> p c t", t=128)), reads=[T1], writes=[kTs])
        c.dma("sp", qT_v[:, :, q * 512:(q + 1) * 512], qTs[:], reads=[qTs], writes=[P.QT], sem=s_st[1])
        c.dma("sp", kT_v[:, :, q * 512:(q + 1) * 512], kTs[:], reads=[kTs], writes=[P.KT], sem=s_st[2])
        c.dma("sp", P.v_d[q * 512:(q + 1) * 512, :].rearrange("(s p) n -> p s n", p=128), vst[:], reads=[vst], writes=[P.VD], sem=s_st[3])
    c.barrier()
    c.release(m1)
    if getattr(build_program, "mixer_stop", 0) == 1:
        c.release(mM)
        return

    o_sb = c.sbuf("o_sb", [128, NT, D], BF16)
    m2 = c.mark()
    utri = c.sbuf("utri", [128, 128], F32); cmask = c.sbuf("cmask", [128, 4, 512], F32); dist0 = c.sbuf("dist0", [128, 512], F32)
    swM = c.sbuf("swM", [128, 5, 512], F32); swD = c.sbuf("swD", [128, 5, 512], F32)
    for t_, nm_ in ((utri, "utri"), (cmask, "cmask"), (dist0, "dist0"), (swM, "swM"), (swD, "swD")):
        c.dma("sp", t_[:], P.CD[nm_].rearrange("p (a b) -> p a b", b=512) if nm_ in ("cmask", "swM", "swD") else P.CD[nm_], reads=[P.CONSTD], writes=[t_], sem=s_const)
    sinkE = c.sbuf("sinkE", [128, 8], F32)
    c.dma("sp", sinkE[:], P.attn_sinks[l:l + 1, :].partition_broadcast(128), reads=[WCONST], writes=[sinkE], sem=s_const)
    c.op("act", lambda e: e.activation(out=sinkE[:], in_=sinkE[:], func=AF.Exp, bias=-CSH), reads=[sinkE], writes=[sinkE])

    cumK = c.sbuf("cumK", [128, 4, NT], F32)
    negck = c.sbuf("negck", [128, 4, NT], F32)
    tot = c.sbuf("tot", [128, 4, NT], F32)
    off = c.sbuf("off", [128, 4, NT], F32)
    cTs = c.sbuf("cTs", [128, 128], F32)
    S0 = c.psum("S0", [128, 512], F32); S1 = c.psum("S1", [128, 512], F32)
    OA = [c.psum(f"OA{j}", [128, 512], F32) for j in range(2)]
    G0 = c.psum("G0", [128, 512], F32); G1 = c.psum("G1", [128, 512], F32)
    lf2 = lf_all[:].rearrange("p h i -> p (h i)")
    c.op("pe", lambda e: e.matmul(S0[:, 0:4 * NT], lhsT=ones_f[:], rhs=lf2, start=True, stop=True), reads=[ones_f, lf_all], writes=[S0])
    c.op("pe", lambda e: e.matmul(S1[:, 0:4 * NT], lhsT=utri[:], rhs=lf2, start=True, stop=True), reads=[utri, lf_all], writes=[S1])
    c.op("dve", lambda e: e.tensor_copy(out=tot[:].rearrange("p h i -> p (h i)"), in_=S0[:, 0:4 * NT]), reads=[S0], writes=[tot])
    c.op("dve", lambda e: e.memset(off[:], 0.0), writes=[off])
    for i in range(1, NT):
        c.op("dve", lambda e: e.tensor_tensor(out=off[:, :, i], in0=off[:, :, i - 1], in1=tot[:, :, i - 1], op=ALU.add), reads=[off, tot], writes=[off])
    c.op("dve", lambda e: e.tensor_tensor(out=cumK[:].rearrange("p h i -> p (h i)"), in0=S1[:, 0:4 * NT], in1=off[:].rearrange("p h i -> p (h i)"), op=ALU.add),
         reads=[S1, off], writes=[cumK])
    c.op("dve", lambda e: e.tensor_scalar(out=negck[:], in0=cumK[:], scalar1=-1.0, scalar2=-CSH, op0=ALU.mult, op1=ALU.add), reads=[cumK], writes=[negck])
    c.op("pe", lambda e: e.transpose(S0[0:4 * NT, 0:128], cumK[:].rearrange("p h i -> p (h i)"), ident[:]), reads=[cumK, ident], writes=[S0])
    c.op("dve", lambda e: e.tensor_copy(out=cTs[0:4 * NT, :], in_=S0[0:4 * NT, 0:128]), reads=[S0], writes=[cTs])
    c.dma("sp", P.cumT_d.rearrange("h (i t) -> (h i) t", t=128), cTs[0:4 * NT, :], reads=[cTs], writes=[P.CUMT], sem=s_st[0])

    qTc = [c.sbuf(f"qTc{i}", [128, S], BF16) for i in range(2)]
    kTc = [c.sbuf(f"kTc{i}", [128, S], BF16) for i in range(2)]
    vaug = [c.sbuf(f"vaug{i}", [128, NT, 65], BF16) for i in range(2)]
    biasb = [c.sbuf(f"biasb{i}", [128, max(S, 2560)], F32) for i in range(2)]
    tts = [c.sbuf(f"tts{i}", [128, 512], F32) for i in range(4)]
    pex = [c.sbuf(f"pex{i}", [128, 512], BF16) for i in range(4)]
    rz = c.sbuf("rz", [128, 8], F32)
    S2 = c.psum("S2", [128, 512], F32); S3 = c.psum("S3", [128, 512], F32)
    SS = [S0, S1, S2, S3]
    cnt = [0]

    seq = []

    def attn_tile(qap, kap, extra, bias_ap, mask_ap, act_bias, vap, first, last, rd, oset, fin=None, qlo=0, qhi=512):
        k_ = cnt[0] % 4
        cnt[0] += 1
        ps = SS[k_]; tt = tts[k_]; pe_ = pex[k_]
        OAb = OA[oset]

        def A():
            c.op("pe", lambda e: e.matmul(ps[:, qlo:qhi], lhsT=kap, rhs=qap[:, qlo:qhi], start=True, stop=(extra is None)), reads=rd, writes=[ps], signal=(extra is None))
            if extra is not None:
                c.op("pe", lambda e: e.matmul(ps[:, qlo:qhi], lhsT=extra[0], rhs=extra[1][:, qlo:qhi], start=False, stop=True), reads=rd, writes=[ps])

        def B():
            c.op("dve", lambda e: e.tensor_tensor(out=tt[:, qlo:qhi], in0=ps[:, qlo:qhi], in1=bias_ap[:, qlo:qhi], op=ALU.add), reads=[ps] + rd, writes=[tt])
            if mask_ap is not None:
                c.op("dve", lambda e: e.tensor_tensor(out=tt[:, qlo:qhi], in0=tt[:, qlo:qhi], in1=mask_ap[:, qlo:qhi], op=ALU.add), reads=[tt] + rd, writes=[tt])
            c.op("act", lambda e: e.activation(out=pe_[:, qlo:qhi], in_=tt[:, qlo:qhi], func=AF.Exp, bias=act_bias), reads=[tt] + rd, writes=[pe_])

        def C():
            for j in range(qlo // 128, qhi // 128):
                c.op("pe", lambda e: e.matmul(OAb[:, j * 128:j * 128 + 65], lhsT=pe_[:, j * 128:(j + 1) * 128], rhs=vap, start=(first and j == 0), stop=last,
                                              skip_group_check=True),
                     reads=[pe_] + rd, writes=[OAb], signal=(j == qhi // 128 - 1))
            if fin is not None:
                qt, col, sink_ap = fin
                for j in range(4):
                    zc = OAb[:, j * 128 + 64:j * 128 + 65]
                    if sink_ap is not None:
                        c.op("dve", lambda e: e.tensor_tensor(out=rz[:, j:j + 1], in0=zc, in1=sink_ap, op=ALU.add), reads=[OAb, sinkE], writes=[rz])
                        c.op("dve", lambda e: e.reciprocal(out=rz[:, 4 + j:5 + j], in_=rz[:, j:j + 1]), reads=[rz], writes=[rz])
                    else:
                        c.op("dve", lambda e: e.reciprocal(out=rz[:, 4 + j:5 + j], in_=zc), reads=[OAb], writes=[rz])
                    c.op("dve", lambda e: e.tensor_scalar(out=o_sb[:, qt * 4 + j, col:col + 64], in0=OAb[:, j * 128:j * 128 + 64], scalar1=rz[:, 4 + j:5 + j],
                                                          scalar2=None, op0=ALU.mult), reads=[OAb, rz], writes=[o_sb])
        seq.append(("tile", A, B, C))

    def load_v(buf, vh, semi):
        with nc.allow_non_contiguous_dma("v head slice"):
            c.dma("sp", buf[:], P.v_d[:, vh * 65:(vh + 1) * 65].rearrange("(i p) n -> p i n", p=128), reads=[P.VD], writes=[buf], sem=s_a[semi])

    hcount = [0]
    qtc = [0]
    for h in range(4):
        b = hcount[0] % 2; hcount[0] += 1
        hp = (h % 2) * 64
        qc, kc_, vb, bb = qTc[b], kTc[b], vaug[b], biasb[b]

        def prep(h=h, b=b, hp=hp, qc=qc, kc_=kc_, vb=vb, bb=bb):
            c.dma("sp", qc[hp:hp + 64, :], P.qT_d[h * 64:(h + 1) * 64, :], reads=[P.QT], writes=[qc], sem=s_a[b])
            c.dma("sp", kc_[hp:hp + 64, :], P.kT_d[h * 64:(h + 1) * 64, :], reads=[P.KT], writes=[kc_], sem=s_a[2 + b])
            load_v(vb, h, 4 + b)
            c.dma("sp", bb[:, 0:S], P.cumT_d[h:h + 1, :].partition_broadcast(128), reads=[P.CUMT], writes=[bb], sem=s_x[b])
        seq.append(("prep", prep))
        rd = [qc, kc_, vb, bb, negck, cmask]
        for qt in range(NG):
            nk = 4 * qt + 4
            oset = qtc[0] % 2; qtc[0] += 1
            for kt in range(nk):
                j = kt - 4 * qt
                attn_tile(qc[hp:hp + 64, qt * 512:(qt + 1) * 512], kc_[hp:hp + 64, kt * 128:(kt + 1) * 128], None,
                          bb[:, qt * 512:(qt + 1) * 512], cmask[:, j, :] if j >= 0 else None, negck[:, h, kt:kt + 1],
                          vb[:, kt, :], kt == 0, kt == nk - 1, rd, oset, (qt, h * 64, None) if kt == nk - 1 else None, qlo=max(j, 0) * 128)
    for h in range(8):
        b = hcount[0] % 2; hcount[0] += 1
        hp = (h % 2) * 64
        kvh = h // 4
        qc, kc_, vb, bb = qTc[b], kTc[b], vaug[b], biasb[b]

        def prep(h=h, b=b, hp=hp, kvh=kvh, qc=qc, kc_=kc_, vb=vb, bb=bb):
            c.dma("sp", qc[hp:hp + 64, :], P.qT_d[256 + h * 64:256 + (h + 1) * 64, :], reads=[P.QT], writes=[qc], sem=s_a[b])
            c.dma("sp", kc_[hp:hp + 64, :], P.kT_d[256 + kvh * 64:256 + (kvh + 1) * 64, :], reads=[P.KT], writes=[kc_], sem=s_a[2 + b])
            load_v(vb, 4 + kvh, 4 + b)
            for jj in range(5):
                c.op("dve", lambda e: e.scalar_tensor_tensor(out=bb[:, jj * 512:(jj + 1) * 512], in0=swD[:, jj, :], scalar=-slopes[h], in1=swM[:, jj, :],
                                                             op0=ALU.mult, op1=ALU.add), reads=[swD, swM], writes=[bb])
        seq.append(("prep", prep))
        rd = [qc, kc_, vb, bb]
        for qt in range(NG):
            kts = [kt for kt in range(4 * qt - 1, 4 * qt + 4) if kt >= 0]
            oset = qtc[0] % 2; qtc[0] += 1
            for kt in kts:
                jj = kt - 4 * qt + 1
                attn_tile(qc[hp:hp + 64, qt * 512:(qt + 1) * 512], kc_[hp:hp + 64, kt * 128:(kt + 1) * 128], None,
                          bb[:, jj * 512:(jj + 1) * 512], None, -CSH, vb[:, kt, :], kt == kts[0], kt == kts[-1], rd, oset,
                          (qt, 256 + h * 64, sinkE[:, h:h + 1]) if kt == kts[-1] else None,
                          qlo=max(jj - 1, 0) * 128, qhi=min(jj + 1, 4) * 128)
    m3 = c.mark()
    ownN = c.sbuf("ownN", [128, NB, NB], F32); vmask = c.sbuf("vmask", [128, NB, NB], F32); eqB = c.sbuf("eqB", [128, NB, NB], F32)
    for t_, nm_ in ((ownN, "ownN"), (vmask, "vmask"), (eqB, "eqB")):
        c.dma("sp", t_[:], P.CD[nm_].rearrange("p (a b) -> p a b", b=NB), reads=[P.CONSTD], writes=[t_], sem=s_const)
    kms = c.sbuf("kms", [128, NB], F32); kmb = c.sbuf("kmb", [128, NB], BF16)
    gw = [c.sbuf(f"gw{i}", [128, 4 * NB + 8], F32) for i in range(2)]
    mvp = [c.sbuf(f"mvp{i}", [128, 80], F32) for i in range(2)]
    for t_ in mvp:
        c.op("dve", lambda e: e.memset(t_[:], 0.0), writes=[t_])
    GG = [G0, G1]
    for h in range(4):
        b = hcount[0] % 2; hcount[0] += 1
        hp = 0
        qc, kc_, vb, bb = qTc[b], kTc[b], vaug[b], biasb[b]
        sl = slopes[8 + h]
        R0 = 64 if hp == 0 else 48
        CP0 = 64 if hp == 0 else 32
        CPN = 16 if hp == 0 else 32
        KB, KE = (0, 80) if hp == 0 else (32, 128)

        def prep(h=h, b=b, hp=hp, qc=qc, kc_=kc_, vb=vb, bb=bb, sl=sl, R0=R0, CP0=CP0, CPN=CPN):
            c.dma("sp", qc[hp:hp + 64, :], P.qT_d[768 + h * 64:768 + (h + 1) * 64, :], reads=[P.QT], writes=[qc], sem=s_a[b])
            c.dma("sp", kc_[hp:hp + 64, :], P.kT_d[384 + h * 64:384 + (h + 1) * 64, :], reads=[P.KT], writes=[kc_], sem=s_a[2 + b])
            if hp == 0:
                c.dma("sp", kc_[64:80, :], P.CD["eselk"][16:32, :], reads=[P.CONSTD], writes=[kc_], sem=s_a[2 + b])
            else:
                c.dma("sp", kc_[32:64, :], P.CD["eselk"][0:32, :], reads=[P.CONSTD], writes=[kc_], sem=s_a[2 + b])
            load_v(vb, 6 + h, 4 + b)
            c.op("dve", lambda e: e.tensor_scalar(out=bb[:, 0:512], in0=dist0[:], scalar1=-sl, scalar2=None, op0=ALU.mult), reads=[dist0], writes=[bb])
            c.op("dve", lambda e: e.tensor_reduce(out=kms[hp:hp + 64, :], in_=kc_[hp:hp + 64, :].rearrange("p (n t) -> p n t", t=256), axis=AX.X, op=ALU.add), reads=[kc_], writes=[kms])
            c.op("dve", lambda e: e.tensor_scalar(out=kmb[hp:hp + 64, :], in0=kms[hp:hp + 64, :], scalar1=1.0 / 256, scalar2=None, op0=ALU.mult), reads=[kms], writes=[kmb])
            for i in range(NT):
                own = i // 2
                gp = GG[i % 2]; w = gw[i % 2]; mp = mvp[i % 2]
                gm = w[:, 0:NB]; sel = w[:, NB:2 * NB]; m8 = w[:, 4 * NB:4 * NB + 8]
                c.op("pe", lambda e: e.matmul(gp[:, 0:NB], lhsT=qc[hp:hp + 64, i * 128:(i + 1) * 128], rhs=kmb[hp:hp + 64, :], start=True, stop=True), reads=[qc, kmb], writes=[gp])
                c.op("dve", lambda e: e.tensor_tensor(out=gm, in0=gp[:, 0:NB], in1=ownN[:, own, :], op=ALU.add), reads=[gp, ownN], writes=[w])
                c.op("dve", lambda e: e.max(out=m8, in_=gm), reads=[w], writes=[w])
                c.op("dve", lambda e: e.tensor_scalar(out=sel, in0=gm, scalar1=m8[:, 2:3], scalar2=None, op0=ALU.is_ge), reads=[w], writes=[w])
                c.op("dve", lambda e: e.tensor_tensor(out=sel, in0=sel, in1=vmask[:, own, :], op=ALU.mult), reads=[w, vmask], writes=[w])
                c.op("dve", lambda e: e.scalar_tensor_tensor(out=mp[:, R0:R0 + NB], in0=sel, scalar=BIG, in1=eqB[:, own, :], op0=ALU.mult, op1=ALU.add), reads=[w, eqB], writes=[mp])
                c.op("pe", lambda e: e.matmul(gp[0:R0 + 16, 128:256], lhsT=mp[:, 0:R0 + 16], rhs=ident[:], start=True, stop=True), reads=[mp, ident], writes=[gp])
                c.op("act", lambda e: e.copy(out=qc[CP0:CP0 + CPN, i * 128:(i + 1) * 128], in_=gp[CP0:CP0 + CPN, 128:256]), reads=[gp], writes=[qc])
        seq.append(("prep", prep))
        rd = [qc, kc_, vb, bb, cmask]
        for qt in range(NG):
            nk = 4 * qt + 4
            oset = qtc[0] % 2; qtc[0] += 1
            for kt in range(nk):
                j = kt - 4 * qt
                attn_tile(qc[KB:KE, qt * 512:(qt + 1) * 512], kc_[KB:KE, kt * 128:(kt + 1) * 128], None,
                          bb[:, 0:512], cmask[:, j, :] if j >= 0 else None, float(-sl * (qt * 512 - kt * 128) - CSH),
                          vb[:, kt, :], kt == 0, kt == nk - 1, rd, oset, (qt, 768 + h * 64, None) if kt == nk - 1 else None, qlo=max(j, 0) * 128)
    LOOK = getattr(build_program, "look", 3)
    pend = []

    def flush(n):
        while len(pend) > n:
            it_ = pend.pop(0)
            it_[2](); it_[3]()
    for item in seq:
        if item[0] == "prep":
            flush(0)
            item[1]()
        else:
            item[1]()
            pend.append(item)
            flush(LOOK)
    flush(0)
    c.barrier()
    c.release(m2)
    if getattr(build_program, "mixer_stop", 0) == 2:
        c.release(mM)
        return

    m4 = c.mark()
    g1B = c.sbuf("g1B", [128, D], F32)
    c.dma("sp", g1B[:], P.mod_d[l:l + 1, 2 * D:3 * D].partition_broadcast(128), reads=[P.MODD], writes=[g1B], sem=s_const)
    wg = c.sbuf("wg", [128, 8, 3072], BF16)
    wbr = c.sbuf("wbr", [128, 8, D], BF16)
    wo = c.sbuf("wo", [128, 8, D], BF16)
    st3 = [c.sbuf(f"st3_{i}", [128, D], F32) for i in range(2)]
    pc = [0]

    def load_cast(dst_ap, src_ap):
        k_ = pc[0] % 2; pc[0] += 1
        st = st3[k_]
        c.dma("sp", st[:], src_ap, reads=[WCONST], writes=[st], sem=s_w[k_])
        return st

    for kc in range(8):
        for part in range(3):
            st = load_cast(None, P.w_in[l, kc * 128:(kc + 1) * 128, 2308 + part * 1024:2308 + (part + 1) * 1024])
            c.op("act", lambda e: e.copy(out=wg[:, kc, part * 1024:(part + 1) * 1024], in_=st[:]), reads=[st], writes=[wg])
        srcbr = (P.w_br_fox[l, kc * 128:(kc + 1) * 128, :] if kc < 2 else
                 P.w_br_swa[l, (kc - 2) * 128:(kc - 1) * 128, :] if kc < 6 else P.w_br_moba[l, (kc - 6) * 128:(kc - 5) * 128, :])
        st = load_cast(None, srcbr)
        c.op("dve", lambda e: e.tensor_copy(out=wbr[:, kc, :], in_=st[:]), reads=[st], writes=[wbr])
        st = load_cast(None, P.w_out[l, kc * 128:(kc + 1) * 128, :])
        c.op("dve", lambda e: e.tensor_copy(out=wo[:, kc, :], in_=st[:]), reads=[st], writes=[wo])
    hTg = c.sbuf("hTg3", [128, 8, 512], BF16)
    oT = c.sbuf("oT", [128, 8, 512], BF16)
    mT = c.sbuf("mT", [128, 8, 512], BF16)
    sg = [c.sbuf(f"sg{i}", [128, 512], F32) for i in range(2)]
    macc = [c.sbuf(f"macc{i}", [128, 512], F32) for i in range(2)]
    xt = [c.sbuf(f"xt{i}", [128, D], F32) for i in range(2)]
    xo = [c.sbuf(f"xo{i}", [128, D], F32) for i in range(2)]
    PT = c.psum("PT", [128, 512], F32)
    PG = [c.psum(f"PG{i}", [128, 512], F32) for i in range(2)]
    PY = [c.psum(f"PY{i}", [128, 512], F32) for i in range(2)]
    PO = c.psum("PO", [128, D], F32)
    br_k = [(0, 2), (2, 6), (6, 8)]
    for q in range(NG):
        c.dma("sp", hTg[:], hT_v[:, :, q * 512:(q + 1) * 512], reads=[P.HT], writes=[hTg], sem=s_a[0])
        ptb = PT[:].bitcast(BF16)
        for cc in range(8):
            for sub in range(4):
                c.op("pe", lambda e: e.transpose(ptb[:, sub * 128:(sub + 1) * 128], o_sb[:, q * 4 + sub, cc * 128:(cc + 1) * 128], identb[:]),
                     reads=[o_sb, identb], writes=[PT], signal=(sub == 3))
            c.op("act", lambda e: e.copy(out=oT[:, cc, :], in_=ptb[:, 0:512]), reads=[PT], writes=[oT])
        for jc in range(8):
            ma = macc[jc % 2]
            for br in range(3):
                pg_ = PG[(jc * 3 + br) % 2]; py_ = PY[(jc * 3 + br) % 2]; sgt = sg[(jc * 3 + br) % 2]
                gcol = br * 1024 + jc * 128
                for kc in range(8):
                    c.op("pe", lambda e: e.matmul(pg_[:], lhsT=wg[:, kc, gcol:gcol + 128], rhs=hTg[:, kc, :], start=(kc == 0), stop=(kc == 7)),
                         reads=[wg, hTg], writes=[pg_], signal=(kc == 7))
                k0, k1 = br_k[br]
                for kc in range(k0, k1):
                    c.op("pe", lambda e: e.matmul(py_[:], lhsT=wbr[:, kc, jc * 128:(jc + 1) * 128], rhs=oT[:, kc, :], start=(kc == k0), stop=(kc == k1 - 1)),
                         reads=[wbr, oT], writes=[py_], signal=(kc == k1 - 1))
                c.op("act", lambda e: e.activation(out=sgt[:], in_=pg_[:], func=AF.Sigmoid), reads=[pg_], writes=[sgt])
                if br == 0:
                    c.op("dve", lambda e: e.tensor_tensor(out=ma[:], in0=py_[:], in1=sgt[:], op=ALU.mult), reads=[py_, sgt], writes=[ma])
                else:
                    c.op("dve", lambda e: e.tensor_tensor(out=sgt[:], in0=py_[:], in1=sgt[:], op=ALU.mult), reads=[py_, sgt], writes=[sgt])
                    if br == 1:
                        c.op("dve", lambda e: e.tensor_tensor(out=ma[:], in0=ma[:], in1=sgt[:], op=ALU.add), reads=[ma, sgt], writes=[ma])
                    else:
                        c.op("dve", lambda e: e.tensor_tensor(out=mT[:, jc, :], in0=ma[:], in1=sgt[:], op=ALU.add), reads=[ma, sgt], writes=[mT])
        for sub in range(4):
            i = q * 4 + sub
            r0 = i * 128
            xb = xt[i % 2]; ob = xo[i % 2]
            c.dma("sp", xb[:], x_src[r0:r0 + 128, :], reads=[X_SRC], writes=[xb], sem=s_x[i % 2])
            for half in range(2):
                for kc in range(8):
                    c.op("pe", lambda e: e.matmul(PO[:, half * 512:(half + 1) * 512], lhsT=mT[:, kc, sub * 128:(sub + 1) * 128], rhs=wo[:, kc, half * 512:(half + 1) * 512],
                                                  start=(kc == 0), stop=(kc == 7)), reads=[mT, wo], writes=[PO], signal=(kc == 7))
            c.op("dve", lambda e: e.tensor_tensor(out=ob[:], in0=PO[:], in1=g1B[:], op=ALU.mult), reads=[PO, g1B], writes=[ob])
            c.op("dve", lambda e: e.tensor_tensor(out=ob[:], in0=ob[:], in1=xb[:], op=ALU.add), reads=[ob, xb], writes=[ob])
            c.dma("sp", P.xres_d[r0:r0 + 128, :], ob[:], reads=[ob], writes=[P.XRES], sem=s_st[i % 2])
    c.barrier()
    c.release(mM)


def moe_layer(P):
    c, nc, l, S, NE = P.c, P.nc, P.l, P.S, P.NE
    GT = min(2048, S)
    NGRP = S // GT
    NTG = GT // 128
    NQ = GT // 512
    ident, modc, a2c = P.ident, P.modc, P.a2c
    s_const, s_x, s_w, s_st = P.s_const, P.s_x, P.s_w, P.s_st
    WCONST = P.WCONST
    x_src, X_SRC = P.x_src, P.X_SRC
    if not (P.skip_mixer and l == 0):
        x_src, X_SRC = P.xres_d, P.XRES
    x_dst, X_DST = (P.y_out, P.YOUT) if P.last else (P.xres_d, P.XRES)

    mL = c.mark()
    g2B = c.sbuf("g2B", [128, D], F32)
    c.dma("sp", g2B[:], P.mod_d[l:l + 1, 5 * D:6 * D].partition_broadcast(128), reads=[P.MODD], writes=[g2B], sem=s_const)
    yacc = c.sbuf("yacc", [128, NTG, D], F32)
    h2T = c.sbuf("h2T", [128, 8, GT], BF16)
    gates = c.sbuf("gates", [128, NTG, NE], F32)
    b1T = c.sbuf("b1T", [128, NE, 16], F32)
    b2s = c.sbuf("b2s", [NE, D], F32)
    brB = c.sbuf("brB", [128, NE], F32)
    wr = c.sbuf("wr", [128, 8, NE], F32)

    m1 = c.mark()
    b1tm = c.sbuf("b1tm", [NE, 2 * D], F32)
    pt = c.psum("pt_b1", [128, 512], F32)
    c.dma("sp", b1tm[:], P.b_exp1[l], reads=[WCONST], writes=[b1tm], sem=s_const)
    c.dma("sp", b2s[:], P.b_exp2[l], reads=[WCONST], writes=[b2s], sem=s_const)
    c.dma("sp", brB[:], P.b_router[l:l + 1, :].partition_broadcast(128), reads=[WCONST], writes=[brB], sem=s_const)
    with nc.allow_non_contiguous_dma("router weights relayout"):
        c.dma("sp", wr[:], P.w_router[l].rearrange("(kc p) e -> p kc e", p=128), reads=[WCONST], writes=[wr], sem=s_const)
    for t in range(2):
        for fc in range(8):
            src = b1tm[:, fc * 256: (fc + 1) * 256].rearrange("e (p t) -> e t p", t=2)[:, t, :]
            c.op("pe", lambda e: e.transpose(pt[:, 0:NE], src, ident[0:NE, 0:NE]), reads=[b1tm, ident], writes=[pt])
            if t == 0:
                c.op("dve", lambda e: e.tensor_copy(out=b1T[:, :, t * 8 + fc], in_=pt[:, 0:NE]), reads=[pt], writes=[b1T])
            else:
                c.op("dve", lambda e: e.tensor_scalar(out=b1T[:, :, t * 8 + fc], in0=pt[:, 0:NE], scalar1=1.0, scalar2=None, op0=ALU.add), reads=[pt], writes=[b1T])
    c.barrier()
    c.release(m1)

    for g in range(NGRP):
        tok0 = g * GT
        m2 = c.mark()
        xs = [c.sbuf(f"xs{i}", [128, D], F32) for i in range(2)]
        junk = c.sbuf("junk", [128, D], F32)
        ss = [c.sbuf(f"ss{i}", [128, 4], F32) for i in range(2)]
        xn = c.sbuf("xn", [128, 4, D], F32)
        h32 = c.sbuf("h32", [128, 8, 512], F32)
        sm = [c.sbuf(f"sm{i}", [128, 4 * NE + 16], F32) for i in range(2)]
        gTs = c.sbuf("gTs", [NE, 128], F32)
        ptr = [c.psum(f"ptr{i}", [128, 512], F32) for i in range(2)]
        pr = [c.psum(f"pr{i}", [128, 512], F32) for i in range(2)]
        pb = c.psum("pb", [128, D], F32)
        for q in range(NQ):
            for sub in range(4):
                i = q * 4 + sub
                r0 = tok0 + i * 128
                xb = xs[i % 2]; sb = ss[i % 2]
                c.dma("sp", xb[:], x_src[r0:r0 + 128, :], reads=[X_SRC], writes=[xb], sem=s_x[i % 2])
                c.op("act", lambda e: e.activation(out=junk[:], in_=xb[:], func=AF.Square, accum_out=sb[:, 0:1]),
                     reads=[xb], writes=[junk, sb])
                c.op("act", lambda e: e.activation(out=sb[:, 1:2], in_=sb[:, 0:1], func=AF.Sqrt, scale=1.0 / D, bias=EPS),
                     reads=[sb], writes=[sb])
                c.op("dve", lambda e: e.reciprocal(out=sb[:, 2:3], in_=sb[:, 1:2]), reads=[sb], writes=[sb])
                c.op("dve", lambda e: e.tensor_scalar(out=xn[:, sub, :], in0=xb[:], scalar1=sb[:, 2:3], scalar2=None, op0=ALU.mult),
                     reads=[xb, sb], writes=[xn])
            for kc in range(8):
                pp = ptr[kc % 2]
                for sub in range(4):
                    c.op("pe", lambda e: e.transpose(pp[:, sub * 128:(sub + 1) * 128], xn[:, sub, kc * 128:(kc + 1) * 128], ident[:]),
                         reads=[xn, ident], writes=[pp], signal=(sub == 3))
                c.op("act", lambda e: e.activation(out=h32[:, kc, :], in_=pp[:], func=AF.Identity,
                                                   scale=a2c[:, kc:kc + 1], bias=modc[:, l, 24 + kc:25 + kc]),
                     reads=[pp, a2c, modc], writes=[h32])
                c.op("dve", lambda e: e.tensor_copy(out=h2T[:, kc, q * 512:(q + 1) * 512], in_=h32[:, kc, :]),
                     reads=[h32], writes=[h2T])
            for sub in range(4):
                ti = q * 4 + sub
                pq = pr[sub % 2]; w = sm[sub % 2]
                for kc in range(8):
                    c.op("pe", lambda e: e.matmul(pq[:, 0:NE], lhsT=h32[:, kc, sub * 128:(sub + 1) * 128], rhs=wr[:, kc, :],
                                                  start=(kc == 0), stop=(kc == 7)), reads=[h32, wr], writes=[pq], signal=(kc == 7))
                lg = w[:, 0:NE]; mk = w[:, NE:2 * NE]; ex = w[:, 2 * NE:3 * NE]; em = w[:, 3 * NE:4 * NE]
                m8 = w[:, 4 * NE:4 * NE + 8]; nm = w[:, 4 * NE + 8:4 * NE + 9]; sv = w[:, 4 * NE + 9:4 * NE + 10]
                rs = w[:, 4 * NE + 10:4 * NE + 11]
                c.op("dve", lambda e: e.tensor_tensor(out=lg, in0=pq[:, 0:NE], in1=brB[:], op=ALU.add), reads=[pq, brB], writes=[w])
                c.op("dve", lambda e: e.max(out=m8, in_=lg), reads=[w], writes=[w])
                c.op("dve", lambda e: e.tensor_scalar(out=mk, in0=lg, scalar1=m8[:, 3:4], scalar2=None, op0=ALU.is_ge), reads=[w], writes=[w])
                c.op("dve", lambda e: e.tensor_scalar(out=nm, in0=m8[:, 0:1], scalar1=-1.0, scalar2=None, op0=ALU.mult), reads=[w], writes=[w])
                c.op("act", lambda e: e.activation(out=ex, in_=lg, func=AF.Exp, bias=nm), reads=[w], writes=[w])
                c.op("dve", lambda e: e.scalar_tensor_tensor(out=em, in0=ex, scalar=1.0, in1=mk, op0=ALU.mult, op1=ALU.mult, accum_out=sv),
                     reads=[w], writes=[w])
                c.op("dve", lambda e: e.reciprocal(out=rs, in_=sv), reads=[w], writes=[w])
                c.op("dve", lambda e: e.tensor_scalar(out=gates[:, ti, :], in0=em, scalar1=rs, scalar2=None, op0=ALU.mult),
                     reads=[w], writes=[gates])
                c.op("pe", lambda e: e.transpose(pq[0:NE, 128:256], gates[:, ti, :], ident[:]), reads=[gates, ident], writes=[pq])
                c.op("act", lambda e: e.copy(out=gTs[:], in_=pq[0:NE, 128:256]), reads=[pq], writes=[gTs])
                for half in range(2):
                    c.op("pe", lambda e: e.matmul(pb[:, half * 512:(half + 1) * 512], lhsT=gTs[:], rhs=b2s[:, half * 512:(half + 1) * 512],
                                                  start=True, stop=True), reads=[gTs, b2s], writes=[pb])
                c.op("act", lambda e: e.copy(out=yacc[:, ti, :], in_=pb[:]), reads=[pb], writes=[yacc])
        c.barrier()
        c.release(m2)

        m3 = c.mark()
        w1b = [c.sbuf(f"w1b{i}", [128, 8, 2, 512], BF16) for i in range(2)]
        w2b = [c.sbuf(f"w2b{i}", [128, 4, D], BF16) for i in range(2)]
        stg = [c.sbuf(f"stg{i}", [128, 2, D], F32) for i in range(2)]
        abuf = [c.sbuf(f"abuf{i}", [128, 4, 512], BF16) for i in range(2)]
        t1 = [c.sbuf(f"t1_{i}", [128, 512], F32) for i in range(2)]
        t2 = [c.sbuf(f"t2_{i}", [128, 512], F32) for i in range(2)]
        t3 = [c.sbuf(f"t3_{i}", [128, 512], F32) for i in range(2)]
        pg = [c.psum(f"pg{i}", [128, 512], F32) for i in range(2)]
        pl = [c.psum(f"pl{i}", [128, 512], F32) for i in range(2)]
        py = [c.psum(f"py{i}", [128, D], F32) for i in range(2)]
        units = [(e, hf) for e in range(NE) for hf in range(2)]
        pcount = [0]

        def load_piece(u, pi):
            e, hf = units[u]
            bi = u % 2
            si = pcount[0] % 2
            pcount[0] += 1
            st = stg[si]
            if pi < 4:
                kc0 = pi * 2
                src = P.w_exp1[l, e, kc0 * 128:(kc0 + 2) * 128, hf * 1024:(hf + 1) * 1024].rearrange("(kc p) n -> p kc n", p=128)
                c.dma("sp", st[:], src, reads=[WCONST], writes=[st], sem=s_w[si])
                c.op("act", lambda en: en.copy(out=w1b[bi][:, kc0:kc0 + 2, :, :],
                                               in_=st[:].rearrange("p kc (f t) -> p kc t f", t=2)),
                     reads=[st], writes=[w1b[bi]])
            else:
                fc0 = (pi - 4) * 2
                r0 = (hf * 4 + fc0) * 128
                src = P.w_exp2[l, e, r0:r0 + 256, :].rearrange("(fc p) n -> p fc n", p=128)
                c.dma("sp", st[:], src, reads=[WCONST], writes=[st], sem=s_w[si])
                c.op("act", lambda en: en.copy(out=w2b[bi][:, fc0:fc0 + 2, :], in_=st[:]), reads=[st], writes=[w2b[bi]])

        for pi in range(6):
            load_piece(0, pi)
        its = [(u, q) for u in range(len(units)) for q in range(NQ)]

        def stage_w1(it):
            u, q = its[it]
            e, hf = units[u]
            bi = u % 2
            ab = abuf[it % 2]
            for fc in range(4):
                bg = pg[fc % 2]; bl = pl[fc % 2]
                a1_, a2_, a3_ = t1[fc % 2], t2[fc % 2], t3[fc % 2]
                for kc in range(8):
                    c.op("pe", lambda en: en.matmul(bg[:], lhsT=w1b[bi][:, kc, 0, fc * 128:(fc + 1) * 128],
                                                    rhs=h2T[:, kc, q * 512:(q + 1) * 512], start=(kc == 0), stop=(kc == 7)),
                         reads=[w1b[bi], h2T], writes=[bg], signal=(kc == 7))
                for kc in range(8):
                    c.op("pe", lambda en: en.matmul(bl[:], lhsT=w1b[bi][:, kc, 1, fc * 128:(fc + 1) * 128],
                                                    rhs=h2T[:, kc, q * 512:(q + 1) * 512], start=(kc == 0), stop=(kc == 7)),
                         reads=[w1b[bi], h2T], writes=[bl], signal=(kc == 7))
                bcol = hf * 4 + fc
                c.op("dve", lambda en: en.tensor_scalar(out=a1_[:], in0=bg[:], scalar1=b1T[:, e, bcol:bcol + 1], scalar2=7.0,
                                                        op0=ALU.add, op1=ALU.min), reads=[bg, b1T], writes=[a1_])
                c.op("act", lambda en: en.activation(out=a2_[:], in_=a1_[:], func=AF.Sigmoid, scale=1.702), reads=[a1_], writes=[a2_])
                c.op("dve", lambda en: en.tensor_scalar(out=a3_[:], in0=bl[:], scalar1=b1T[:, e, 8 + bcol:9 + bcol], scalar2=8.0,
                                                        op0=ALU.add, op1=ALU.min), reads=[bl, b1T], writes=[a3_])
                c.op("dve", lambda en: en.tensor_tensor(out=a1_[:], in0=a1_[:], in1=a2_[:], op=ALU.mult), reads=[a1_, a2_], writes=[a1_])
                c.op("dve", lambda en: en.scalar_tensor_tensor(out=ab[:, fc, :], in0=a3_[:], scalar=-6.0, in1=a1_[:], op0=ALU.max, op1=ALU.mult),
                     reads=[a1_, a3_], writes=[ab])

        def stage_w2(it):
            u, q = its[it]
            e, hf = units[u]
            bi = u % 2
            ab = abuf[it % 2]
            for sub in range(4):
                ti = q * 4 + sub
                yy = py[sub % 2]
                for half in range(2):
                    for fc in range(4):
                        c.op("pe", lambda en: en.matmul(yy[:, half * 512:(half + 1) * 512], lhsT=ab[:, fc, sub * 128:(sub + 1) * 128],
                                                        rhs=w2b[bi][:, fc, half * 512:(half + 1) * 512], start=(fc == 0), stop=(fc == 3)),
                             reads=[ab, w2b[bi]], writes=[yy], signal=(fc == 3 and half == 1))
                c.op("dve", lambda en: en.scalar_tensor_tensor(out=yacc[:, ti, :], in0=yy[:], scalar=gates[:, ti, e:e + 1], in1=yacc[:, ti, :],
                                                               op0=ALU.mult, op1=ALU.add), reads=[yy, gates, yacc], writes=[yacc])

        stage_w1(0)
        for it, (u, q) in enumerate(its):
            if u + 1 < len(units):
                lo, hi = (q * 6) // NQ, ((q + 1) * 6) // NQ
                for pi in range(lo, hi):
                    load_piece(u + 1, pi)
            if it + 1 < len(its):
                stage_w1(it + 1)
            stage_w2(it)
        c.barrier()
        c.release(m3)

        m4 = c.mark()
        xt = [c.sbuf(f"xt{i}", [128, D], F32) for i in range(2)]
        xo = [c.sbuf(f"xo{i}", [128, D], F32) for i in range(2)]
        for ti in range(NTG):
            r0 = tok0 + ti * 128
            xb = xt[ti % 2]; ob = xo[ti % 2]
            c.dma("sp", xb[:], x_src[r0:r0 + 128, :], reads=[X_SRC], writes=[xb], sem=s_x[ti % 2])
            c.op("dve", lambda en: en.tensor_tensor(out=ob[:], in0=yacc[:, ti, :], in1=g2B[:], op=ALU.mult), reads=[yacc, g2B], writes=[ob])
            c.op("dve", lambda en: en.tensor_tensor(out=ob[:], in0=ob[:], in1=xb[:], op=ALU.add), reads=[ob, xb], writes=[ob])
            c.dma("sp", x_dst[r0:r0 + 128, :], ob[:], reads=[ob], writes=[X_DST], sem=s_st[ti % 2])
        c.barrier()
        c.release(m4)
    c.release(mL)


def alibi_slopes():
    return [2.0 ** (-8.0 * i / 12.0) for i in range(1, 13)]


def build_program(S=4096, L=4, NE=32, dbg=False):
    NT = S // 128
    NG = S // 512
    NB = S // 256
    nc = bass.Bass("TRN2", target_bir_lowering=False)
    c = Ctx(nc)

    def din(name, shape):
        return nc.dram_tensor(name, shape, F32, kind="ExternalInput").ap()

    x_in = din("x", [S, D]); c_in = din("c", [1, D])
    w_ada = din("w_ada", [L, D, 6 * D]); b_ada = din("b_ada", [L, 6 * D])
    norm_gain = din("norm_gain", [L, 2, D]); w_in = din("w_in", [L, D, 5380])
    b_fgate = din("b_fgate", [L, 4]); qk_gain = din("qk_gain", [L, 6, 64])
    attn_sinks = din("attn_sinks", [L, 8])
    w_br_fox = din("w_br_fox", [L, 256, D]); w_br_swa = din("w_br_swa", [L, 512, D])
    w_br_moba = din("w_br_moba", [L, 256, D]); w_out = din("w_out", [L, D, D])
    w_router = din("w_router", [L, D, NE]); b_router = din("b_router", [L, NE])
    w_exp1 = din("w_exp1", [L, NE, D, 2 * D]); b_exp1 = din("b_exp1", [L, NE, 2 * D])
    w_exp2 = din("w_exp2", [L, NE, D, D]); b_exp2 = din("b_exp2", [L, NE, D])
    y_out = nc.dram_tensor("y", [S, D], F32, kind="ExternalOutput").ap()

    def dscr(name, shape, dt):
        kind = "ExternalOutput" if dbg else "Internal"
        return nc.dram_tensor(name, shape, dt, kind=kind).ap()

    xres_d = dscr("xres_d", [S, D], F32)
    hT_d = dscr("hT_d", [D, S], BF16)
    qT_d = dscr("qT_d", [1024, S], BF16)
    kT_d = dscr("kT_d", [640, S], BF16)
    v_d = dscr("v_d", [S, 650], BF16)
    cumT_d = dscr("cumT_d", [4, S], F32)
    mod_d = dscr("mod_d", [L, 6 * D], F32)

    X_IN = Obj("x_in"); XRES = Obj("xres"); HT = Obj("hT"); QT = Obj("qT"); KT = Obj("kT")
    VD = Obj("vd"); CUMT = Obj("cumT"); MODD = Obj("modd"); YOUT = Obj("yout"); WCONST = Obj("w")

    s_const = c.new_sem(16, "s_const")
    s_x = [c.new_sem(16, f"s_x{i}") for i in range(2)]
    s_w = [c.new_sem(16, f"s_w{i}") for i in range(4)]
    s_st = [c.new_sem(16, f"s_st{i}") for i in range(4)]
    s_a = [c.new_sem(16, f"s_a{i}") for i in range(6)]

    slopes = alibi_slopes()

    ident = c.sbuf("ident", [128, 128], F32)
    identb = c.sbuf("identb", [128, 128], BF16)
    ones_f = c.sbuf("ones_f", [128, 128], F32)
    c.op("pool", lambda e: e.memset(ident[:], 0.0), writes=[ident])
    c.op("pool", lambda e: e.affine_select(out=ident[:], in_=ident[:], pattern=[[-1, 128]],
                                           compare_op=ALU.not_equal, fill=1.0, base=0,
                                           channel_multiplier=1), reads=[ident], writes=[ident])
    c.op("dve", lambda e: e.tensor_copy(out=identb[:], in_=ident[:]), reads=[ident], writes=[identb])
    c.op("pool", lambda e: e.memset(ones_f[:], 1.0), writes=[ones_f])
    modc = c.sbuf("modc", [128, L, 48], F32)
    a1c = c.sbuf("a1c", [128, 8], F32); a2c = c.sbuf("a2c", [128, 8], F32)
    ngc = c.sbuf("ngc", [128, 2, 8], F32)

    m0 = c.mark()
    condc = c.sbuf("condc", [128, 8], F32)
    ccol = c.sbuf("ccol", [128, 8], F32)
    with nc.allow_non_contiguous_dma("small vector relayout"):
        c.dma("sp", ccol[:], c_in.rearrange("o (k p) -> p (o k)", p=128), reads=[WCONST], writes=[ccol], sem=s_const)
    c.op("act", lambda e: e.activation(out=condc[:], in_=ccol[:], func=AF.Silu), reads=[ccol], writes=[condc])
    wa = [c.sbuf(f"wa{i}", [128, 6 * D], F32) for i in range(2)]
    modrow = c.sbuf("modrow", [1, 6 * D], F32)
    brow = c.sbuf("brow", [1, 6 * D], F32)
    pm = [c.psum(f"pm{i}", [128, 512], F32) for i in range(4)]
    for l in range(L):
        c.dma("sp", brow[:], b_ada[l:l + 1, :], reads=[WCONST], writes=[brow], sem=s_const)
        for half in range(3):
            for kc in range(8):
                wt = wa[kc % 2]
                c.dma("sp", wt[:, half * 2048:(half + 1) * 2048],
                      w_ada[l, kc * 128:(kc + 1) * 128, half * 2048:(half + 1) * 2048],
                      reads=[WCONST], writes=[wt], sem=s_w[kc % 2])
                for n in range(4):
                    c.op("pe", lambda e: e.matmul(pm[n][0:1, :], lhsT=condc[:, kc:kc + 1],
                                                  rhs=wt[:, half * 2048 + n * 512: half * 2048 + (n + 1) * 512],
                                                  start=(kc == 0), stop=(kc == 7)),
                         reads=[condc, wt], writes=[pm[n]], signal=(kc == 7 or n == 3))
            for n in range(4):
                cs = half * 2048 + n * 512
                c.op("dve", lambda e: e.tensor_tensor(out=modrow[:, cs:cs + 512], in0=pm[n][0:1, :],
                                                      in1=brow[:, cs:cs + 512], op=ALU.add),
                     reads=[pm[n], brow], writes=[modrow])
        c.dma("sp", mod_d[l:l + 1, :], modrow[:], reads=[modrow], writes=[MODD], sem=s_st[0])
    with nc.allow_non_contiguous_dma("small vector relayout"):
        for l in range(L):
            c.dma("sp", modc[:, l, :], mod_d[l:l + 1, :].rearrange("o (j p) -> p (o j)", p=128),
                  reads=[MODD], writes=[modc], sem=s_const)
    c.barrier()
    c.release(m0)

    CD, CONSTD = attn_consts(c, nc, NB, s_const, dscr, ident, S)
    for l in range(L):
        last = (l == L - 1)
        x_src, X_SRC = (x_in, X_IN) if l == 0 else (xres_d, XRES)

        with nc.allow_non_contiguous_dma("small vector relayout"):
            c.dma("sp", ngc[:], norm_gain[l].rearrange("t (j p) -> p t j", p=128), reads=[WCONST], writes=[ngc], sem=s_const)
        c.op("dve", lambda e: e.scalar_tensor_tensor(out=a1c[:], in0=modc[:, l, 8:16], scalar=1.0, in1=ngc[:, 0, :],
                                                     op0=ALU.add, op1=ALU.mult), reads=[modc, ngc], writes=[a1c])
        c.op("dve", lambda e: e.scalar_tensor_tensor(out=a2c[:], in0=modc[:, l, 32:40], scalar=1.0, in1=ngc[:, 1, :],
                                                     op0=ALU.add, op1=ALU.mult), reads=[modc, ngc], writes=[a2c])

        skip_mixer = getattr(build_program, 'skip_mixer', False)
        skip_moe = getattr(build_program, 'skip_moe', False)
        P = SimpleNamespace(**locals())
        if not skip_mixer:
            mixer_layer(P)
        if not skip_moe:
            moe_layer(P)

    sp = c.E["sp"]
    c._collect(sp, [YOUT], ())
    c.close()
    return nc


_NAMES = ["w_ada", "b_ada", "norm_gain", "w_in", "b_fgate", "qk_gain", "attn_sinks", "w_br_fox", "w_br_swa",
          "w_br_moba", "w_out", "w_router", "b_router", "w_exp1", "b_exp1", "w_exp2", "b_exp2"]


def kernel(**inputs):
    x = np.ascontiguousarray(np.asarray(inputs["x"], dtype=np.float32))
    cvec = np.ascontiguousarray(np.asarray(inputs["c"], dtype=np.float32))
    B, S, _ = x.shape
    L = int(np.asarray(inputs["w_ada"]).shape[0])
    NE = int(np.asarray(inputs["w_router"]).shape[2])
    shared = {k: np.ascontiguousarray(np.asarray(inputs[k], dtype=np.float32)) for k in _NAMES}
    nc = build_program(S=S, L=L, NE=NE)
    in_maps = []
    for b in range(B):
        m = {"x": x[b], "c": cvec[b:b + 1]}
        m.update(shared)
        in_maps.append(m)
    res = run_bass_kernel_spmd(nc, in_maps, core_ids=list(range(B)))
    return np.stack([np.asarray(r["y"], dtype=np.float32) for r in res.results], axis=0)
```

```python
import numpy as np
from types import SimpleNamespace
import concourse.bass as bass
import concourse.mybir as mybir
from concourse.bass_utils import run_bass_kernel_spmd

F32 = mybir.dt.float32
BF16 = mybir.dt.bfloat16
AF = mybir.ActivationFunctionType
ALU = mybir.AluOpType
AX = mybir.AxisListType

D = 1024
NEG = -1.0e30
BIG = 30000.0
CSH = 8.0
EPS = 1e-6


class Sem:
    __slots__ = ("h", "issued", "step")

    def __init__(self, h, step):
        self.h = h
        self.issued = 0
        self.step = step


class Obj:
    __slots__ = ("name", "w", "r", "dsem", "t")

    def __init__(self, name, t=None):
        self.name = name
        self.w = []
        self.r = []
        self.dsem = None
        self.t = t

    def __getitem__(self, k):
        return self.t[k]


class Eng:
    __slots__ = ("name", "h", "sem", "seen")

    def __init__(self, name, h, sem):
        self.name = name
        self.h = h
        self.sem = sem
        self.seen = {}


class Ctx:
    def __init__(self, nc):
        self.nc = nc
        self._stack = []
        self.sems = []
        self.E = {}
        for name, h in (("pe", nc.tensor), ("dve", nc.vector), ("act", nc.scalar),
                        ("pool", nc.gpsimd), ("sp", nc.sync)):
            self.E[name] = Eng(name, h, self.new_sem(1, "e_" + name))
        self.n_wait = 0
        self.n_ins = 0

    def new_sem(self, step, name=None):
        cm = self.nc.semaphore(name)
        h = cm.__enter__()
        s = Sem(h, step)
        self.sems.append(s)
        return s

    def mark(self):
        return len(self._stack)

    def release(self, mark):
        while len(self._stack) > mark:
            self._stack.pop().__exit__(None, None, None)

    def sbuf(self, name, shape, dt, dsem=None):
        self.n_ins += 0
        self._uid = getattr(self, "_uid", 0) + 1
        name = f"{name}_u{self._uid}"
        cm = self.nc.sbuf_tensor(name, shape, dt)
        t = cm.__enter__()
        self._stack.append(cm)
        o = Obj(name, t)
        o.dsem = dsem
        return o

    def psum(self, name, shape, dt):
        self._uid = getattr(self, "_uid", 0) + 1
        name = f"{name}_u{self._uid}"
        cm = self.nc.psum_tensor(name, shape, dt)
        t = cm.__enter__()
        self._stack.append(cm)
        return Obj(name, t)

    def _collect(self, eng, reads, writes):
        need = {}
        for o in reads:
            for (s, v) in o.w:
                if need.get(s, 0) < v:
                    need[s] = v
        for o in writes:
            for (s, v) in o.w:
                if need.get(s, 0) < v:
                    need[s] = v
            for (s, v) in o.r:
                if need.get(s, 0) < v:
                    need[s] = v
        for s, v in need.items():
            if s is eng.sem and eng.name == "pe":
                continue
            if s.step == 16:
                v = s.issued
            assert v <= s.issued, f"wait on unsignaled ticket eng={eng.name}"
            if eng.seen.get(s, 0) < v:
                eng.h.wait_ge(s.h, v)
                eng.seen[s] = v
                self.n_wait += 1

    def _record(self, tk, reads, writes):
        for o in writes:
            o.w = [tk]
            o.r = []
        for o in reads:
            if o not in writes:
                o.r = [t for t in o.r if t[0] is not tk[0]] + [tk]

    def op(self, en, fn, reads=(), writes=(), signal=True):
        eng = self.E[en]
        self._collect(eng, reads, writes)
        ins = fn(eng.h)
        self.n_ins += 1
        if signal:
            ins.then_inc(eng.sem.h, 1)
            eng.sem.issued += 1
            tk = (eng.sem, eng.sem.issued)
        else:
            tk = (eng.sem, eng.sem.issued + 1)
        self._record(tk, reads, writes)
        return ins

    def dma(self, qn, out, in_, reads=(), writes=(), sem=None, **kw):
        eng = self.E[qn]
        self._collect(eng, reads, writes)
        ins = eng.h.dma_start(out=out, in_=in_, **kw)
        ins.then_inc(sem.h, 16)
        sem.issued += 16
        self.n_ins += 1
        self._record((sem, sem.issued), reads, writes)
        return ins

    def barrier(self):
        for eng in self.E.values():
            for s in self.sems:
                if s is eng.sem:
                    continue
                if s.issued > 0 and eng.seen.get(s, 0) < s.issued:
                    eng.h.wait_ge(s.h, s.issued)
                    eng.seen[s] = s.issued

    def close(self):
        self.release(0)


def attn_consts(c, nc, NB, s_const, dscr, ident, S):
    m0 = c.mark()
    CONSTD = Obj("constd")
    CD = {}
    utri = c.sbuf("utri", [128, 128], F32)
    c.op("pool", lambda e: e.memset(utri[:], 1.0), writes=[utri])
    c.op("pool", lambda e: e.affine_select(out=utri[:], in_=utri[:], pattern=[[1, 128]], compare_op=ALU.is_ge, fill=0.0, base=0,
                                           channel_multiplier=-1), reads=[utri], writes=[utri])
    cmask = c.sbuf("cmask", [128, 4, 512], F32)
    c.op("pool", lambda e: e.memset(cmask[:], 0.0), writes=[cmask])
    for j in range(4):
        c.op("pool", lambda e: e.affine_select(out=cmask[:, j, :], in_=cmask[:, j, :], pattern=[[1, 512]], compare_op=ALU.is_ge, fill=NEG,
                                               base=-128 * j, channel_multiplier=-1), reads=[cmask], writes=[cmask])
    dist0 = c.sbuf("dist0", [128, 512], F32)
    c.op("pool", lambda e: e.iota(dist0[:], pattern=[[1, 512]], base=0, channel_multiplier=-1, allow_small_or_imprecise_dtypes=True), writes=[dist0])
    swM = c.sbuf("swM", [128, 5, 512], F32); swD = c.sbuf("swD", [128, 5, 512], F32)
    c.op("pool", lambda e: e.memset(swM[:], 0.0), writes=[swM])
    for jj in range(5):
        j = jj - 1
        c.op("pool", lambda e: e.affine_select(out=swM[:, jj, :], in_=swM[:, jj, :], pattern=[[1, 512]], compare_op=ALU.is_ge, fill=NEG,
                                               base=-128 * j, channel_multiplier=-1), reads=[swM], writes=[swM])
        c.op("pool", lambda e: e.affine_select(out=swM[:, jj, :], in_=swM[:, jj, :], pattern=[[-1, 512]], compare_op=ALU.is_ge, fill=NEG,
                                               base=127 + 128 * j, channel_multiplier=1), reads=[swM], writes=[swM])
        c.op("pool", lambda e: e.iota(swD[:, jj, :], pattern=[[1, 512]], base=-128 * j, channel_multiplier=-1, allow_small_or_imprecise_dtypes=True), writes=[swD])

    ownN = c.sbuf("ownN", [128, NB, NB], F32); vmask = c.sbuf("vmask", [128, NB, NB], F32); eqB = c.sbuf("eqB", [128, NB, NB], F32)
    eself = c.sbuf("eself", [NB, NB, 128], F32)
    c.op("pool", lambda e: e.memset(ownN[:], 0.0), writes=[ownN])
    c.op("pool", lambda e: e.memset(vmask[:], 1.0), writes=[vmask])
    c.op("pool", lambda e: e.memset(eqB[:], 0.0), writes=[eqB])
    c.op("pool", lambda e: e.memset(eself[:], 1.0), writes=[eself])
    for o_ in range(NB):
        pat = [[-1, NB]]
        c.op("pool", lambda e: e.affine_select(out=ownN[:, o_, :], in_=ownN[:, o_, :], pattern=pat, compare_op=ALU.is_ge, fill=NEG, base=o_ - 1, channel_multiplier=0), reads=[ownN], writes=[ownN])
        c.op("pool", lambda e: e.affine_select(out=eqB[:, o_, :], in_=eqB[:, o_, :], pattern=pat, compare_op=ALU.is_equal, fill=-BIG, base=o_, channel_multiplier=0), reads=[eqB], writes=[eqB])
    c.op("dve", lambda e: e.tensor_scalar(out=vmask[:], in0=ownN[:], scalar1=0.0, scalar2=None, op0=ALU.is_equal), reads=[ownN], writes=[vmask])
    for n_ in range(NB):
        c.op("dve", lambda e: e.tensor_scalar(out=eself[:, n_, :], in0=eself[:, n_, :], scalar1=ident[0:NB, n_:n_ + 1], scalar2=None, op0=ALU.mult),
             reads=[eself, ident], writes=[eself])

    for t_, nm_, shp in ((utri, "utri", [128, 128]), (cmask, "cmask", [128, 2048]), (dist0, "dist0", [128, 512]), (swM, "swM", [128, 2560]),
                         (swD, "swD", [128, 2560]), (ownN, "ownN", [128, NB * NB]), (vmask, "vmask", [128, NB * NB]), (eqB, "eqB", [128, NB * NB]),
                         (eself, "eself", [NB, NB * 128])):
        CD[nm_] = dscr("cd_" + nm_, shp, F32)
        src = t_[:] if len(t_[:].shape) == 2 else t_[:].rearrange("p a b -> p (a b)")
        c.dma("sp", CD[nm_], src, reads=[t_], writes=[CONSTD], sem=s_const)
    ek = c.sbuf("ek", [32, S], F32); ekb = c.sbuf("ekb", [32, S], BF16)
    c.op("pool", lambda e: e.memset(ek[:], 0.0), writes=[ek])
    c.op("pool", lambda e: e.memset(ek[0:NB, :], 1.0), reads=[ek], writes=[ek])
    c.op("pool", lambda e: e.affine_select(out=ek[0:NB, :], in_=ek[0:NB, :], pattern=[[1, S]], compare_op=ALU.is_ge, fill=0.0, base=0,
                                           channel_multiplier=-256), reads=[ek], writes=[ek])
    c.op("pool", lambda e: e.affine_select(out=ek[0:NB, :], in_=ek[0:NB, :], pattern=[[-1, S]], compare_op=ALU.is_ge, fill=0.0, base=255,
                                           channel_multiplier=256), reads=[ek], writes=[ek])
    c.op("dve", lambda e: e.tensor_copy(out=ekb[:], in_=ek[:]), reads=[ek], writes=[ekb])
    CD["eselk"] = dscr("cd_eselk", [32, S], BF16)
    c.dma("sp", CD["eselk"][16:16 + NB, :], ekb[0:NB, :], reads=[ekb], writes=[CONSTD], sem=s_const)
    c.dma("sp", CD["eselk"][0:16, :], ekb[16:32, :], reads=[ekb], writes=[CONSTD], sem=s_const)
    if NB < 16:
        c.dma("sp", CD["eselk"][16 + NB:32, :], ekb[16:32 - NB, :], reads=[ekb], writes=[CONSTD], sem=s_const)
    c.barrier()
    c.release(m0)
    return CD, CONSTD

def mixer_layer(P):
    c, nc, l, S = P.c, P.nc, P.l, P.S
    NT = S // 128; NG = S // 512; NB = S // 256
    ident, identb, ones_f, modc, a1c = P.ident, P.identb, P.ones_f, P.modc, P.a1c
    s_const, s_x, s_w, s_st, s_a = P.s_const, P.s_x, P.s_w, P.s_st, P.s_a
    WCONST = P.WCONST
    x_src, X_SRC = P.x_src, P.X_SRC
    slopes = P.slopes
    hT_v = P.hT_d.rearrange("(kc p) t -> p kc t", p=128)
    qT_v = P.qT_d.rearrange("(cc p) t -> p cc t", p=128)
    kT_v = P.kT_d.rearrange("(cc p) t -> p cc t", p=128)

    mM = c.mark()
    lf_all = c.sbuf("lf_all", [128, 4, NT], F32)
    m1 = c.mark()
    gQ = c.sbuf("gQ", [128, 1024], F32); gK = c.sbuf("gK", [128, 640], F32)
    bfB = c.sbuf("bfB", [128, 4], F32)
    qrow = [0] * 4 + [2] * 8 + [4] * 4
    krow = [1] * 4 + [3] * 2 + [5] * 4
    for h in range(16):
        c.dma("sp", gQ[:, h * 64:(h + 1) * 64], P.qk_gain[l, qrow[h]:qrow[h] + 1, :].partition_broadcast(128), reads=[WCONST], writes=[gQ], sem=s_const)
    for h in range(10):
        c.dma("sp", gK[:, h * 64:(h + 1) * 64], P.qk_gain[l, krow[h]:krow[h] + 1, :].partition_broadcast(128), reads=[WCONST], writes=[gK], sem=s_const)
    c.dma("sp", bfB[:], P.b_fgate[l:l + 1, :].partition_broadcast(128), reads=[WCONST], writes=[bfB], sem=s_const)
    c.op("dve", lambda e: e.tensor_scalar(out=gQ[:], in0=gQ[:], scalar1=0.125, scalar2=None, op0=ALU.mult), reads=[gQ], writes=[gQ])
    wqkv = c.sbuf("wqkv", [128, 8, 2308], BF16)
    wst = [c.sbuf(f"wst{i}", [128, 2308], F32) for i in range(2)]
    segs = [(0, 256, 0), (772, 1284, 256), (1540, 1796, 768),
            (256, 512, 1024), (1284, 1412, 1280), (1796, 2052, 1408),
            (512, 768, 1664), (1412, 1540, 1920), (2052, 2308, 2048),
            (768, 772, 2304)]
    for kc in range(8):
        st = wst[kc % 2]
        c.dma("sp", st[:], P.w_in[l, kc * 128:(kc + 1) * 128, 0:2308], reads=[WCONST], writes=[st], sem=s_w[kc % 2])
        for si, (a, b, d0) in enumerate(segs):
            en = "act" if si % 2 == 0 else "dve"
            if en == "act":
                c.op("act", lambda e: e.copy(out=wqkv[:, kc, d0:d0 + (b - a)], in_=st[:, a:b]), reads=[st], writes=[wqkv])
            else:
                c.op("dve", lambda e: e.tensor_copy(out=wqkv[:, kc, d0:d0 + (b - a)], in_=st[:, a:b]), reads=[st], writes=[wqkv])
    xs = [c.sbuf(f"xs{i}", [128, D], F32) for i in range(2)]
    junk = c.sbuf("junk", [128, D], F32)
    ss = [c.sbuf(f"ss{i}", [128, 4], F32) for i in range(2)]
    xn = c.sbuf("xn", [128, 4, D], F32)
    hTg = c.sbuf("hTg", [128, 8, 512], BF16)
    sqs = c.sbuf("sqs", [128, 1664], F32)
    rq = c.sbuf("rq", [128, 64], F32)
    qn = c.sbuf("qn", [128, 1024], BF16); kn = c.sbuf("kn", [128, 640], BF16)
    qTs = c.sbuf("qTs", [128, 8, 512], BF16); kTs = c.sbuf("kTs", [128, 5, 512], BF16)
    vst = c.sbuf("vst", [128, 4, 650], BF16)
    lft = c.sbuf("lft", [128, 8], F32)
    c.op("pool", lambda e: e.memset(vst[:], 1.0), writes=[vst])
    T0 = c.psum("T0", [128, 512], F32); T1 = c.psum("T1", [128, 512], F32)
    pq = c.psum("pq", [128, 1024], F32); pk = c.psum("pk", [128, 1024], F32); pv = c.psum("pv", [128, 1024], F32)
    TT = [T0, T1]
    for q in range(NG):
        for sub in range(4):
            i = q * 4 + sub
            r0 = i * 128
            xb = xs[i % 2]; sb = ss[i % 2]
            c.dma("sp", xb[:], x_src[r0:r0 + 128, :], reads=[X_SRC], writes=[xb], sem=s_x[i % 2])
            c.op("act", lambda e: e.activation(out=junk[:], in_=xb[:], func=AF.Square, accum_out=sb[:, 0:1]), reads=[xb], writes=[junk, sb])
            c.op("act", lambda e: e.activation(out=sb[:, 1:2], in_=sb[:, 0:1], func=AF.Sqrt, scale=1.0 / D, bias=EPS), reads=[sb], writes=[sb])
            c.op("dve", lambda e: e.reciprocal(out=sb[:, 2:3], in_=sb[:, 1:2]), reads=[sb], writes=[sb])
            c.op("dve", lambda e: e.tensor_scalar(out=xn[:, sub, :], in0=xb[:], scalar1=sb[:, 2:3], scalar2=None, op0=ALU.mult), reads=[xb, sb], writes=[xn])
        for kc in range(8):
            pp = TT[kc % 2]
            for sub in range(4):
                c.op("pe", lambda e: e.transpose(pp[:, sub * 128:(sub + 1) * 128], xn[:, sub, kc * 128:(kc + 1) * 128], ident[:]),
                     reads=[xn, ident], writes=[pp], signal=(sub == 3))
            c.op("act", lambda e: e.activation(out=hTg[:, kc, :], in_=pp[:], func=AF.Identity, scale=a1c[:, kc:kc + 1], bias=modc[:, l, kc:kc + 1]),
                 reads=[pp, a1c, modc], writes=[hTg])
        c.dma("pool", hT_v[:, :, q * 512:(q + 1) * 512], hTg[:], reads=[hTg], writes=[P.HT], sem=s_st[0])
        for sub in range(4):
            i = q * 4 + sub
            lhs = lambda kc: hTg[:, kc, sub * 128:(sub + 1) * 128]
            for (pt_, c0, n, w0) in ((pq, 0, 512, 0), (pq, 512, 512, 512), (pk, 0, 512, 1024), (pk, 512, 128, 1536),
                                     (pv, 0, 512, 1664), (pv, 512, 132, 2176)):
                for kc in range(8):
                    c.op("pe", lambda e: e.matmul(pt_[:, c0:c0 + n], lhsT=lhs(kc), rhs=wqkv[:, kc, w0:w0 + n], start=(kc == 0), stop=(kc == 7)),
                         reads=[hTg, wqkv], writes=[pt_], signal=(kc == 7))
            c.op("act", lambda e: e.activation(out=sqs[:, 0:1024], in_=pq[:], func=AF.Square), reads=[pq], writes=[sqs])
            c.op("act", lambda e: e.activation(out=sqs[:, 1024:1664], in_=pk[:, 0:640], func=AF.Square), reads=[pk], writes=[sqs])
            c.op("dve", lambda e: e.tensor_reduce(out=rq[:, 0:26], in_=sqs[:].rearrange("p (h d) -> p h d", d=64), axis=AX.X, op=ALU.add), reads=[sqs], writes=[rq])
            c.op("act", lambda e: e.activation(out=rq[:, 32:58], in_=rq[:, 0:26], func=AF.Sqrt, scale=1.0 / 64, bias=EPS), reads=[rq], writes=[rq])
            c.op("dve", lambda e: e.reciprocal(out=rq[:, 0:26], in_=rq[:, 32:58]), reads=[rq], writes=[rq])
            for h in range(16):
                c.op("dve", lambda e: e.scalar_tensor_tensor(out=qn[:, h * 64:(h + 1) * 64], in0=pq[:, h * 64:(h + 1) * 64], scalar=rq[:, h:h + 1],
                                                             in1=gQ[:, h * 64:(h + 1) * 64], op0=ALU.mult, op1=ALU.mult),
                     reads=[pq, rq, gQ], writes=[qn])
            for h in range(10):
                c.op("dve", lambda e: e.scalar_tensor_tensor(out=kn[:, h * 64:(h + 1) * 64], in0=pk[:, h * 64:(h + 1) * 64], scalar=rq[:, 16 + h:17 + h],
                                                             in1=gK[:, h * 64:(h + 1) * 64], op0=ALU.mult, op1=ALU.mult),
                     reads=[pk, rq, gK], writes=[kn])
            c.op("act", lambda e: e.copy(out=vst[:, sub, :].rearrange("p (h d) -> p h d", d=65)[:, :, 0:64],
                                         in_=pv[:, 0:640].rearrange("p (h d) -> p h d", d=64)), reads=[pv], writes=[vst])
            c.op("dve", lambda e: e.tensor_tensor(out=lft[:, 0:4], in0=pv[:, 640:644], in1=bfB[:], op=ALU.add), reads=[pv, bfB], writes=[lft])
            c.op("act", lambda e: e.activation(out=lft[:, 4:8], in_=lft[:, 0:4], func=AF.Exp, scale=-1.0), reads=[lft], writes=[lft])
            c.op("act", lambda e: e.activation(out=lft[:, 0:4], in_=lft[:, 4:8], func=AF.Ln, bias=1.0), reads=[lft], writes=[lft])
            c.op("dve", lambda e: e.tensor_scalar(out=lf_all[:, :, i], in0=lft[:, 0:4], scalar1=-1.0, scalar2=None, op0=ALU.mult), reads=[lft], writes=[lf_all])
            tq = T0[:].bitcast(BF16); tk = T1[:].bitcast(BF16)
            for cc in range(8):
                c.op("pe", lambda e: e.transpose(tq[:, cc * 128:(cc + 1) * 128], qn[:, cc * 128:(cc + 1) * 128], identb[:]),
                     reads=[qn, identb], writes=[T0], signal=(cc == 7))
            for cc in range(5):
                c.op("pe", lambda e: e.transpose(tk[:, cc * 128:(cc + 1) * 128], kn[:, cc * 128:(cc + 1) * 128], identb[:]),
                     reads=[kn, identb], writes=[T1], signal=(cc == 4))
            c.op("act", lambda e: e.copy(out=qTs[:, :, sub * 128:(sub + 1) * 128], in_=tq.rearrange("p (c t) -> p c t", t=128)), reads=[T0], writes=[qTs])
            c.op("dve", lambda e: e.tensor_copy(out=kTs[:, :, sub * 128:(sub + 1) * 128], in_=tk[:, 0:640].rearrange("p (c t) -> p c t", t=128)), reads=[T1], writes=[kTs])
        c.dma("pool", qT_v[:, :, q * 512:(q + 1) * 512], qTs[:], reads=[qTs], writes=[P.QT], sem=s_st[1])
        c.dma("pool", kT_v[:, :, q * 512:(q + 1) * 512], kTs[:], reads=[kTs], writes=[P.KT], sem=s_st[2])
        c.dma("pool", P.v_d[q * 512:(q + 1) * 512, :].rearrange("(s p) n -> p s n", p=128), vst[:], reads=[vst], writes=[P.VD], sem=s_st[3])
    c.barrier()
    c.release(m1)
    if getattr(build_program, "mixer_stop", 0) == 1:
        c.release(mM)
        return

    o_sb = c.sbuf("o_sb", [128, NT, D], BF16)
    m2 = c.mark()
    utri = c.sbuf("utri", [128, 128], F32); cmask = c.sbuf("cmask", [128, 4, 512], F32); dist0 = c.sbuf("dist0", [128, 512], F32)
    swM = c.sbuf("swM", [128, 5, 512], F32); swD = c.sbuf("swD", [128, 5, 512], F32)
    for t_, nm_ in ((utri, "utri"), (cmask, "cmask"), (dist0, "dist0"), (swM, "swM"), (swD, "swD")):
        c.dma("sp", t_[:], P.CD[nm_].rearrange("p (a b) -> p a b", b=512) if nm_ in ("cmask", "swM", "swD") else P.CD[nm_], reads=[P.CONSTD], writes=[t_], sem=s_const)
    sinkE = c.sbuf("sinkE", [128, 8], F32)
    c.dma("sp", sinkE[:], P.attn_sinks[l:l + 1, :].partition_broadcast(128), reads=[WCONST], writes=[sinkE], sem=s_const)
    c.op("act", lambda e: e.activation(out=sinkE[:], in_=sinkE[:], func=AF.Exp, bias=-CSH), reads=[sinkE], writes=[sinkE])

    cumK = c.sbuf("cumK", [128, 4, NT], F32)
    negck = c.sbuf("negck", [128, 4, NT], F32)
    tot = c.sbuf("tot", [128, 4, NT], F32)
    off = c.sbuf("off", [128, 4, NT], F32)
    cTs = c.sbuf("cTs", [128, 128], F32)
    S0 = c.psum("S0", [128, 512], F32); S1 = c.psum("S1", [128, 512], F32)
    OA = [c.psum(f"OA{j}", [128, 512], F32) for j in range(2)]
    G0 = c.psum("G0", [128, 512], F32); G1 = c.psum("G1", [128, 512], F32)
    lf2 = lf_all[:].rearrange("p h i -> p (h i)")
    c.op("pe", lambda e: e.matmul(S0[:, 0:4 * NT], lhsT=ones_f[:], rhs=lf2, start=True, stop=True), reads=[ones_f, lf_all], writes=[S0])
    c.op("pe", lambda e: e.matmul(S1[:, 0:4 * NT], lhsT=utri[:], rhs=lf2, start=True, stop=True), reads=[utri, lf_all], writes=[S1])
    c.op("dve", lambda e: e.tensor_copy(out=tot[:].rearrange("p h i -> p (h i)"), in_=S0[:, 0:4 * NT]), reads=[S0], writes=[tot])
    c.op("dve", lambda e: e.memset(off[:], 0.0), writes=[off])
    for i in range(1, NT):
        c.op("dve", lambda e: e.tensor_tensor(out=off[:, :, i], in0=off[:, :, i - 1], in1=tot[:, :, i - 1], op=ALU.add), reads=[off, tot], writes=[off])
    c.op("dve", lambda e: e.tensor_tensor(out=cumK[:].rearrange("p h i -> p (h i)"), in0=S1[:, 0:4 * NT], in1=off[:].rearrange("p h i -> p (h i)"), op=ALU.add),
         reads=[S1, off], writes=[cumK])
    c.op("dve", lambda e: e.tensor_scalar(out=negck[:], in0=cumK[:], scalar1=-1.0, scalar2=-CSH, op0=ALU.mult, op1=ALU.add), reads=[cumK], writes=[negck])
    c.op("pe", lambda e: e.transpose(S0[0:4 * NT, 0:128], cumK[:].rearrange("p h i -> p (h i)"), ident[:]), reads=[cumK, ident], writes=[S0])
    c.op("dve", lambda e: e.tensor_copy(out=cTs[0:4 * NT, :], in_=S0[0:4 * NT, 0:128]), reads=[S0], writes=[cTs])
    c.dma("sp", P.cumT_d.rearrange("h (i t) -> (h i) t", t=128), cTs[0:4 * NT, :], reads=[cTs], writes=[P.CUMT], sem=s_st[0])

    qTc = [c.sbuf(f"qTc{i}", [128, S], BF16) for i in range(2)]
    kTc = [c.sbuf(f"kTc{i}", [128, S], BF16) for i in range(2)]
    vaug = [c.sbuf(f"vaug{i}", [128, NT, 65], BF16) for i in range(2)]
    biasb = [c.sbuf(f"biasb{i}", [128, max(S, 2560)], F32) for i in range(2)]
    tts = [c.sbuf(f"tts{i}", [128, 512], F32) for i in range(4)]
    pex = [c.sbuf(f"pex{i}", [128, 512], BF16) for i in range(4)]
    rz = c.sbuf("rz", [128, 8], F32)
    S2 = c.psum("S2", [128, 512], F32); S3 = c.psum("S3", [128, 512], F32)
    SS = [S0, S1, S2, S3]
    cnt = [0]

    seq = []

    def attn_tile(qap, kap, extra, bias_ap, mask_ap, act_bias, vap, first, last, rd, oset, fin=None, qlo=0, qhi=512):
        k_ = cnt[0] % 4
        cnt[0] += 1
        ps = SS[k_]; tt = tts[k_]; pe_ = pex[k_]
        OAb = OA[oset]

        def A():
            c.op("pe", lambda e: e.matmul(ps[:, qlo:qhi], lhsT=kap, rhs=qap[:, qlo:qhi], start=True, stop=(extra is None)), reads=rd, writes=[ps], signal=(extra is None))
            if extra is not None:
                c.op("pe", lambda e: e.matmul(ps[:, qlo:qhi], lhsT=extra[0], rhs=extra[1][:, qlo:qhi], start=False, stop=True), reads=rd, writes=[ps])

        def B():
            c.op("dve", lambda e: e.tensor_tensor(out=tt[:, qlo:qhi], in0=ps[:, qlo:qhi], in1=bias_ap[:, qlo:qhi], op=ALU.add), reads=[ps] + rd, writes=[tt])
            if mask_ap is not None:
                c.op("dve", lambda e: e.tensor_tensor(out=tt[:, qlo:qhi], in0=tt[:, qlo:qhi], in1=mask_ap[:, qlo:qhi], op=ALU.add), reads=[tt] + rd, writes=[tt])
            c.op("act", lambda e: e.activation(out=pe_[:, qlo:qhi], in_=tt[:, qlo:qhi], func=AF.Exp, bias=act_bias), reads=[tt] + rd, writes=[pe_])

        def C():
            for j in range(qlo // 128, qhi // 128):
                c.op("pe", lambda e: e.matmul(OAb[:, j * 128:j * 128 + 65], lhsT=pe_[:, j * 128:(j + 1) * 128], rhs=vap, start=(first and j == 0), stop=last,
                                              skip_group_check=True),
                     reads=[pe_] + rd, writes=[OAb], signal=(j == qhi // 128 - 1))
            if fin is not None:
                qt, col, sink_ap = fin
                for j in range(4):
                    zc = OAb[:, j * 128 + 64:j * 128 + 65]
                    if sink_ap is not None:
                        c.op("dve", lambda e: e.tensor_tensor(out=rz[:, j:j + 1], in0=zc, in1=sink_ap, op=ALU.add), reads=[OAb, sinkE], writes=[rz])
                        c.op("dve", lambda e: e.reciprocal(out=rz[:, 4 + j:5 + j], in_=rz[:, j:j + 1]), reads=[rz], writes=[rz])
                    else:
                        c.op("dve", lambda e: e.reciprocal(out=rz[:, 4 + j:5 + j], in_=zc), reads=[OAb], writes=[rz])
                    c.op("dve", lambda e: e.tensor_scalar(out=o_sb[:, qt * 4 + j, col:col + 64], in0=OAb[:, j * 128:j * 128 + 64], scalar1=rz[:, 4 + j:5 + j],
                                                          scalar2=None, op0=ALU.mult), reads=[OAb, rz], writes=[o_sb])
        seq.append(("tile", A, B, C))

    def load_v(buf, vh, semi):
        with nc.allow_non_contiguous_dma("v head slice"):
            c.dma("sp", buf[:], P.v_d[:, vh * 65:(vh + 1) * 65].rearrange("(i p) n -> p i n", p=128), reads=[P.VD], writes=[buf], sem=s_a[semi])

    hcount = [0]
    qtc = [0]
    for h in range(4):
        b = hcount[0] % 2; hcount[0] += 1
        hp = (h % 2) * 64
        qc, kc_, vb, bb = qTc[b], kTc[b], vaug[b], biasb[b]

        def prep(h=h, b=b, hp=hp, qc=qc, kc_=kc_, vb=vb, bb=bb):
            c.dma("sp", qc[hp:hp + 64, :], P.qT_d[h * 64:(h + 1) * 64, :], reads=[P.QT], writes=[qc], sem=s_a[b])
            c.dma("sp", kc_[hp:hp + 64, :], P.kT_d[h * 64:(h + 1) * 64, :], reads=[P.KT], writes=[kc_], sem=s_a[2 + b])
            load_v(vb, h, 4 + b)
            c.dma("sp", bb[:, 0:S], P.cumT_d[h:h + 1, :].partition_broadcast(128), reads=[P.CUMT], writes=[bb], sem=s_x[b])
        seq.append(("prep", prep))
        rd = [qc, kc_, vb, bb, negck, cmask]
        for qt in range(NG):
            nk = 4 * qt + 4
            oset = qtc[0] % 2; qtc[0] += 1
            for kt in range(nk):
                j = kt - 4 * qt
                attn_tile(qc[hp:hp + 64, qt * 512:(qt + 1) * 512], kc_[hp:hp + 64, kt * 128:(kt + 1) * 128], None,
                          bb[:, qt * 512:(qt + 1) * 512], cmask[:, j, :] if j >= 0 else None, negck[:, h, kt:kt + 1],
                          vb[:, kt, :], kt == 0, kt == nk - 1, rd, oset, (qt, h * 64, None) if kt == nk - 1 else None, qlo=max(j, 0) * 128)
    for h in range(8):
        b = hcount[0] % 2; hcount[0] += 1
        hp = (h % 2) * 64
        kvh = h // 4
        qc, kc_, vb, bb = qTc[b], kTc[b], vaug[b], biasb[b]

        def prep(h=h, b=b, hp=hp, kvh=kvh, qc=qc, kc_=kc_, vb=vb, bb=bb):
            c.dma("sp", qc[hp:hp + 64, :], P.qT_d[256 + h * 64:256 + (h + 1) * 64, :], reads=[P.QT], writes=[qc], sem=s_a[b])
            c.dma("sp", kc_[hp:hp + 64, :], P.kT_d[256 + kvh * 64:256 + (kvh + 1) * 64, :], reads=[P.KT], writes=[kc_], sem=s_a[2 + b])
            load_v(vb, 4 + kvh, 4 + b)
            for jj in range(5):
                c.op("dve", lambda e: e.scalar_tensor_tensor(out=bb[:, jj * 512:(jj + 1) * 512], in0=swD[:, jj, :], scalar=-slopes[h], in1=swM[:, jj, :],
                                                             op0=ALU.mult, op1=ALU.add), reads=[swD, swM], writes=[bb])
        seq.append(("prep", prep))
        rd = [qc, kc_, vb, bb]
        for qt in range(NG):
            kts = [kt for kt in range(4 * qt - 1, 4 * qt + 4) if kt >= 0]
            oset = qtc[0] % 2; qtc[0] += 1
            for kt in kts:
                jj = kt - 4 * qt + 1
                attn_tile(qc[hp:hp + 64, qt * 512:(qt + 1) * 512], kc_[hp:hp + 64, kt * 128:(kt + 1) * 128], None,
                          bb[:, jj * 512:(jj + 1) * 512], None, -CSH, vb[:, kt, :], kt == kts[0], kt == kts[-1], rd, oset,
                          (qt, 256 + h * 64, sinkE[:, h:h + 1]) if kt == kts[-1] else None,
                          qlo=max(jj - 1, 0) * 128, qhi=min(jj + 1, 4) * 128)
    m3 = c.mark()
    ownN = c.sbuf("ownN", [128, NB, NB], F32); vmask = c.sbuf("vmask", [128, NB, NB], F32); eqB = c.sbuf("eqB", [128, NB, NB], F32)
    for t_, nm_ in ((ownN, "ownN"), (vmask, "vmask"), (eqB, "eqB")):
        c.dma("sp", t_[:], P.CD[nm_].rearrange("p (a b) -> p a b", b=NB), reads=[P.CONSTD], writes=[t_], sem=s_const)
    kms = c.sbuf("kms", [128, NB], F32); kmb = c.sbuf("kmb", [128, NB], BF16)
    gw = [c.sbuf(f"gw{i}", [128, 4 * NB + 8], F32) for i in range(2)]
    mvp = [c.sbuf(f"mvp{i}", [128, 80], F32) for i in range(2)]
    for t_ in mvp:
        c.op("dve", lambda e: e.memset(t_[:], 0.0), writes=[t_])
    GG = [G0, G1]
    for h in range(4):
        b = hcount[0] % 2; hcount[0] += 1
        hp = 0
        qc, kc_, vb, bb = qTc[b], kTc[b], vaug[b], biasb[b]
        sl = slopes[8 + h]
        R0 = 64 if hp == 0 else 48
        CP0 = 64 if hp == 0 else 32
        CPN = 16 if hp == 0 else 32
        KB, KE = (0, 80) if hp == 0 else (32, 128)

        def prep(h=h, b=b, hp=hp, qc=qc, kc_=kc_, vb=vb, bb=bb, sl=sl, R0=R0, CP0=CP0, CPN=CPN):
            c.dma("sp", qc[hp:hp + 64, :], P.qT_d[768 + h * 64:768 + (h + 1) * 64, :], reads=[P.QT], writes=[qc], sem=s_a[b])
            c.dma("sp", kc_[hp:hp + 64, :], P.kT_d[384 + h * 64:384 + (h + 1) * 64, :], reads=[P.KT], writes=[kc_], sem=s_a[2 + b])
            if hp == 0:
                c.dma("sp", kc_[64:80, :], P.CD["eselk"][16:32, :], reads=[P.CONSTD], writes=[kc_], sem=s_a[2 + b])
            else:
                c.dma("sp", kc_[32:64, :], P.CD["eselk"][0:32, :], reads=[P.CONSTD], writes=[kc_], sem=s_a[2 + b])
            load_v(vb, 6 + h, 4 + b)
            c.op("dve", lambda e: e.tensor_scalar(out=bb[:, 0:512], in0=dist0[:], scalar1=-sl, scalar2=None, op0=ALU.mult), reads=[dist0], writes=[bb])
            c.op("dve", lambda e: e.tensor_reduce(out=kms[hp:hp + 64, :], in_=kc_[hp:hp + 64, :].rearrange("p (n t) -> p n t", t=256), axis=AX.X, op=ALU.add), reads=[kc_], writes=[kms])
            c.op("dve", lambda e: e.tensor_scalar(out=kmb[hp:hp + 64, :], in0=kms[hp:hp + 64, :], scalar1=1.0 / 256, scalar2=None, op0=ALU.mult), reads=[kms], writes=[kmb])
            def gate_mm(i):
                gp = GG[i % 2]
                c.op("pe", lambda e: e.matmul(gp[:, 0:NB], lhsT=qc[hp:hp + 64, i * 128:(i + 1) * 128], rhs=kmb[hp:hp + 64, :], start=True, stop=True), reads=[qc, kmb], writes=[gp])

            def gate_rest(i):
                own = i // 2
                gp = GG[i % 2]; w = gw[i % 2]; mp = mvp[i % 2]
                gm = w[:, 0:NB]; sel = w[:, NB:2 * NB]; m8 = w[:, 4 * NB:4 * NB + 8]
                c.op("dve", lambda e: e.tensor_tensor(out=gm, in0=gp[:, 0:NB], in1=ownN[:, own, :], op=ALU.add), reads=[gp, ownN], writes=[w])
                c.op("dve", lambda e: e.max(out=m8, in_=gm), reads=[w], writes=[w])
                c.op("dve", lambda e: e.tensor_scalar(out=sel, in0=gm, scalar1=m8[:, 2:3], scalar2=None, op0=ALU.is_ge), reads=[w], writes=[w])
                c.op("dve", lambda e: e.tensor_tensor(out=sel, in0=sel, in1=vmask[:, own, :], op=ALU.mult), reads=[w, vmask], writes=[w])
                c.op("dve", lambda e: e.scalar_tensor_tensor(out=mp[:, R0:R0 + NB], in0=sel, scalar=BIG, in1=eqB[:, own, :], op0=ALU.mult, op1=ALU.add), reads=[w, eqB], writes=[mp])
                c.op("pe", lambda e: e.matmul(gp[0:R0 + 16, 128:256], lhsT=mp[:, 0:R0 + 16], rhs=ident[:], start=True, stop=True), reads=[mp, ident], writes=[gp])
                c.op("act", lambda e: e.copy(out=qc[CP0:CP0 + CPN, i * 128:(i + 1) * 128], in_=gp[CP0:CP0 + CPN, 128:256]), reads=[gp], writes=[qc])

            gate_mm(0)
            for i in range(NT):
                if i + 1 < NT:
                    gate_mm(i + 1)
                gate_rest(i)
        seq.append(("prep", prep))
        rd = [qc, kc_, vb, bb, cmask]
        for qt in range(NG):
            nk = 4 * qt + 4
            oset = qtc[0] % 2; qtc[0] += 1
            for kt in range(nk):
                j = kt - 4 * qt
                attn_tile(qc[KB:KE, qt * 512:(qt + 1) * 512], kc_[KB:KE, kt * 128:(kt + 1) * 128], None,
                          bb[:, 0:512], cmask[:, j, :] if j >= 0 else None, float(-sl * (qt * 512 - kt * 128) - CSH),
                          vb[:, kt, :], kt == 0, kt == nk - 1, rd, oset, (qt, 768 + h * 64, None) if kt == nk - 1 else None, qlo=max(j, 0) * 128)
    LOOK = getattr(build_program, "look", 3)
    pend = []

    def flush(n):
        while len(pend) > n:
            it_ = pend.pop(0)
            it_[2](); it_[3]()
    for item in seq:
        if item[0] == "prep":
            flush(0)
            item[1]()
        else:
            item[1]()
            pend.append(item)
            flush(LOOK)
    flush(0)
    c.barrier()
    c.release(m2)
    if getattr(build_program, "mixer_stop", 0) == 2:
        c.release(mM)
        return

    m4 = c.mark()
    g1B = c.sbuf("g1B", [128, D], F32)
    c.dma("sp", g1B[:], P.mod_d[l:l + 1, 2 * D:3 * D].partition_broadcast(128), reads=[P.MODD], writes=[g1B], sem=s_const)
    wg = c.sbuf("wg", [128, 8, 3072], BF16)
    wbr = c.sbuf("wbr", [128, 8, D], BF16)
    wo = c.sbuf("wo", [128, 8, D], BF16)
    st3 = [c.sbuf(f"st3_{i}", [128, D], F32) for i in range(2)]
    pc = [0]

    def load_cast(dst_ap, src_ap):
        k_ = pc[0] % 2; pc[0] += 1
        st = st3[k_]
        c.dma("sp", st[:], src_ap, reads=[WCONST], writes=[st], sem=s_w[k_])
        return st

    for kc in range(8):
        for part in range(3):
            st = load_cast(None, P.w_in[l, kc * 128:(kc + 1) * 128, 2308 + part * 1024:2308 + (part + 1) * 1024])
            c.op("act", lambda e: e.copy(out=wg[:, kc, part * 1024:(part + 1) * 1024], in_=st[:]), reads=[st], writes=[wg])
        srcbr = (P.w_br_fox[l, kc * 128:(kc + 1) * 128, :] if kc < 2 else
                 P.w_br_swa[l, (kc - 2) * 128:(kc - 1) * 128, :] if kc < 6 else P.w_br_moba[l, (kc - 6) * 128:(kc - 5) * 128, :])
        st = load_cast(None, srcbr)
        c.op("dve", lambda e: e.tensor_copy(out=wbr[:, kc, :], in_=st[:]), reads=[st], writes=[wbr])
        st = load_cast(None, P.w_out[l, kc * 128:(kc + 1) * 128, :])
        c.op("dve", lambda e: e.tensor_copy(out=wo[:, kc, :], in_=st[:]), reads=[st], writes=[wo])
    hTg = c.sbuf("hTg3", [128, 8, 512], BF16)
    oT = c.sbuf("oT", [128, 8, 512], BF16)
    mT = c.sbuf("mT", [128, 8, 512], BF16)
    sg = [c.sbuf(f"sg{i}", [128, 512], F32) for i in range(2)]
    macc = [c.sbuf(f"macc{i}", [128, 512], F32) for i in range(2)]
    xt = [c.sbuf(f"xt{i}", [128, D], F32) for i in range(2)]
    xo = [c.sbuf(f"xo{i}", [128, D], F32) for i in range(2)]
    PT = c.psum("PT", [128, 512], F32)
    PG = [c.psum(f"PG{i}", [128, 512], F32) for i in range(2)]
    PY = [c.psum(f"PY{i}", [128, 512], F32) for i in range(2)]
    PO = c.psum("PO", [128, D], F32)
    br_k = [(0, 2), (2, 6), (6, 8)]
    for q in range(NG):
        c.dma("sp", hTg[:], hT_v[:, :, q * 512:(q + 1) * 512], reads=[P.HT], writes=[hTg], sem=s_a[0])
        ptb = PT[:].bitcast(BF16)
        for cc in range(8):
            for sub in range(4):
                c.op("pe", lambda e: e.transpose(ptb[:, sub * 128:(sub + 1) * 128], o_sb[:, q * 4 + sub, cc * 128:(cc + 1) * 128], identb[:]),
                     reads=[o_sb, identb], writes=[PT], signal=(sub == 3))
            c.op("act", lambda e: e.copy(out=oT[:, cc, :], in_=ptb[:, 0:512]), reads=[PT], writes=[oT])
        for jc in range(8):
            ma = macc[jc % 2]
            for br in range(3):
                pg_ = PG[(jc * 3 + br) % 2]; py_ = PY[(jc * 3 + br) % 2]; sgt = sg[(jc * 3 + br) % 2]
                gcol = br * 1024 + jc * 128
                for kc in range(8):
                    c.op("pe", lambda e: e.matmul(pg_[:], lhsT=wg[:, kc, gcol:gcol + 128], rhs=hTg[:, kc, :], start=(kc == 0), stop=(kc == 7)),
                         reads=[wg, hTg], writes=[pg_], signal=(kc == 7))
                k0, k1 = br_k[br]
                for kc in range(k0, k1):
                    c.op("pe", lambda e: e.matmul(py_[:], lhsT=wbr[:, kc, jc * 128:(jc + 1) * 128], rhs=oT[:, kc, :], start=(kc == k0), stop=(kc == k1 - 1)),
                         reads=[wbr, oT], writes=[py_], signal=(kc == k1 - 1))
                c.op("act", lambda e: e.activation(out=sgt[:], in_=pg_[:], func=AF.Sigmoid), reads=[pg_], writes=[sgt])
                if br == 0:
                    c.op("dve", lambda e: e.tensor_tensor(out=ma[:], in0=py_[:], in1=sgt[:], op=ALU.mult), reads=[py_, sgt], writes=[ma])
                else:
                    c.op("dve", lambda e: e.tensor_tensor(out=sgt[:], in0=py_[:], in1=sgt[:], op=ALU.mult), reads=[py_, sgt], writes=[sgt])
                    if br == 1:
                        c.op("dve", lambda e: e.tensor_tensor(out=ma[:], in0=ma[:], in1=sgt[:], op=ALU.add), reads=[ma, sgt], writes=[ma])
                    else:
                        c.op("dve", lambda e: e.tensor_tensor(out=mT[:, jc, :], in0=ma[:], in1=sgt[:], op=ALU.add), reads=[ma, sgt], writes=[mT])
        for sub in range(4):
            i = q * 4 + sub
            r0 = i * 128
            xb = xt[i % 2]; ob = xo[i % 2]
            c.dma("sp", xb[:], x_src[r0:r0 + 128, :], reads=[X_SRC], writes=[xb], sem=s_x[i % 2])
            for half in range(2):
                for kc in range(8):
                    c.op("pe", lambda e: e.matmul(PO[:, half * 512:(half + 1) * 512], lhsT=mT[:, kc, sub * 128:(sub + 1) * 128], rhs=wo[:, kc, half * 512:(half + 1) * 512],
                                                  start=(kc == 0), stop=(kc == 7)), reads=[mT, wo], writes=[PO], signal=(kc == 7))
            c.op("dve", lambda e: e.tensor_tensor(out=ob[:], in0=PO[:], in1=g1B[:], op=ALU.mult), reads=[PO, g1B], writes=[ob])
            c.op("dve", lambda e: e.tensor_tensor(out=ob[:], in0=ob[:], in1=xb[:], op=ALU.add), reads=[ob, xb], writes=[ob])
            c.dma("pool", P.xres_d[r0:r0 + 128, :], ob[:], reads=[ob], writes=[P.XRES], sem=s_st[i % 2])
    c.barrier()
    c.release(mM)


def moe_layer(P):
    c, nc, l, S, NE = P.c, P.nc, P.l, P.S, P.NE
    GT = min(2048, S)
    NGRP = S // GT
    NTG = GT // 128
    NQ = GT // 512
    ident, modc, a2c = P.ident, P.modc, P.a2c
    s_const, s_x, s_w, s_st = P.s_const, P.s_x, P.s_w, P.s_st
    WCONST = P.WCONST
    x_src, X_SRC = P.x_src, P.X_SRC
    if not (P.skip_mixer and l == 0):
        x_src, X_SRC = P.xres_d, P.XRES
    x_dst, X_DST = (P.y_out, P.YOUT) if P.last else (P.xres_d, P.XRES)

    mL = c.mark()
    g2B = c.sbuf("g2B", [128, D], F32)
    c.dma("sp", g2B[:], P.mod_d[l:l + 1, 5 * D:6 * D].partition_broadcast(128), reads=[P.MODD], writes=[g2B], sem=s_const)
    yacc = c.sbuf("yacc", [128, NTG, D], F32)
    h2T = c.sbuf("h2T", [128, 8, GT], BF16)
    gates = c.sbuf("gates", [128, NTG, NE], F32)
    b1T = c.sbuf("b1T", [128, NE, 16], F32)
    b2s = c.sbuf("b2s", [NE, D], F32)
    brB = c.sbuf("brB", [128, NE], F32)
    wr = c.sbuf("wr", [128, 8, NE], F32)

    m1 = c.mark()
    b1tm = c.sbuf("b1tm", [NE, 2 * D], F32)
    pt = c.psum("pt_b1", [128, 512], F32)
    c.dma("sp", b1tm[:], P.b_exp1[l], reads=[WCONST], writes=[b1tm], sem=s_const)
    c.dma("sp", b2s[:], P.b_exp2[l], reads=[WCONST], writes=[b2s], sem=s_const)
    c.dma("sp", brB[:], P.b_router[l:l + 1, :].partition_broadcast(128), reads=[WCONST], writes=[brB], sem=s_const)
    with nc.allow_non_contiguous_dma("router weights relayout"):
        c.dma("sp", wr[:], P.w_router[l].rearrange("(kc p) e -> p kc e", p=128), reads=[WCONST], writes=[wr], sem=s_const)
    for t in range(2):
        for fc in range(8):
            src = b1tm[:, fc * 256: (fc + 1) * 256].rearrange("e (p t) -> e t p", t=2)[:, t, :]
            c.op("pe", lambda e: e.transpose(pt[:, 0:NE], src, ident[0:NE, 0:NE]), reads=[b1tm, ident], writes=[pt])
            if t == 0:
                c.op("dve", lambda e: e.tensor_copy(out=b1T[:, :, t * 8 + fc], in_=pt[:, 0:NE]), reads=[pt], writes=[b1T])
            else:
                c.op("dve", lambda e: e.tensor_scalar(out=b1T[:, :, t * 8 + fc], in0=pt[:, 0:NE], scalar1=1.0, scalar2=None, op0=ALU.add), reads=[pt], writes=[b1T])
    c.barrier()
    c.release(m1)

    for g in range(NGRP):
        tok0 = g * GT
        m2 = c.mark()
        xs = [c.sbuf(f"xs{i}", [128, D], F32) for i in range(2)]
        junk = c.sbuf("junk", [128, D], F32)
        ss = [c.sbuf(f"ss{i}", [128, 4], F32) for i in range(2)]
        xn = c.sbuf("xn", [128, 4, D], F32)
        h32 = c.sbuf("h32", [128, 8, 512], F32)
        sm = [c.sbuf(f"sm{i}", [128, 4 * NE + 16], F32) for i in range(2)]
        gTs = c.sbuf("gTs", [NE, 128], F32)
        ptr = [c.psum(f"ptr{i}", [128, 512], F32) for i in range(2)]
        pr = [c.psum(f"pr{i}", [128, 512], F32) for i in range(2)]
        pb = c.psum("pb", [128, D], F32)
        for q in range(NQ):
            for sub in range(4):
                i = q * 4 + sub
                r0 = tok0 + i * 128
                xb = xs[i % 2]; sb = ss[i % 2]
                c.dma("sp", xb[:], x_src[r0:r0 + 128, :], reads=[X_SRC], writes=[xb], sem=s_x[i % 2])
                c.op("act", lambda e: e.activation(out=junk[:], in_=xb[:], func=AF.Square, accum_out=sb[:, 0:1]),
                     reads=[xb], writes=[junk, sb])
                c.op("act", lambda e: e.activation(out=sb[:, 1:2], in_=sb[:, 0:1], func=AF.Sqrt, scale=1.0 / D, bias=EPS),
                     reads=[sb], writes=[sb])
                c.op("dve", lambda e: e.reciprocal(out=sb[:, 2:3], in_=sb[:, 1:2]), reads=[sb], writes=[sb])
                c.op("dve", lambda e: e.tensor_scalar(out=xn[:, sub, :], in0=xb[:], scalar1=sb[:, 2:3], scalar2=None, op0=ALU.mult),
                     reads=[xb, sb], writes=[xn])
            for kc in range(8):
                pp = ptr[kc % 2]
                for sub in range(4):
                    c.op("pe", lambda e: e.transpose(pp[:, sub * 128:(sub + 1) * 128], xn[:, sub, kc * 128:(kc + 1) * 128], ident[:]),
                         reads=[xn, ident], writes=[pp], signal=(sub == 3))
                c.op("act", lambda e: e.activation(out=h32[:, kc, :], in_=pp[:], func=AF.Identity,
                                                   scale=a2c[:, kc:kc + 1], bias=modc[:, l, 24 + kc:25 + kc]),
                     reads=[pp, a2c, modc], writes=[h32])
                c.op("dve", lambda e: e.tensor_copy(out=h2T[:, kc, q * 512:(q + 1) * 512], in_=h32[:, kc, :]),
                     reads=[h32], writes=[h2T])
            for sub in range(4):
                ti = q * 4 + sub
                pq = pr[sub % 2]; w = sm[sub % 2]
                for kc in range(8):
                    c.op("pe", lambda e: e.matmul(pq[:, 0:NE], lhsT=h32[:, kc, sub * 128:(sub + 1) * 128], rhs=wr[:, kc, :],
                                                  start=(kc == 0), stop=(kc == 7)), reads=[h32, wr], writes=[pq], signal=(kc == 7))
                lg = w[:, 0:NE]; mk = w[:, NE:2 * NE]; ex = w[:, 2 * NE:3 * NE]; em = w[:, 3 * NE:4 * NE]
                m8 = w[:, 4 * NE:4 * NE + 8]; nm = w[:, 4 * NE + 8:4 * NE + 9]; sv = w[:, 4 * NE + 9:4 * NE + 10]
                rs = w[:, 4 * NE + 10:4 * NE + 11]
                c.op("dve", lambda e: e.tensor_tensor(out=lg, in0=pq[:, 0:NE], in1=brB[:], op=ALU.add), reads=[pq, brB], writes=[w])
                c.op("dve", lambda e: e.max(out=m8, in_=lg), reads=[w], writes=[w])
                c.op("dve", lambda e: e.tensor_scalar(out=mk, in0=lg, scalar1=m8[:, 3:4], scalar2=None, op0=ALU.is_ge), reads=[w], writes=[w])
                c.op("dve", lambda e: e.tensor_scalar(out=nm, in0=m8[:, 0:1], scalar1=-1.0, scalar2=None, op0=ALU.mult), reads=[w], writes=[w])
                c.op("act", lambda e: e.activation(out=ex, in_=lg, func=AF.Exp, bias=nm), reads=[w], writes=[w])
                c.op("dve", lambda e: e.scalar_tensor_tensor(out=em, in0=ex, scalar=1.0, in1=mk, op0=ALU.mult, op1=ALU.mult, accum_out=sv),
                     reads=[w], writes=[w])
                c.op("dve", lambda e: e.reciprocal(out=rs, in_=sv), reads=[w], writes=[w])
                c.op("dve", lambda e: e.tensor_scalar(out=gates[:, ti, :], in0=em, scalar1=rs, scalar2=None, op0=ALU.mult),
                     reads=[w], writes=[gates])
                c.op("pe", lambda e: e.transpose(pq[0:NE, 128:256], gates[:, ti, :], ident[:]), reads=[gates, ident], writes=[pq])
                c.op("act", lambda e: e.copy(out=gTs[:], in_=pq[0:NE, 128:256]), reads=[pq], writes=[gTs])
                for half in range(2):
                    c.op("pe", lambda e: e.matmul(pb[:, half * 512:(half + 1) * 512], lhsT=gTs[:], rhs=b2s[:, half * 512:(half + 1) * 512],
                                                  start=True, stop=True), reads=[gTs, b2s], writes=[pb])
                c.op("act", lambda e: e.copy(out=yacc[:, ti, :], in_=pb[:]), reads=[pb], writes=[yacc])
        c.barrier()
        c.release(m2)

        m3 = c.mark()
        w1b = [c.sbuf(f"w1b{i}", [128, 8, 2, 512], BF16) for i in range(2)]
        w2b = [c.sbuf(f"w2b{i}", [128, 4, D], BF16) for i in range(2)]
        stg = [c.sbuf(f"stg{i}", [128, 2, D], F32) for i in range(2)]
        abuf = [c.sbuf(f"abuf{i}", [128, 4, 512], BF16) for i in range(2)]
        t1 = [c.sbuf(f"t1_{i}", [128, 512], F32) for i in range(2)]
        t2 = [c.sbuf(f"t2_{i}", [128, 512], F32) for i in range(2)]
        t3 = [c.sbuf(f"t3_{i}", [128, 512], F32) for i in range(2)]
        pg = [c.psum(f"pg{i}", [128, 512], F32) for i in range(2)]
        pl = [c.psum(f"pl{i}", [128, 512], F32) for i in range(2)]
        py = [c.psum(f"py{i}", [128, D], F32) for i in range(2)]
        units = [(e, hf) for e in range(NE) for hf in range(2)]
        pcount = [0]

        def load_piece(u, pi):
            e, hf = units[u]
            bi = u % 2
            si = pcount[0] % 2
            pcount[0] += 1
            st = stg[si]
            if pi < 4:
                kc0 = pi * 2
                src = P.w_exp1[l, e, kc0 * 128:(kc0 + 2) * 128, hf * 1024:(hf + 1) * 1024].rearrange("(kc p) n -> p kc n", p=128)
                c.dma("sp", st[:], src, reads=[WCONST], writes=[st], sem=s_w[si])
                c.op("act", lambda en: en.copy(out=w1b[bi][:, kc0:kc0 + 2, :, :],
                                               in_=st[:].rearrange("p kc (f t) -> p kc t f", t=2)),
                     reads=[st], writes=[w1b[bi]])
            else:
                fc0 = (pi - 4) * 2
                r0 = (hf * 4 + fc0) * 128
                src = P.w_exp2[l, e, r0:r0 + 256, :].rearrange("(fc p) n -> p fc n", p=128)
                c.dma("sp", st[:], src, reads=[WCONST], writes=[st], sem=s_w[si])
                c.op("act", lambda en: en.copy(out=w2b[bi][:, fc0:fc0 + 2, :], in_=st[:]), reads=[st], writes=[w2b[bi]])

        for pi in range(6):
            load_piece(0, pi)
        its = [(u, q) for u in range(len(units)) for q in range(NQ)]

        def stage_w1(it):
            u, q = its[it]
            e, hf = units[u]
            bi = u % 2
            ab = abuf[it % 2]
            for fc in range(4):
                bg = pg[fc % 2]; bl = pl[fc % 2]
                a1_, a2_, a3_ = t1[fc % 2], t2[fc % 2], t3[fc % 2]
                for kc in range(8):
                    c.op("pe", lambda en: en.matmul(bg[:], lhsT=w1b[bi][:, kc, 0, fc * 128:(fc + 1) * 128],
                                                    rhs=h2T[:, kc, q * 512:(q + 1) * 512], start=(kc == 0), stop=(kc == 7)),
                         reads=[w1b[bi], h2T], writes=[bg], signal=(kc == 7))
                for kc in range(8):
                    c.op("pe", lambda en: en.matmul(bl[:], lhsT=w1b[bi][:, kc, 1, fc * 128:(fc + 1) * 128],
                                                    rhs=h2T[:, kc, q * 512:(q + 1) * 512], start=(kc == 0), stop=(kc == 7)),
                         reads=[w1b[bi], h2T], writes=[bl], signal=(kc == 7))
                bcol = hf * 4 + fc
                c.op("dve", lambda en: en.tensor_scalar(out=a1_[:], in0=bg[:], scalar1=b1T[:, e, bcol:bcol + 1], scalar2=7.0,
                                                        op0=ALU.add, op1=ALU.min), reads=[bg, b1T], writes=[a1_])
                c.op("act", lambda en: en.activation(out=a2_[:], in_=a1_[:], func=AF.Sigmoid, scale=1.702), reads=[a1_], writes=[a2_])
                c.op("dve", lambda en: en.tensor_scalar(out=a3_[:], in0=bl[:], scalar1=b1T[:, e, 8 + bcol:9 + bcol], scalar2=8.0,
                                                        op0=ALU.add, op1=ALU.min), reads=[bl, b1T], writes=[a3_])
                c.op("dve", lambda en: en.tensor_tensor(out=a1_[:], in0=a1_[:], in1=a2_[:], op=ALU.mult), reads=[a1_, a2_], writes=[a1_])
                c.op("dve", lambda en: en.scalar_tensor_tensor(out=ab[:, fc, :], in0=a3_[:], scalar=-6.0, in1=a1_[:], op0=ALU.max, op1=ALU.mult),
                     reads=[a1_, a3_], writes=[ab])

        def stage_w2(it):
            u, q = its[it]
            e, hf = units[u]
            bi = u % 2
            ab = abuf[it % 2]
            for sub in range(4):
                ti = q * 4 + sub
                yy = py[sub % 2]
                for half in range(2):
                    for fc in range(4):
                        c.op("pe", lambda en: en.matmul(yy[:, half * 512:(half + 1) * 512], lhsT=ab[:, fc, sub * 128:(sub + 1) * 128],
                                                        rhs=w2b[bi][:, fc, half * 512:(half + 1) * 512], start=(fc == 0), stop=(fc == 3)),
                             reads=[ab, w2b[bi]], writes=[yy], signal=(fc == 3 and half == 1))
                c.op("dve", lambda en: en.scalar_tensor_tensor(out=yacc[:, ti, :], in0=yy[:], scalar=gates[:, ti, e:e + 1], in1=yacc[:, ti, :],
                                                               op0=ALU.mult, op1=ALU.add), reads=[yy, gates, yacc], writes=[yacc])

        stage_w1(0)
        for it, (u, q) in enumerate(its):
            if u + 1 < len(units):
                lo, hi = (q * 6) // NQ, ((q + 1) * 6) // NQ
                for pi in range(lo, hi):
                    load_piece(u + 1, pi)
            if it + 1 < len(its):
                stage_w1(it + 1)
            stage_w2(it)
        c.barrier()
        c.release(m3)

        m4 = c.mark()
        xt = [c.sbuf(f"xt{i}", [128, D], F32) for i in range(2)]
        xo = [c.sbuf(f"xo{i}", [128, D], F32) for i in range(2)]
        for ti in range(NTG):
            r0 = tok0 + ti * 128
            xb = xt[ti % 2]; ob = xo[ti % 2]
            c.dma("sp", xb[:], x_src[r0:r0 + 128, :], reads=[X_SRC], writes=[xb], sem=s_x[ti % 2])
            c.op("dve", lambda en: en.tensor_tensor(out=ob[:], in0=yacc[:, ti, :], in1=g2B[:], op=ALU.mult), reads=[yacc, g2B], writes=[ob])
            c.op("dve", lambda en: en.tensor_tensor(out=ob[:], in0=ob[:], in1=xb[:], op=ALU.add), reads=[ob, xb], writes=[ob])
            c.dma("pool", x_dst[r0:r0 + 128, :], ob[:], reads=[ob], writes=[X_DST], sem=s_st[ti % 2])
        c.barrier()
        c.release(m4)
    c.release(mL)


def alibi_slopes():
    return [2.0 ** (-8.0 * i / 12.0) for i in range(1, 13)]


def build_program(S=4096, L=4, NE=32, dbg=False):
    NT = S // 128
    NG = S // 512
    NB = S // 256
    nc = bass.Bass("TRN2", target_bir_lowering=False)
    c = Ctx(nc)

    def din(name, shape):
        return nc.dram_tensor(name, shape, F32, kind="ExternalInput").ap()

    x_in = din("x", [S, D]); c_in = din("c", [1, D])
    w_ada = din("w_ada", [L, D, 6 * D]); b_ada = din("b_ada", [L, 6 * D])
    norm_gain = din("norm_gain", [L, 2, D]); w_in = din("w_in", [L, D, 5380])
    b_fgate = din("b_fgate", [L, 4]); qk_gain = din("qk_gain", [L, 6, 64])
    attn_sinks = din("attn_sinks", [L, 8])
    w_br_fox = din("w_br_fox", [L, 256, D]); w_br_swa = din("w_br_swa", [L, 512, D])
    w_br_moba = din("w_br_moba", [L, 256, D]); w_out = din("w_out", [L, D, D])
    w_router = din("w_router", [L, D, NE]); b_router = din("b_router", [L, NE])
    w_exp1 = din("w_exp1", [L, NE, D, 2 * D]); b_exp1 = din("b_exp1", [L, NE, 2 * D])
    w_exp2 = din("w_exp2", [L, NE, D, D]); b_exp2 = din("b_exp2", [L, NE, D])
    y_out = nc.dram_tensor("y", [S, D], F32, kind="ExternalOutput").ap()

    def dscr(name, shape, dt):
        kind = "ExternalOutput" if dbg else "Internal"
        return nc.dram_tensor(name, shape, dt, kind=kind).ap()

    xres_d = dscr("xres_d", [S, D], F32)
    hT_d = dscr("hT_d", [D, S], BF16)
    qT_d = dscr("qT_d", [1024, S], BF16)
    kT_d = dscr("kT_d", [640, S], BF16)
    v_d = dscr("v_d", [S, 650], BF16)
    cumT_d = dscr("cumT_d", [4, S], F32)
    mod_d = dscr("mod_d", [L, 6 * D], F32)

    X_IN = Obj("x_in"); XRES = Obj("xres"); HT = Obj("hT"); QT = Obj("qT"); KT = Obj("kT")
    VD = Obj("vd"); CUMT = Obj("cumT"); MODD = Obj("modd"); YOUT = Obj("yout"); WCONST = Obj("w")

    s_const = c.new_sem(16, "s_const")
    s_x = [c.new_sem(16, f"s_x{i}") for i in range(2)]
    s_w = [c.new_sem(16, f"s_w{i}") for i in range(4)]
    s_st = [c.new_sem(16, f"s_st{i}") for i in range(4)]
    s_a = [c.new_sem(16, f"s_a{i}") for i in range(6)]

    slopes = alibi_slopes()

    ident = c.sbuf("ident", [128, 128], F32)
    identb = c.sbuf("identb", [128, 128], BF16)
    ones_f = c.sbuf("ones_f", [128, 128], F32)
    c.op("pool", lambda e: e.memset(ident[:], 0.0), writes=[ident])
    c.op("pool", lambda e: e.affine_select(out=ident[:], in_=ident[:], pattern=[[-1, 128]],
                                           compare_op=ALU.not_equal, fill=1.0, base=0,
                                           channel_multiplier=1), reads=[ident], writes=[ident])
    c.op("dve", lambda e: e.tensor_copy(out=identb[:], in_=ident[:]), reads=[ident], writes=[identb])
    c.op("pool", lambda e: e.memset(ones_f[:], 1.0), writes=[ones_f])
    modc = c.sbuf("modc", [128, L, 48], F32)
    a1c = c.sbuf("a1c", [128, 8], F32); a2c = c.sbuf("a2c", [128, 8], F32)
    ngc = c.sbuf("ngc", [128, 2, 8], F32)

    m0 = c.mark()
    condc = c.sbuf("condc", [128, 8], F32)
    ccol = c.sbuf("ccol", [128, 8], F32)
    with nc.allow_non_contiguous_dma("small vector relayout"):
        c.dma("sp", ccol[:], c_in.rearrange("o (k p) -> p (o k)", p=128), reads=[WCONST], writes=[ccol], sem=s_const)
    c.op("act", lambda e: e.activation(out=condc[:], in_=ccol[:], func=AF.Silu), reads=[ccol], writes=[condc])
    wa = [c.sbuf(f"wa{i}", [128, 6 * D], F32) for i in range(2)]
    modrow = c.sbuf("modrow", [1, 6 * D], F32)
    brow = c.sbuf("brow", [1, 6 * D], F32)
    pm = [c.psum(f"pm{i}", [128, 512], F32) for i in range(4)]
    for l in range(L):
        c.dma("sp", brow[:], b_ada[l:l + 1, :], reads=[WCONST], writes=[brow], sem=s_const)
        for half in range(3):
            for kc in range(8):
                wt = wa[kc % 2]
                c.dma("sp", wt[:, half * 2048:(half + 1) * 2048],
                      w_ada[l, kc * 128:(kc + 1) * 128, half * 2048:(half + 1) * 2048],
                      reads=[WCONST], writes=[wt], sem=s_w[kc % 2])
                for n in range(4):
                    c.op("pe", lambda e: e.matmul(pm[n][0:1, :], lhsT=condc[:, kc:kc + 1],
                                                  rhs=wt[:, half * 2048 + n * 512: half * 2048 + (n + 1) * 512],
                                                  start=(kc == 0), stop=(kc == 7)),
                         reads=[condc, wt], writes=[pm[n]], signal=(kc == 7 or n == 3))
            for n in range(4):
                cs = half * 2048 + n * 512
                c.op("dve", lambda e: e.tensor_tensor(out=modrow[:, cs:cs + 512], in0=pm[n][0:1, :],
                                                      in1=brow[:, cs:cs + 512], op=ALU.add),
                     reads=[pm[n], brow], writes=[modrow])
        c.dma("sp", mod_d[l:l + 1, :], modrow[:], reads=[modrow], writes=[MODD], sem=s_st[0])
    with nc.allow_non_contiguous_dma("small vector relayout"):
        for l in range(L):
            c.dma("sp", modc[:, l, :], mod_d[l:l + 1, :].rearrange("o (j p) -> p (o j)", p=128),
                  reads=[MODD], writes=[modc], sem=s_const)
    c.barrier()
    c.release(m0)

    CD, CONSTD = attn_consts(c, nc, NB, s_const, dscr, ident, S)
    for l in range(L):
        last = (l == L - 1)
        x_src, X_SRC = (x_in, X_IN) if l == 0 else (xres_d, XRES)

        with nc.allow_non_contiguous_dma("small vector relayout"):
            c.dma("sp", ngc[:], norm_gain[l].rearrange("t (j p) -> p t j", p=128), reads=[WCONST], writes=[ngc], sem=s_const)
        c.op("dve", lambda e: e.scalar_tensor_tensor(out=a1c[:], in0=modc[:, l, 8:16], scalar=1.0, in1=ngc[:, 0, :],
                                                     op0=ALU.add, op1=ALU.mult), reads=[modc, ngc], writes=[a1c])
        c.op("dve", lambda e: e.scalar_tensor_tensor(out=a2c[:], in0=modc[:, l, 32:40], scalar=1.0, in1=ngc[:, 1, :],
                                                     op0=ALU.add, op1=ALU.mult), reads=[modc, ngc], writes=[a2c])

        skip_mixer = getattr(build_program, 'skip_mixer', False)
        skip_moe = getattr(build_program, 'skip_moe', False)
        P = SimpleNamespace(**locals())
        if not skip_mixer:
            mixer_layer(P)
        if not skip_moe:
            moe_layer(P)

    sp = c.E["sp"]
    c._collect(sp, [YOUT], ())
    c.close()
    return nc


_NAMES = ["w_ada", "b_ada", "norm_gain", "w_in", "b_fgate", "qk_gain", "attn_sinks", "w_br_fox", "w_br_swa",
          "w_br_moba", "w_out", "w_router", "b_router", "w_exp1", "b_exp1", "w_exp2", "b_exp2"]


def kernel(**inputs):
    x = np.ascontiguousarray(np.asarray(inputs["x"], dtype=np.float32))
    cvec = np.ascontiguousarray(np.asarray(inputs["c"], dtype=np.float32))
    B, S, _ = x.shape
    L = int(np.asarray(inputs["w_ada"]).shape[0])
    NE = int(np.asarray(inputs["w_router"]).shape[2])
    shared = {k: np.ascontiguousarray(np.asarray(inputs[k], dtype=np.float32)) for k in _NAMES}
    nc = build_program(S=S, L=L, NE=NE)
    in_maps = []
    for b in range(B):
        m = {"x": x[b], "c": cvec[b:b + 1]}
        m.update(shared)
        in_maps.append(m)
    res = run_bass_kernel_spmd(nc, in_maps, core_ids=list(range(B)))
    return np.stack([np.asarray(r["y"], dtype=np.float32) for r in res.results], axis=0)
```
